# Optimizing a Trainium2 kernel written in Bass

```python
import math
import jax, jax.numpy as jnp
from jax import lax
import numpy as np

D_MODEL = 1024
BATCH = 4
SEQ = 8192
DEPTH = 1

RG_WIDTH = 1024
RG_BLOCKS = 4
RG_BLOCK_DIM = RG_WIDTH // RG_BLOCKS
RG_C = 8.0
CONV_WIDTH = 4
DN_HEADS = 8
DN_DK = 128
DN_DV = 128
DN_CHUNK = 64
N_GROUPS = 4
EXPERTS_PER_GROUP = 8
N_EXPERTS = N_GROUPS * EXPERTS_PER_GROUP
TOP_K = 2
D_EXPERT = 512
MOE_BLOCK = 128
NORM_EPS = 1e-6
IN_SIZES = (RG_WIDTH, RG_WIDTH, DN_HEADS * DN_DK, DN_HEADS * DN_DK, DN_HEADS * DN_DV, DN_HEADS * DN_DV, DN_HEADS, DN_HEADS, D_MODEL, D_MODEL)
D_IN = sum(IN_SIZES)
DN_CONV_CH = 2 * DN_HEADS * DN_DK + DN_HEADS * DN_DV

kernel_name = "hybrid_rglru_gdn_hmoe_adaln"


def rms_norm(x, w):
    xf = x.astype(jnp.float32)
    y = xf * lax.rsqrt(jnp.mean(xf * xf, axis=-1, keepdims=True) + NORM_EPS)
    return (y * w.astype(jnp.float32)).astype(x.dtype)


def l2_norm(t):
    return t * lax.rsqrt(jnp.sum(t * t, axis=-1, keepdims=True) + NORM_EPS)


def split_cols(t, sizes):
    idx = np.cumsum(sizes)[:-1].tolist()
    return jnp.split(t, idx, axis=-1)


def causal_conv(x, w):
    k_width, ch = w.shape
    return lax.conv_general_dilated(x, w.astype(x.dtype)[:, None, :], window_strides=(1,), padding=[(k_width - 1, 0)], dimension_numbers=('NWC', 'WIO', 'NWC'), feature_group_count=ch)


def rg_lru(xr, w_a, b_a, w_x, b_x, lam):
    B, S, _ = xr.shape
    f32 = jnp.float32
    xf = xr.astype(f32)
    xb = xf.reshape(B, S, RG_BLOCKS, RG_BLOCK_DIM)
    r = jax.nn.sigmoid(jnp.einsum('bsgi,gij->bsgj', xb, w_a.astype(f32)) + b_a.astype(f32)).reshape(B, S, RG_WIDTH)
    i = jax.nn.sigmoid(jnp.einsum('bsgi,gij->bsgj', xb, w_x.astype(f32)) + b_x.astype(f32)).reshape(B, S, RG_WIDTH)
    log_a = -RG_C * r * jax.nn.softplus(-lam.astype(f32))
    a = jnp.exp(log_a)
    b = jnp.sqrt(-jnp.expm1(2.0 * log_a)) * (i * xf)

    def combine(left, right):
        a1, b1 = left
        a2, b2 = right
        return a1 * a2, a2 * b1 + b2

    _, h = lax.associative_scan(combine, (a, b), axis=1)
    return h


def chunk_gated_delta_rule(q, k, v, beta, g):
    B, S, H, DK = q.shape
    DV = v.shape[-1]
    C = DN_CHUNK
    N = S // C

    def chunks(t):
        return jnp.moveaxis(t.reshape(B, N, C, H, -1), 3, 1)

    q, k, v = chunks(q), chunks(k), chunks(v)
    beta = jnp.moveaxis(beta.reshape(B, N, C, H), 3, 1)
    g = jnp.cumsum(jnp.moveaxis(g.reshape(B, N, C, H), 3, 1), axis=-1)
    causal = jnp.tril(jnp.ones((C, C), bool))
    strict = jnp.tril(jnp.ones((C, C), bool), -1)
    decay = jnp.exp(jnp.where(causal, g[..., :, None] - g[..., None, :], -jnp.inf))
    kb = k * beta[..., None]
    a_mat = jnp.where(strict, jnp.einsum('bhnik,bhnjk->bhnij', kb, k) * decay, 0.0)
    lower = a_mat + jnp.eye(C, dtype=a_mat.dtype)
    rhs = jnp.concatenate([v * beta[..., None], kb * jnp.exp(g)[..., None]], axis=-1)
    sol = lax.linalg.triangular_solve(lower, rhs, left_side=True, lower=True, unit_diagonal=True)
    u, w = sol[..., :DV], sol[..., DV:]
    qk = jnp.where(causal, jnp.einsum('bhnik,bhnjk->bhnij', q, k) * decay, 0.0)
    q_dec = q * jnp.exp(g)[..., None]
    k_dec = k * jnp.exp(g[..., -1:] - g)[..., None]
    g_end = jnp.exp(g[..., -1])

    def step(state, inp):
        q_c, k_c, u_c, w_c, qk_c, ge = inp
        v_new = u_c - jnp.einsum('bhck,bhkv->bhcv', w_c, state)
        o_c = jnp.einsum('bhck,bhkv->bhcv', q_c, state) + jnp.einsum('bhij,bhjv->bhiv', qk_c, v_new)
        state = state * ge[..., None, None] + jnp.einsum('bhck,bhcv->bhkv', k_c, v_new)
        return state, o_c

    xs = tuple(jnp.moveaxis(t, 2, 0) for t in (q_dec, k_dec, u, w, qk, g_end))
    state0 = jnp.zeros((B, H, DK, DV), q.dtype)
    _, o = lax.scan(step, state0, xs)
    return o.transpose(1, 0, 3, 2, 4).reshape(B, S, H, DV)


def hybrid_mixer(h, w_in, rg_conv_w, rg_conv_b, rg_gate_a_w, rg_gate_a_b, rg_gate_x_w, rg_gate_x_b, rg_lambda, dn_conv_w, dn_a_log, dn_dt_bias, dn_norm_w, w_branch_rg, w_branch_dn, w_out):
    B, S, _ = h.shape
    f32 = jnp.float32
    proj = h @ w_in
    rg_x, rg_y, q, k, v, z, beta_in, alpha_in, gate_rg, gate_dn = split_cols(proj, IN_SIZES)
    rg_x = causal_conv(rg_x, rg_conv_w) + rg_conv_b
    rg_h = rg_lru(rg_x, rg_gate_a_w, rg_gate_a_b, rg_gate_x_w, rg_gate_x_b, rg_lambda)
    y_rg = (rg_h * jax.nn.gelu(rg_y.astype(f32))).astype(h.dtype)
    qkv = jax.nn.silu(causal_conv(jnp.concatenate([q, k, v], axis=-1), dn_conv_w)).astype(f32)
    q, k, v = split_cols(qkv, (DN_HEADS * DN_DK, DN_HEADS * DN_DK, DN_HEADS * DN_DV))
    q = l2_norm(q.reshape(B, S, DN_HEADS, DN_DK)) * (DN_DK ** -0.5)
    k = l2_norm(k.reshape(B, S, DN_HEADS, DN_DK))
    v = v.reshape(B, S, DN_HEADS, DN_DV)
    beta = jax.nn.sigmoid(beta_in.astype(f32))
    g = -jnp.exp(dn_a_log.astype(f32)) * jax.nn.softplus(alpha_in.astype(f32) + dn_dt_bias.astype(f32))
    o = chunk_gated_delta_rule(q, k, v, beta, g)
    o = rms_norm(o, dn_norm_w) * jax.nn.silu(z.astype(f32).reshape(B, S, DN_HEADS, DN_DV))
    y_dn = o.reshape(B, S, DN_HEADS * DN_DV).astype(h.dtype)
    merged = jax.nn.sigmoid(gate_rg) * (y_rg @ w_branch_rg) + jax.nn.sigmoid(gate_dn) * (y_dn @ w_branch_dn)
    return merged @ w_out


def hier_moe(h, w_group, b_group, w_router, b_router, w_gate, w_up, w_down):
    B, S, D = h.shape
    T = B * S
    f32 = jnp.float32
    xf = h.reshape(T, D)
    glog = (xf @ w_group).astype(f32) + b_group.astype(f32)
    gprob = jax.nn.softmax(glog, axis=-1)
    gsel = jnp.argmax(glog, axis=-1)
    p_g = jnp.take_along_axis(gprob, gsel[:, None], axis=-1)
    elog = ((xf @ w_router).astype(f32) + b_router.astype(f32)).reshape(T, N_GROUPS, EXPERTS_PER_GROUP)
    elog_g = jnp.take_along_axis(elog, gsel[:, None, None], axis=1)[:, 0]
    top_v, top_i = lax.top_k(elog_g, TOP_K)
    wts = jax.nn.softmax(top_v, axis=-1) * p_g
    eid = gsel[:, None] * EXPERTS_PER_GROUP + top_i
    A = T * TOP_K
    e_flat = eid.reshape(A)
    w_flat = wts.reshape(A)
    tok = jnp.arange(A, dtype=jnp.int32) // TOP_K
    order = jnp.argsort(e_flat)
    e_sorted = e_flat[order]
    counts = jnp.bincount(e_flat, length=N_EXPERTS)
    padded = (counts + MOE_BLOCK - 1) // MOE_BLOCK * MOE_BLOCK
    pad_end = jnp.cumsum(padded)
    pad_start = pad_end - padded
    start = jnp.cumsum(counts) - counts
    dest = pad_start[e_sorted] + (jnp.arange(A) - start[e_sorted])
    P = (A + N_EXPERTS * (MOE_BLOCK - 1) + MOE_BLOCK - 1) // MOE_BLOCK * MOE_BLOCK
    NB = P // MOE_BLOCK
    slot_tok = jnp.full((P,), T, jnp.int32).at[dest].set(tok[order])
    slot_w = jnp.zeros((P,), f32).at[dest].set(w_flat[order])
    blk_e = jnp.minimum(jnp.searchsorted(pad_end, jnp.arange(NB) * MOE_BLOCK, side='right'), N_EXPERTS - 1)
    x_pad = jnp.concatenate([xf, jnp.zeros((1, D), xf.dtype)], axis=0)
    xb = x_pad[slot_tok].reshape(NB, MOE_BLOCK, D)

    def expert_block(args):
        xblk, e = args
        return (jax.nn.silu(xblk @ w_gate[e]) * (xblk @ w_up[e])) @ w_down[e]

    yb = lax.map(expert_block, (xb, blk_e)).reshape(P, D)
    y = jax.ops.segment_sum(yb * slot_w[:, None].astype(yb.dtype), slot_tok, num_segments=T + 1)[:T]
    return y.reshape(B, S, D)


def setup_inputs(seed: int = 0) -> dict:
    key = jax.random.key(seed)
    ks = jax.random.split(key, 32)
    f32 = jnp.float32
    L, D, H = DEPTH, D_MODEL, DN_HEADS

    def nrm(k, shape, scale):
        return jax.random.normal(k, shape, f32) * scale

    a0 = jax.random.uniform(ks[12], (L, RG_WIDTH), f32, 0.9, 0.999)
    root = a0 ** (1.0 / RG_C)
    dt = jnp.exp(jax.random.uniform(ks[15], (L, H), f32, math.log(1e-3), math.log(1e-1)))
    return {
        'x': nrm(ks[0], (BATCH, SEQ, D), 1.0),
        'c': nrm(ks[1], (BATCH, D), 1.0),
        'w_ada': nrm(ks[2], (L, D, 6 * D), D ** -0.5),
        'b_ada': nrm(ks[3], (L, 6 * D), 0.01),
        'norm1_w': 1.0 + nrm(ks[4], (L, D), 0.01),
        'w_in': nrm(ks[5], (L, D, D_IN), D ** -0.5),
        'rg_conv_w': nrm(ks[6], (L, CONV_WIDTH, RG_WIDTH), CONV_WIDTH ** -0.5),
        'rg_conv_b': nrm(ks[7], (L, RG_WIDTH), 0.01),
        'rg_gate_a_w': nrm(ks[8], (L, RG_BLOCKS, RG_BLOCK_DIM, RG_BLOCK_DIM), RG_BLOCK_DIM ** -0.5),
        'rg_gate_a_b': nrm(ks[9], (L, RG_BLOCKS, RG_BLOCK_DIM), 0.01),
        'rg_gate_x_w': nrm(ks[10], (L, RG_BLOCKS, RG_BLOCK_DIM, RG_BLOCK_DIM), RG_BLOCK_DIM ** -0.5),
        'rg_gate_x_b': nrm(ks[11], (L, RG_BLOCKS, RG_BLOCK_DIM), 0.01),
        'rg_lambda': jnp.log(root) - jnp.log1p(-root),
        'dn_conv_w': nrm(ks[13], (L, CONV_WIDTH, DN_CONV_CH), CONV_WIDTH ** -0.5),
        'dn_a_log': jnp.log(jax.random.uniform(ks[14], (L, H), f32, 1.0, 16.0)),
        'dn_dt_bias': dt + jnp.log(-jnp.expm1(-dt)),
        'dn_norm_w': 1.0 + nrm(ks[16], (L, DN_DV), 0.01),
        'w_branch_rg': nrm(ks[17], (L, RG_WIDTH, D), RG_WIDTH ** -0.5),
        'w_branch_dn': nrm(ks[18], (L, H * DN_DV, D), (H * DN_DV) ** -0.5),
        'w_out': nrm(ks[19], (L, D, D), D ** -0.5),
        'norm2_w': 1.0 + nrm(ks[20], (L, D), 0.01),
        'moe_w_group': nrm(ks[21], (L, D, N_GROUPS), D ** -0.5),
        'moe_b_group': nrm(ks[22], (L, N_GROUPS), 0.01),
        'moe_w_router': nrm(ks[23], (L, D, N_EXPERTS), D ** -0.5),
        'moe_b_router': nrm(ks[24], (L, N_EXPERTS), 0.01),
        'moe_w_gate': nrm(ks[25], (L, N_EXPERTS, D, D_EXPERT), D ** -0.5),
        'moe_w_up': nrm(ks[26], (L, N_EXPERTS, D, D_EXPERT), D ** -0.5),
        'moe_w_down': nrm(ks[27], (L, N_EXPERTS, D_EXPERT, D), D_EXPERT ** -0.5),
        'final_norm_w': 1.0 + nrm(ks[28], (D,), 0.01),
    }


def reference(x, c, w_ada, b_ada, norm1_w, w_in, rg_conv_w, rg_conv_b, rg_gate_a_w, rg_gate_a_b, rg_gate_x_w, rg_gate_x_b, rg_lambda, dn_conv_w, dn_a_log, dn_dt_bias, dn_norm_w, w_branch_rg, w_branch_dn, w_out, norm2_w, moe_w_group, moe_b_group, moe_w_router, moe_b_router, moe_w_gate, moe_w_up, moe_w_down, final_norm_w):
    c_act = jax.nn.silu(c)
    for l in range(DEPTH):
        mod = c_act @ w_ada[l] + b_ada[l]
        shift1, scale1, gate1, shift2, scale2, gate2 = jnp.split(mod[:, None, :], 6, axis=-1)
        h = rms_norm(x, norm1_w[l]) * (1.0 + scale1) + shift1
        x = x + gate1 * hybrid_mixer(h, w_in[l], rg_conv_w[l], rg_conv_b[l], rg_gate_a_w[l], rg_gate_a_b[l], rg_gate_x_w[l], rg_gate_x_b[l], rg_lambda[l], dn_conv_w[l], dn_a_log[l], dn_dt_bias[l], dn_norm_w[l], w_branch_rg[l], w_branch_dn[l], w_out[l])
        h = rms_norm(x, norm2_w[l]) * (1.0 + scale2) + shift2
        x = x + gate2 * hier_moe(h, moe_w_group[l], moe_b_group[l], moe_w_router[l], moe_b_router[l], moe_w_gate[l], moe_w_up[l], moe_w_down[l])
    return rms_norm(x, final_norm_w)
```

```python
import numpy as np
from contextlib import ExitStack
import concourse.bass as bass
import concourse.mybir as mybir
from concourse.bass_utils import run_bass_kernel_spmd

F32 = mybir.dt.float32
BF16 = mybir.dt.bfloat16
AF = mybir.ActivationFunctionType
ALU = mybir.AluOpType
AX = mybir.AxisListType

D = 1024
D_IN = 8208
OFF_RGX, OFF_RGY, OFF_Q, OFF_K, OFF_V, OFF_Z, OFF_BA, OFF_GRG, OFF_GDN = 0, 1024, 2048, 3072, 4096, 5120, 6144, 6160, 7184
NE = 32
DE = 512
EPS = 1e-6
TT = 256
CH = 64
NCH = TT // CH


class Buf:
    __slots__ = ("w", "r")

    def __init__(self):
        self.w = None
        self.r = {}


class T:
    def __init__(self, t):
        self.t = t
        self.b = Buf()

    def __getitem__(self, k):
        return self.t[k]


class TV:
    def __init__(self, t, lo, hi):
        self.t = t; self.lo = lo; self.hi = hi
        self.b = Buf()

    def __getitem__(self, k):
        assert k == slice(None)
        return self.t[:, self.lo:self.hi]


class TV3:
    def __init__(self, parent, sub):
        self.p = parent; self.sub = sub
        self.b = parent.b

    def __getitem__(self, k):
        assert k == slice(None)
        return self.p.t[:, self.sub, :]


class SemCounter:
    def __init__(self, nc, stack, name):
        self.h = stack.enter_context(nc.semaphore(name))
        self.n = 0


class Sync:
    def __init__(self, nc, stack):
        self.nc = nc
        self.eng = {"pe": nc.tensor, "act": nc.scalar, "dve": nc.vector, "pool": nc.gpsimd, "sp": nc.sync}
        self.sem = {k: stack.enter_context(nc.semaphore("s_" + k)) for k in ("pe", "act", "dve", "pool")}
        self.cnt = {k: 0 for k in self.sem}
        self.seen = {k: {} for k in self.eng}
        self.dsems = []
        self.stack = stack

    def newsem(self, name):
        s = SemCounter(self.nc, self.stack, name)
        self.dsems.append(s)
        return s

    def _need(self, e, tok, waits):
        if tok is None:
            return
        k, v = tok
        if k == e and e == "pe":
            return
        if self.seen[e].get(k, 0) < v:
            waits[k] = max(waits.get(k, 0), v)

    def _waits(self, e, reads, writes):
        waits = {}
        for b in reads:
            self._need(e, b.b.w, waits)
        for b in writes:
            self._need(e, b.b.w, waits)
            for k, v in b.b.r.items():
                self._need(e, (k, v), waits)
        h = self.eng[e]
        for k, v in waits.items():
            h.wait_ge(self.sem[k] if isinstance(k, str) else k, v)
            self.seen[e][k] = v
        return h

    def op(self, e, fn, R=(), W=()):
        h = self._waits(e, R, W)
        ins = fn(h)
        self.cnt[e] += 1
        ins.then_inc(self.sem[e], 1)
        tok = (e, self.cnt[e])
        for b in R:
            b.b.r[e] = self.cnt[e]
        for b in W:
            b.b.w = tok
            b.b.r = {}
        return tok

    def group(self, e, fns, R=(), W=()):
        h = self._waits(e, R, W)
        ins = None
        for fn in fns:
            ins = fn(h)
        self.cnt[e] += 1
        ins.then_inc(self.sem[e], 1)
        tok = (e, self.cnt[e])
        for b in R:
            b.b.r[e] = self.cnt[e]
        for b in W:
            b.b.w = tok
            b.b.r = {}
        return tok

    def dma(self, out, in_, sem, R=(), W=(), q="sp", **kw):
        h = self._waits(q, R, W)
        sem.n += 16
        h.dma_start(out=out, in_=in_, **kw).then_inc(sem.h, 16)
        tok = (sem.h, sem.n)
        for b in R:
            b.b.r[sem.h] = sem.n
        for b in W:
            b.b.w = tok
            b.b.r = {}
        return tok

    def barrier(self):
        for e, h in self.eng.items():
            for k in self.sem:
                if k != e and self.seen[e].get(k, 0) < self.cnt[k]:
                    h.wait_ge(self.sem[k], self.cnt[k])
                    self.seen[e][k] = self.cnt[k]
            for s in self.dsems:
                if s.n and self.seen[e].get(s.h, 0) < s.n:
                    h.wait_ge(s.h, s.n)
                    self.seen[e][s.h] = s.n


def build(NP, NO, phases=3, dbg=False, cut=99):
    assert NP % TT == 0 and NO % TT == 0
    nc = bass.Bass("TRN2", target_bir_lowering=False)

    def din(name, shape, dt=F32):
        return nc.dram_tensor(name, list(shape), dt, kind="ExternalInput").ap()

    def dscr(name, shape, dt):
        return nc.dram_tensor(name, list(shape), dt, kind="Internal").ap()

    xp = din("xp", [NP, D]); xo = din("xo", [NO, D]); flag_d = din("flag", [128, 1]); ccol_d = din("ccol", [128, 8])
    w_ada = din("w_ada", [D, 6 * D]); b_ada_rep = din("b_ada_rep", [128, 6 * D])
    n1w_d = din("n1w_col", [128, 8]); n2w_d = din("n2w_col", [128, 8]); fnw_d = din("fnw_rep", [128, D])
    w_in = din("w_in", [D, D_IN])
    rgcw_d = din("rgcw", [128, 8, 4]); rgcb_d = din("rgcb", [128, 8]); dncw_d = din("dncw", [128, 24, 4])
    rgaw_d = din("rga_w", [128, 8, 256]); rgxw_d = din("rgx_w", [128, 8, 256])
    rgab_d = din("rga_b", [128, 8]); rgxb_d = din("rgx_b", [128, 8]); lam_d = din("lam", [128, 8])
    alog_d = din("alog_rep", [64, 8]); dtb_d = din("dtb_rep", [64, 8]); dnw_d = din("dnw_rep", [64, D])
    w_brg = din("w_brg", [D, D]); w_bdn = din("w_bdn", [D, D]); w_out = din("w_out", [D, D])
    w_r36 = din("w_r36", [D, 36]); b_r36_d = din("b_r36_rep", [128, 36])
    moe_wg = din("moe_wg", [NE, D, DE]); moe_wu = din("moe_wu", [NE, D, DE]); moe_wd = din("moe_wd", [NE, DE, D])
    c_ident = din("c_ident", [128, 128]); c_ut = din("c_ut", [64, 64]); c_maskgt = din("c_maskgt", [64, 64])
    c_neg = din("c_neg", [64, 64]); c_strict = din("c_strict", [64, 64])
    out_d = nc.dram_tensor("out", [NO, D], F32, kind="ExternalOutput").ap()
    w_in_bf = dscr("w_in_bf", [D, D_IN], BF16)
    yrgT_d = dscr("yrgT", [D, NO], BF16)
    ydn_d = dscr("ydn", [NO, D], BF16)
    x2_d = dscr("x2", [NO, D], F32)
    h2T_d = dscr("h2T", [D, NO], BF16)
    wte_d = dscr("wte", [NO, NE], F32)
    dbg_out = {}
    if dbg:
        dbg_out["d_yrgT"] = nc.dram_tensor("d_yrgT", [D, NO], BF16, kind="ExternalOutput").ap()
        dbg_out["d_ydn"] = nc.dram_tensor("d_ydn", [NO, D], BF16, kind="ExternalOutput").ap()
        dbg_out["d_x2"] = nc.dram_tensor("d_x2", [NO, D], F32, kind="ExternalOutput").ap()
        dbg_out["d_wte"] = nc.dram_tensor("d_wte", [NO, NE], F32, kind="ExternalOutput").ap()

    with ExitStack() as st:
        S = Sync(nc, st)
        op, dma = S.op, S.dma

        def sb(stk, name, shape, dt=F32):
            return T(stk.enter_context(nc.sbuf_tensor("s_" + name, list(shape), dt)))

        def ps(stk, name, shape, dt=F32):
            return T(stk.enter_context(nc.psum_tensor("p_" + name, list(shape), dt)))

        st.enter_context(nc.Block())
        ld = S.newsem("ld")
        so = [S.newsem("so%d" % i) for i in range(4)]

        ident_f = sb(st, "ident_f", [128, 128]); ident_b = sb(st, "ident_b", [128, 128], BF16)
        ones_b = sb(st, "ones_b", [128, 128], BF16); ones_f = sb(st, "ones_f", [64, 128])
        flag = sb(st, "flag", [128, 1])
        cols = sb(st, "cols", [128, 4, 8])
        A1 = sb(st, "A1", [128, 8]); A2 = sb(st, "A2", [128, 8]); n1w = sb(st, "n1w", [128, 8]); n2w = sb(st, "n2w", [128, 8])
        gate1 = sb(st, "gate1", [128, D]); gate2 = sb(st, "gate2", [128, D]); fnw = sb(st, "fnw", [128, D])
        for (t_, d_) in ((ident_f, c_ident), (flag, flag_d), (n1w, n1w_d), (n2w, n2w_d), (fnw, fnw_d)):
            dma(t_[:], d_, ld, W=[t_])
        S.barrier()
        op("dve", lambda h: h.tensor_copy(out=ident_b[:], in_=ident_f[:]), R=[ident_f], W=[ident_b])
        op("pool", lambda h: h.memset(ones_b[:], 1.0), W=[ones_b])
        op("pool", lambda h: h.memset(ones_f[:], 1.0), W=[ones_f])

        with ExitStack() as s0:
            ccol = sb(s0, "ccol", [128, 8]); crep = sb(s0, "crep", [128, 8, 128])
            wsl = [sb(s0, "wsl%d" % i, [128, 8, 512]) for i in range(2)]
            wsem = [S.newsem("wsl%d" % i) for i in range(2)]
            bsl = sb(s0, "bsl", [128, 512]); mtmp = sb(s0, "mtmp", [128, 4, 128]); mt2 = sb(s0, "mt2", [128, 4, 128])
            pm = ps(s0, "pm", [128, 512])
            bsem = S.newsem("bsl")
            dma(ccol[:], ccol_d, ld, W=[ccol])
            S.barrier()
            op("act", lambda h: h.activation(out=ccol[:], in_=ccol[:], func=AF.Silu), R=[ccol], W=[ccol])
            op("dve", lambda h: h.tensor_copy(out=crep[:], in_=ccol[:].unsqueeze(2).to_broadcast([128, 8, 128])), R=[ccol], W=[crep])
            for s in range(12):
                w_ = wsl[s % 2]
                dma(w_[:], w_ada[:, s * 512:(s + 1) * 512].rearrange("(kc p) n -> p kc n", p=128), wsem[s % 2], W=[w_])
                dma(bsl[:], b_ada_rep[:, s * 512:(s + 1) * 512], bsem, W=[bsl])
                S.group("pe", [lambda h, kc=kc: h.matmul(pm[:], lhsT=crep[:, kc, :], rhs=w_[:, kc, :], start=(kc == 0), stop=(kc == 7)) for kc in range(8)], R=[crep, w_], W=[pm])
                v, half = s // 2, s % 2
                if v in (2, 5):
                    g_ = gate1 if v == 2 else gate2
                    op("dve", lambda h: h.tensor_tensor(out=g_[:, half * 512:(half + 1) * 512], in0=pm[:], in1=bsl[:], op=ALU.add), R=[pm, bsl], W=[g_])
                else:
                    ci = {0: 0, 1: 1, 3: 2, 4: 3}[v]
                    op("dve", lambda h: h.tensor_tensor(out=mtmp[:].rearrange("p a b -> p (a b)"), in0=pm[:], in1=bsl[:], op=ALU.add), R=[pm, bsl], W=[mtmp])
                    op("pool", lambda h: h.tensor_tensor(out=mt2[:], in0=mtmp[:], in1=ident_f[:].unsqueeze(1).to_broadcast([128, 4, 128]), op=ALU.mult), R=[mtmp, ident_f], W=[mt2])
                    op("dve", lambda h: h.reduce_sum(out=cols[:, ci, half * 4:(half + 1) * 4], in_=mt2[:], axis=AX.X), R=[mt2], W=[cols])
            op("dve", lambda h: h.scalar_tensor_tensor(out=A1[:], in0=cols[:, 1, :], scalar=1.0, in1=n1w[:], op0=ALU.add, op1=ALU.mult), R=[cols, n1w], W=[A1])
            op("dve", lambda h: h.scalar_tensor_tensor(out=A2[:], in0=cols[:, 3, :], scalar=1.0, in1=n2w[:], op0=ALU.add, op1=ALU.mult), R=[cols, n2w], W=[A2])
            S.barrier()

        with ExitStack() as s0:
          if cut >= 2:
            wf = [sb(s0, "wf%d" % i, [128, 8, 256]) for i in range(4)]
            wb = [sb(s0, "wb%d" % i, [128, 8, 256], BF16) for i in range(4)]
            lsem = [S.newsem("wfl%d" % i) for i in range(4)]
            nsl = (D_IN + 255) // 256
            for s in range(nsl):
                c0 = s * 256; cw = min(256, D_IN - c0)
                f_, b_ = wf[s % 4], wb[s % 4]
                dma(f_[:, :, 0:cw], w_in[:, c0:c0 + cw].rearrange("(kc p) n -> p kc n", p=128), lsem[s % 4], W=[f_])
                op(("dve", "pool", "dve", "act")[s % 4], (lambda h: h.activation(out=b_[:, :, 0:cw], in_=f_[:, :, 0:cw], func=AF.Copy)) if s % 4 == 3 else (lambda h: h.tensor_copy(out=b_[:, :, 0:cw], in_=f_[:, :, 0:cw])), R=[f_], W=[b_])
                dma(w_in_bf[:, c0:c0 + cw].rearrange("(kc p) n -> p kc n", p=128), b_[:, :, 0:cw], so[s % 4], R=[b_])
            S.barrier()

        def norm_to_hT(xt, junk, ss, xn, pT, hT, col0, Acol, Bcol_ap, tmpf):
            op("act", lambda h: h.activation(out=junk[:], in_=xt[:], func=AF.Square, scale=1.0 / 32, accum_out=ss[:]), R=[xt], W=[junk, ss])
            op("dve", lambda h: h.tensor_scalar_add(out=ss[:], in0=ss[:], scalar1=EPS), R=[ss], W=[ss])
            op("act", lambda h: h.activation(out=ss[:], in_=ss[:], func=AF.Sqrt), R=[ss], W=[ss])
            op("dve", lambda h: h.reciprocal(out=ss[:], in_=ss[:]), R=[ss], W=[ss])
            op("dve", lambda h: h.tensor_scalar_mul(out=xn[:], in0=xt[:], scalar1=ss[:]), R=[xt, ss], W=[xn])
            S.group("pe", [lambda h, kc=kc: h.transpose(out=pT[:, kc * 128:(kc + 1) * 128], in_=xn[:, kc * 128:(kc + 1) * 128], identity=ident_b[:]) for kc in range(8)], R=[xn, ident_b], W=[pT])
            op("dve", lambda h: h.tensor_tensor(out=tmpf[:], in0=pT[:].rearrange("p (a b) -> p a b", a=8), in1=Acol[:].unsqueeze(2).to_broadcast([128, 8, 128]), op=ALU.mult), R=[pT, Acol], W=[tmpf])
            op("pool", lambda h: h.tensor_tensor(out=hT[:, :, col0:col0 + 128], in0=tmpf[:], in1=Bcol_ap.unsqueeze(2).to_broadcast([128, 8, 128]), op=ALU.add), R=[tmpf, cols], W=[hT])

        with ExitStack() as s1:
            rgcw = sb(s1, "rgcw", [128, 8, 4]); rgcb = sb(s1, "rgcb", [128, 8]); dncw = sb(s1, "dncw", [128, 24, 4])
            rgab = sb(s1, "rgab", [128, 8]); rgxb = sb(s1, "rgxb", [128, 8]); lamc = sb(s1, "lamc", [128, 8])
            rgw_f = sb(s1, "rgw_f", [128, 8, 256])
            rgaw = sb(s1, "rgaw", [128, 8, 256], BF16); rgxw = sb(s1, "rgxw", [128, 8, 256], BF16)
            alog = sb(s1, "alog", [64, 8]); dtb = sb(s1, "dtb", [64, 8]); dnw = sb(s1, "dnw", [64, D])
            ut = sb(s1, "ut", [64, 64]); maskgt = sb(s1, "maskgt", [64, 64]); negm = sb(s1, "negm", [64, 64]); strict = sb(s1, "strict", [64, 64])
            for (t_, d_) in ((rgcw, rgcw_d), (rgcb, rgcb_d), (dncw, dncw_d), (rgab, rgab_d), (rgxb, rgxb_d), (lamc, lam_d),
                             (alog, alog_d), (dtb, dtb_d), (dnw, dnw_d), (ut, c_ut), (maskgt, c_maskgt), (negm, c_neg), (strict, c_strict)):
                dma(t_[:], d_, ld, W=[t_])
            rgw_f2 = sb(s1, "rgw_f2", [128, 8, 256])
            dma(rgw_f[:], rgaw_d, ld, W=[rgw_f])
            dma(rgw_f2[:], rgxw_d, ld, W=[rgw_f2])
            S.barrier()
            op("dve", lambda h: h.tensor_copy(out=rgaw[:], in_=rgw_f[:]), R=[rgw_f], W=[rgaw])
            op("dve", lambda h: h.tensor_copy(out=rgxw[:], in_=rgw_f2[:]), R=[rgw_f2], W=[rgxw])
            op("act", lambda h: h.activation(out=lamc[:], in_=lamc[:], func=AF.Exp, scale=-1.0), R=[lamc], W=[lamc])
            op("act", lambda h: h.activation(out=lamc[:], in_=lamc[:], func=AF.Ln, bias=1.0), R=[lamc], W=[lamc])
            op("dve", lambda h: h.tensor_scalar_mul(out=lamc[:], in0=lamc[:], scalar1=-8.0), R=[lamc], W=[lamc])
            op("act", lambda h: h.activation(out=alog[:], in_=alog[:], func=AF.Exp), R=[alog], W=[alog])
            op("dve", lambda h: h.tensor_scalar_mul(out=alog[:], in0=alog[:], scalar1=-1.0), R=[alog], W=[alog])
            i8b = sb(s1, "i8b", [64, 8, 64], BF16); i8f = sb(s1, "i8f", [64, 8, 64])
            strict8 = sb(s1, "strict8", [64, 8, 64]); maskgt8 = sb(s1, "maskgt8", [64, 8, 64]); neg8 = sb(s1, "neg8", [64, 8, 64])
            op("dve", lambda h: h.tensor_copy(out=i8f[:], in_=ident_f[0:64, 0:64].unsqueeze(1).to_broadcast([64, 8, 64])), R=[ident_f], W=[i8f])
            op("dve", lambda h: h.tensor_copy(out=i8b[:], in_=i8f[:]), R=[i8f], W=[i8b])
            op("dve", lambda h: h.tensor_copy(out=strict8[:], in_=strict[:].unsqueeze(1).to_broadcast([64, 8, 64])), R=[strict], W=[strict8])
            op("dve", lambda h: h.tensor_copy(out=maskgt8[:], in_=maskgt[:].unsqueeze(1).to_broadcast([64, 8, 64])), R=[maskgt], W=[maskgt8])
            op("dve", lambda h: h.tensor_copy(out=neg8[:], in_=negm[:].unsqueeze(1).to_broadcast([64, 8, 64])), R=[negm], W=[neg8])

            carry = sb(s1, "carry", [128, 32, 3]); hst = sb(s1, "hst", [128, 8])
            Sf = sb(s1, "Sf", [128, 8, 128]); Sb = sb(s1, "Sb", [128, 8, 128], BF16)
            for t_ in (carry, hst, Sf, Sb):
                op("pool", lambda h: h.memset(t_[:], 0.0), W=[t_])

            xt = [sb(s1, "xt%d" % i, [128, D]) for i in range(2)]; xsem = [S.newsem("xs%d" % i) for i in range(2)]
            junk = sb(s1, "junk", [128, D], BF16); ss = sb(s1, "ss", [128, 1]); xn = sb(s1, "xn", [128, D], BF16)
            tmpf = sb(s1, "tmpf", [128, 8, 128])
            hT = sb(s1, "hT", [128, 8, TT], BF16); hT_b = sb(s1, "hT_b", [128, 8, TT], BF16)
            NSLAB = 4
            slabs = [sb(s1, "slab%d" % i, [128, 8, 128], BF16) for i in range(NSLAB)]; slsem = [S.newsem("sl%d" % i) for i in range(NSLAB)]
            pre_r = sb(s1, "pre", [128, TT + 3]); preD = [sb(s1, "preD%d" % i, [128, TT + 3]) for i in range(2)]; d1 = [sb(s1, "d1_%d" % i, [128, TT]) for i in range(2)]; d2 = [sb(s1, "d2_%d" % i, [128, TT]) for i in range(2)]; d3 = [sb(s1, "d3_%d" % i, [128, TT]) for i in range(2)]; sqbD = [sb(s1, "sqbD%d" % i, [128, TT], BF16) for i in range(2)]; vTD = [sb(s1, "vTD%d" % i, [128, TT], BF16) for i in range(2)]; xc = sb(s1, "xc", [128, 2, TT]); xcb = sb(s1, "xcb", [128, 2, TT], BF16)
            t1 = sb(s1, "t1", [128, TT]); t2 = sb(s1, "t2", [128, TT]); t3 = sb(s1, "t3", [128, TT]); t4 = sb(s1, "t4", [128, TT])
            sqb = sb(s1, "sqb", [128, TT], BF16)
            yst = [sb(s1, "yst%d" % i, [128, TT], BF16) for i in range(2)]
            qT = sb(s1, "qT", [128, 8, TT], BF16); kT = sb(s1, "kT", [128, 8, TT], BF16); vT = sb(s1, "vT", [128, TT], BF16)
            vtok = sb(s1, "vtok", [64, NCH, 8, 128], BF16); ktok = sb(s1, "ktok", [64, NCH, 8, 128], BF16)
            zw = sb(s1, "zw", [64, NCH, D], BF16); ztmp = sb(s1, "ztmp", [64, NCH, 128])
            ba = sb(s1, "ba", [64, NCH, 16])
            beta = sb(s1, "beta", [64, 8]); gstep = sb(s1, "gstep", [64, 8]); gsb = sb(s1, "gsb", [64, 16]); Gam = sb(s1, "Gam", [64, 8])
            dec = sb(s1, "dec", [64, 8]); bG = sb(s1, "bG", [64, 8]); geT = sb(s1, "geT", [128, 8])
            Rm = sb(s1, "Rm", [64, 8, 64]); E = sb(s1, "E", [64, 8, 64]); Es = sb(s1, "Es", [64, 8, 64]); tA = sb(s1, "tA", [64, 8, 64])
            Ak = [sb(s1, "Ak%d" % i, [64, 8, 64]) for i in range(2)]; Bk = [sb(s1, "Bk%d" % i, [64, 8, 64]) for i in range(2)]
            Mk = [sb(s1, "Mk%d" % i, [64, 8, 64]) for i in range(2)]; Mb = sb(s1, "Mb", [64, 8, 64], BF16)
            qk = sb(s1, "qk", [64, 8, 64], BF16); qkT_ = sb(s1, "qkT", [64, 8, 64], BF16)
            bv = sb(s1, "bv", [64, 8, 128], BF16); bgk = sb(s1, "bgk", [64, 8, 128], BF16); kdec = sb(s1, "kdec", [64, 8, 128], BF16)
            wTn = sb(s1, "wTn", [128, 8, 64], BF16); dG = sb(s1, "dG", [64, 8, 64]); qg = sb(s1, "qg", [128, 8, 64], BF16)
            vnew = sb(s1, "vnew", [64, 8, 128], BF16); osb = sb(s1, "osb", [64, 8, 128]); osq = sb(s1, "osq", [64, 8, 128])
            oss = sb(s1, "oss", [64, 8]); ydst = [sb(s1, "ydst%d" % i, [64, D], BF16) for i in range(2)]
            pA = [ps(s1, "pA%d" % i, [128, TT]) for i in range(2)]
            pT = ps(s1, "pT", [128, 1024], BF16)
            pX = ps(s1, "pX", [128, 512]); pK = ps(s1, "pK", [128, 512]); pCh = ps(s1, "pCh", [128, 512]); pV = ps(s1, "pV", [128, 1024])
            S.barrier()

            ntile = (NP + NO) // TT
            npre = NP // TT

            def rg_slabs(own):
                l = []
                for g in range(4):
                    l += [OFF_RGX + (2 * g) * 128, OFF_RGX + (2 * g + 1) * 128]
                    if own:
                        l += [OFF_RGY + (2 * g) * 128, OFF_RGY + (2 * g + 1) * 128]
                return l

            def dnt_slabs(own):
                l = []
                for pp in range(4):
                    for o_ in (OFF_Q, OFF_K, OFF_V):
                        l += [o_ + (2 * pp) * 128, o_ + (2 * pp + 1) * 128]
                l += [OFF_BA]
                if own:
                    l += [OFF_Z + j * 128 for j in range(8)]
                return l

            sched = rg_slabs(0 >= npre)
            for ti in range(ntile):
                sched += dnt_slabs(ti >= npre)
                if ti + 1 < ntile:
                    sched += rg_slabs(ti + 1 >= npre)
            state = {"issued": 0, "used": 0}

            def issue_slab():
                i = state["issued"]
                if i >= len(sched):
                    return
                c0 = sched[i]; cw = min(128, D_IN - c0)
                sl = slabs[i % NSLAB]
                dma(sl[:, :, 0:cw], w_in_bf[:, c0:c0 + cw].rearrange("(kc p) n -> p kc n", p=128), slsem[i % NSLAB], W=[sl])
                state["issued"] += 1

            def next_slab(expect=None):
                assert expect is None or sched[state["used"]] == expect, (expect, sched[state["used"]], state["used"])
                while state["issued"] < min(len(sched), state["used"] + NSLAB - 1) or state["issued"] <= state["used"]:
                    issue_slab()
                sl = slabs[state["used"] % NSLAB]
                state["used"] += 1
                return sl

            pa_i = [0]

            def proj_fm(sl, hT, ptile=None):
                if ptile is not None:
                    p = ptile
                else:
                    p = pA[pa_i[0] % 2]; pa_i[0] += 1
                S.group("pe", [lambda h, kc=kc: h.matmul(p[:], lhsT=sl[:, kc, :], rhs=hT[:, kc, :], start=(kc == 0), stop=(kc == 7)) for kc in range(8)], R=[sl, hT], W=[p])
                return p

            def conv(p, cidx, wcol, bias_ap, dst_ap, dstT, pre=None):
                pre = pre if pre is not None else pre_r
                op("pool", lambda h: h.tensor_copy(out=pre[:, 0:3], in_=carry[:, cidx, :]), R=[carry], W=[pre])
                op("act", lambda h: h.activation(out=pre[:, 3:3 + TT], in_=p[:], func=AF.Copy), R=[p], W=[pre])
                op("pool", lambda h: h.tensor_copy(out=carry[:, cidx, :], in_=pre[:, TT:TT + 3]), R=[pre], W=[carry])
                if bias_ap is not None:
                    op("dve", lambda h: h.tensor_scalar(out=dst_ap, in0=pre[:, 0:TT], scalar1=wcol[:, 0:1], scalar2=bias_ap, op0=ALU.mult, op1=ALU.add), R=[pre], W=[dstT])
                else:
                    op("dve", lambda h: h.tensor_scalar_mul(out=dst_ap, in0=pre[:, 0:TT], scalar1=wcol[:, 0:1]), R=[pre], W=[dstT])
                for j in range(1, 4):
                    op("dve", lambda h: h.scalar_tensor_tensor(out=dst_ap, in0=pre[:, j:j + TT], scalar=wcol[:, j:j + 1], in1=dst_ap, op0=ALU.mult, op1=ALU.add), R=[pre, dstT], W=[dstT])

            st_i = [0]

            hTs = [hT, hT_b]

            def emit_prep(ti):
                hTt = hTs[ti % 2]
                own = ti >= npre
                tok0 = ti * TT
                for sub in range(TT // 128):
                    r0 = tok0 + sub * 128
                    src = xp[r0:r0 + 128, :] if r0 < NP else xo[r0 - NP:r0 - NP + 128, :]
                    xi = (ti * (TT // 128) + sub) % 2
                    dma(xt[xi][:], src, xsem[xi], W=[xt[xi]])
                    norm_to_hT(xt[xi], junk, ss, xn, pT, hTt, sub * 128, A1, cols[:, 0, :], tmpf)

            def gen_rg(ti):
                own = ti >= npre; tok0 = ti * TT; hTt = hTs[ti % 2]
                if ti == npre:
                    op("dve", lambda h: h.tensor_scalar_mul(out=carry[:, 0:8, :], in0=carry[:, 0:8, :], scalar1=flag[:, 0:1]), R=[carry, flag], W=[carry])
                    op("dve", lambda h: h.tensor_scalar_mul(out=hst[:], in0=hst[:], scalar1=flag[:, 0:1]), R=[hst, flag], W=[hst])
                for g in range(4):
                    for jc in range(2):
                        ch = 2 * g + jc
                        p = proj_fm(next_slab(), hTt)
                        yield
                        conv(p, ch, rgcw[:, ch, :], rgcb[:, ch:ch + 1], xc[:, jc, :], xc)
                        yield
                    ypp = []
                    if own:
                        for jc in range(2):
                            ypp.append(next_slab())
                    op("pool", lambda h: h.tensor_copy(out=xcb[:], in_=xc[:]), R=[xc], W=[xcb])
                    yield
                    for oc in range(2):
                        ch = 2 * g + oc
                        p = pA[pa_i[0] % 2]; pa_i[0] += 1
                        S.group("pe", [lambda h, ic=ic: h.matmul(p[:], lhsT=rgaw[:, 2 * g + ic, oc * 128:(oc + 1) * 128], rhs=xcb[:, ic, :], start=(ic == 0), stop=(ic == 1)) for ic in range(2)], R=[rgaw, xcb], W=[p])
                        yield
                        op("act", lambda h: h.activation(out=t1[:], in_=p[:], func=AF.Sigmoid, bias=rgab[:, ch:ch + 1]), R=[p, rgab], W=[t1])
                        yield
                        p = pA[pa_i[0] % 2]; pa_i[0] += 1
                        S.group("pe", [lambda h, ic=ic: h.matmul(p[:], lhsT=rgxw[:, 2 * g + ic, oc * 128:(oc + 1) * 128], rhs=xcb[:, ic, :], start=(ic == 0), stop=(ic == 1)) for ic in range(2)], R=[rgxw, xcb], W=[p])
                        yield
                        op("act", lambda h: h.activation(out=t2[:], in_=p[:], func=AF.Sigmoid, bias=rgxb[:, ch:ch + 1]), R=[p, rgxb], W=[t2])
                        yield
                        op("act", lambda h: h.activation(out=t1[:], in_=t1[:], func=AF.Exp, scale=lamc[:, ch:ch + 1]), R=[t1, lamc], W=[t1])
                        yield
                        op("pool", lambda h: h.tensor_tensor(out=t3[:], in0=t1[:], in1=t1[:], op=ALU.mult), R=[t1], W=[t3])
                        yield
                        op("act", lambda h: h.activation(out=t3[:], in_=t3[:], func=AF.Sqrt, scale=-1.0, bias=1.0), R=[t3], W=[t3])
                        yield
                        op("dve", lambda h: h.tensor_tensor(out=t2[:], in0=t2[:], in1=xc[:, oc, :], op=ALU.mult), R=[t2, xc], W=[t2])
                        yield
                        op("dve", lambda h: h.tensor_tensor(out=t2[:], in0=t2[:], in1=t3[:], op=ALU.mult), R=[t2, t3], W=[t2])
                        yield
                        op("dve", lambda h: h.tensor_tensor_scan(out=t4[:], data0=t1[:], data1=t2[:], initial=hst[:, ch:ch + 1], op0=ALU.mult, op1=ALU.add), R=[t1, t2, hst], W=[t4])
                        yield
                        op("pool", lambda h: h.tensor_copy(out=hst[:, ch:ch + 1], in_=t4[:, TT - 1:TT]), R=[t4], W=[hst])
                        yield
                        if own:
                            p = proj_fm(ypp[oc], hTt)
                            yield
                            op("act", lambda h: h.activation(out=t1[:], in_=p[:], func=AF.Square), R=[p], W=[t1])
                            yield
                            op("dve", lambda h: h.tensor_scalar(out=t1[:], in0=t1[:], scalar1=0.044715, scalar2=1.0, op0=ALU.mult, op1=ALU.add), R=[t1], W=[t1])
                            yield
                            op("dve", lambda h: h.tensor_tensor(out=t1[:], in0=t1[:], in1=p[:], op=ALU.mult), R=[t1, p], W=[t1])
                            yield
                            op("act", lambda h: h.activation(out=t1[:], in_=t1[:], func=AF.Sigmoid, scale=1.5957691216057308), R=[t1], W=[t1])
                            yield
                            op("dve", lambda h: h.tensor_tensor(out=t1[:], in0=t1[:], in1=p[:], op=ALU.mult), R=[t1, p], W=[t1])
                            yield
                            ys = yst[st_i[0] % 2]; ssm = so[st_i[0] % 2]; st_i[0] += 1
                            op("dve", lambda h: h.tensor_tensor(out=ys[:], in0=t1[:], in1=t4[:], op=ALU.mult), R=[t1, t4], W=[ys])
                            yield
                            c0 = tok0 - NP
                            dma(yrgT_d[ch * 128:(ch + 1) * 128, c0:c0 + TT], ys[:], ssm, R=[ys])
                            yield

            def emit_dnt(ti):
                own = ti >= npre; tok0 = ti * TT; hTt = hTs[ti % 2]
                def gen_head(hh, k):
                    t1 = d1[k]; t2 = d2[k]; t3 = d3[k]; sqb = sqbD[k]; vT = vTD[k]
                    for which, dstT in ((0, qT), (1, kT), (2, vT)):
                        cid = which * 8 + hh
                        p = proj_fm(next_slab((OFF_Q, OFF_K, OFF_V)[which] + hh * 128), hTt, ptile=pA[k])
                        yield
                        conv(p, 8 + cid, dncw[:, cid, :], None, t1[:], t1, pre=preD[k])
                        yield
                        if which == 2:
                            op("act", lambda h: h.activation(out=vT[:], in_=t1[:], func=AF.Silu), R=[t1], W=[vT])
                            yield
                            S.group("pe", [lambda h, c=c: h.transpose(out=pT[0:64, c * 128:(c + 1) * 128], in_=vT[:, c * 64:(c + 1) * 64], identity=ident_b[:]) for c in range(NCH)], R=[vT, ident_b], W=[pT])
                            op("dve", lambda h: h.tensor_copy(out=vtok[:, :, hh, :], in_=pT[0:64, 0:NCH * 128].rearrange("p (c d) -> p c d", c=NCH)), R=[pT], W=[vtok])
                            yield
                        else:
                            op("act", lambda h: h.activation(out=t2[:], in_=t1[:], func=AF.Silu), R=[t1], W=[t2])
                            yield
                            op("pool", lambda h: h.tensor_tensor(out=sqb[:], in0=t2[:], in1=t2[:], op=ALU.mult), R=[t2], W=[sqb])
                            yield
                            pn = pA[k]
                            op("pe", lambda h: h.matmul(pn[:], lhsT=ones_b[:], rhs=sqb[:], start=True, stop=True), R=[ones_b, sqb], W=[pn])
                            yield
                            op("dve", lambda h: h.tensor_scalar_add(out=t3[:], in0=pn[:], scalar1=EPS), R=[pn], W=[t3])
                            yield
                            op("act", lambda h: h.activation(out=t3[:], in_=t3[:], func=AF.Sqrt), R=[t3], W=[t3])
                            yield
                            op("dve", lambda h: h.reciprocal(out=t3[:], in_=t3[:]), R=[t3], W=[t3])
                            yield
                            sc = (128.0 ** -0.5) if which == 0 else 1.0
                            op("dve", lambda h: h.scalar_tensor_tensor(out=dstT[:, hh, :], in0=t2[:], scalar=sc, in1=t3[:], op0=ALU.mult, op1=ALU.mult), R=[t2, t3], W=[dstT])
                            yield
                            if which == 1:
                                S.group("pe", [lambda h, c=c: h.transpose(out=pT[0:64, c * 128:(c + 1) * 128], in_=kT[:, hh, c * 64:(c + 1) * 64], identity=ident_b[:]) for c in range(NCH)], R=[kT, ident_b], W=[pT])
                                op("dve", lambda h: h.tensor_copy(out=ktok[:, :, hh, :], in_=pT[0:64, 0:NCH * 128].rearrange("p (c d) -> p c d", c=NCH)), R=[pT], W=[ktok])
                                yield
                interleave([chain(*[gen_head(hh, 0) for hh in (0, 2, 4, 6)]), chain(*[gen_head(hh, 1) for hh in (1, 3, 5, 7)])])
                sl = next_slab()
                for c in range(NCH):
                    S.group("pe", [lambda h, kc=kc: h.matmul(pV[0:64, 0:16], lhsT=hTt[:, kc, c * 64:(c + 1) * 64], rhs=sl[:, kc, 0:16], start=(kc == 0), stop=(kc == 7)) for kc in range(8)], R=[hTt, sl], W=[pV])
                    op("act", lambda h: h.activation(out=ba[:, c, :], in_=pV[0:64, 0:16], func=AF.Copy), R=[pV], W=[ba])
                if own:
                    for j in range(8):
                        sl = next_slab()
                        for c in range(NCH):
                            S.group("pe", [lambda h, kc=kc: h.matmul(pX[0:64, c * 128:(c + 1) * 128], lhsT=hTt[:, kc, c * 64:(c + 1) * 64], rhs=sl[:, kc, :], start=(kc == 0), stop=(kc == 7)) for kc in range(8)], R=[hTt, sl], W=[pX])
                        op("act", lambda h: h.activation(out=ztmp[:], in_=pX[0:64, 0:NCH * 128].rearrange("p (c d) -> p c d", c=NCH), func=AF.Silu), R=[pX], W=[ztmp])
                        op("pool", lambda h: h.tensor_tensor(out=zw[:, :, j * 128:(j + 1) * 128], in0=ztmp[:], in1=dnw[:, j * 128:(j + 1) * 128].unsqueeze(1).to_broadcast([64, NCH, 128]), op=ALU.mult), R=[ztmp, dnw], W=[zw])

            def gen_ch(ti):
                own = ti >= npre; tok0 = ti * TT
                for c in range(NCH):
                    cs = slice(c * 64, (c + 1) * 64)
                    op("act", lambda h: h.activation(out=beta[:], in_=ba[:, c, 0:8], func=AF.Sigmoid), R=[ba], W=[beta])
                    yield
                    op("dve", lambda h: h.tensor_tensor(out=gstep[:], in0=ba[:, c, 8:16], in1=dtb[:], op=ALU.add), R=[ba, dtb], W=[gstep])
                    yield
                    op("act", lambda h: h.activation(out=gstep[:], in_=gstep[:], func=AF.Exp), R=[gstep], W=[gstep])
                    yield
                    op("act", lambda h: h.activation(out=gstep[:], in_=gstep[:], func=AF.Ln, bias=1.0), R=[gstep], W=[gstep])
                    yield
                    op("dve", lambda h: h.tensor_tensor(out=gstep[:], in0=gstep[:], in1=alog[:], op=ALU.mult), R=[gstep, alog], W=[gstep])
                    yield
                    op("dve", lambda h: h.tensor_tensor(out=Rm[:], in0=maskgt8[:], in1=gstep[:].unsqueeze(2).to_broadcast([64, 8, 64]), op=ALU.mult), R=[maskgt8, gstep], W=[Rm])
                    yield
                    S.group("pe", [lambda h: h.matmul(pX[0:64, :], lhsT=ut[:], rhs=Rm[:].rearrange("p a b -> p (a b)"), start=True, stop=False),
                                   lambda h: h.matmul(pX[0:64, :], lhsT=ident_f[0:64, 0:64], rhs=neg8[:].rearrange("p a b -> p (a b)"), start=False, stop=True),
                                   lambda h: h.matmul(pV[0:64, 0:8], lhsT=ut[:], rhs=gstep[:], start=True, stop=True),
                                   lambda h: h.matmul(pV[:, 8:16], lhsT=ones_f[:], rhs=gstep[:], start=True, stop=True)],
                            R=[ut, Rm, ident_f, neg8, gstep, ones_f], W=[pX, pV])
                    yield
                    op("act", lambda h: h.activation(out=E[:].rearrange("p a b -> p (a b)"), in_=pX[0:64, :], func=AF.Exp), R=[pX], W=[E])
                    yield
                    op("act", lambda h: h.activation(out=Gam[:], in_=pV[0:64, 0:8], func=AF.Exp), R=[pV], W=[Gam])
                    yield
                    op("act", lambda h: h.activation(out=geT[:], in_=pV[:, 8:16], func=AF.Exp), R=[pV], W=[geT])
                    yield
                    op("dve", lambda h: h.tensor_copy(out=gsb[:], in_=pV[0:64, 0:16]), R=[pV], W=[gsb])
                    yield
                    op("dve", lambda h: h.tensor_tensor(out=dec[:], in0=gsb[:, 8:16], in1=gsb[:, 0:8], op=ALU.subtract), R=[gsb], W=[dec])
                    yield
                    op("act", lambda h: h.activation(out=dec[:], in_=dec[:], func=AF.Exp), R=[dec], W=[dec])
                    yield
                    op("dve", lambda h: h.tensor_tensor(out=bG[:], in0=beta[:], in1=Gam[:], op=ALU.mult), R=[beta, Gam], W=[bG])
                    yield
                    op("pool", lambda h: h.tensor_tensor(out=Es[:], in0=E[:], in1=strict8[:], op=ALU.mult), R=[E, strict8], W=[Es])
                    yield
                    S.group("pe", [lambda h, hh=hh: h.matmul(pK[0:64, hh * 64:(hh + 1) * 64], lhsT=kT[:, hh, cs], rhs=kT[:, hh, cs], start=True, stop=True) for hh in range(8)], R=[kT], W=[pK])
                    yield
                    op("dve", lambda h: h.tensor_tensor(out=tA[:].rearrange("p a b -> p (a b)"), in0=pK[0:64, :], in1=Es[:].rearrange("p a b -> p (a b)"), op=ALU.mult), R=[pK, Es], W=[tA])
                    yield
                    op("pool", lambda h: h.tensor_tensor(out=Ak[0][:], in0=tA[:], in1=beta[:].unsqueeze(2).to_broadcast([64, 8, 64]), op=ALU.mult), R=[tA, beta], W=[Ak[0]])
                    yield
                    if own:
                        S.group("pe", [lambda h, hh=hh: h.matmul(pK[0:64, hh * 64:(hh + 1) * 64], lhsT=qT[:, hh, cs], rhs=kT[:, hh, cs], start=True, stop=True) for hh in range(8)], R=[qT, kT], W=[pK])
                        yield
                        op("dve", lambda h: h.tensor_tensor(out=qk[:].rearrange("p a b -> p (a b)"), in0=pK[0:64, :], in1=E[:].rearrange("p a b -> p (a b)"), op=ALU.mult), R=[pK, E], W=[qk])
                        yield
                    S.group("pe", [lambda h, hh=hh: h.transpose(out=pCh[0:64, hh * 64:(hh + 1) * 64], in_=Ak[0][:, hh, :], identity=ident_f[0:64, 0:64]) for hh in range(8)], R=[Ak[0], ident_f], W=[pCh])
                    yield
                    op("act", lambda h: h.activation(out=Bk[0][:].rearrange("p a b -> p (a b)"), in_=pCh[0:64, :], func=AF.Copy), R=[pCh], W=[Bk[0]])
                    yield
                    if own:
                        S.group("pe", [lambda h, hh=hh: h.transpose(out=pX[0:64, 0:256].bitcast(BF16)[:, hh * 64:(hh + 1) * 64], in_=qk[:, hh, :], identity=ident_b[0:64, 0:64]) for hh in range(8)], R=[qk, ident_b], W=[pX])
                        yield
                        op("act", lambda h: h.activation(out=qkT_[:].rearrange("p a b -> p (a b)"), in_=pX[0:64, 0:256].bitcast(BF16), func=AF.Copy), R=[pX], W=[qkT_])
                        yield
                    op("dve", lambda h: h.tensor_tensor(out=Mk[0][:], in0=i8f[:], in1=Bk[0][:], op=ALU.subtract), R=[i8f, Bk[0]], W=[Mk[0]])
                    yield
                    cur = 0
                    for lvl in range(1, 6):
                        a0, b0, m0 = Ak[cur], Bk[cur], Mk[cur]
                        a1, b1, m1 = Ak[1 - cur], Bk[1 - cur], Mk[1 - cur]
                        S.group("pe", [lambda h, hh=hh: h.matmul(pCh[0:64, hh * 64:(hh + 1) * 64], lhsT=b0[:, hh, :], rhs=a0[:, hh, :], start=True, stop=True) for hh in range(8)], R=[a0, b0], W=[pCh])
                        yield
                        op("act", lambda h: h.activation(out=a1[:].rearrange("p a b -> p (a b)"), in_=pCh[0:64, :], func=AF.Copy), R=[pCh], W=[a1])
                        yield
                        if lvl < 5:
                            S.group("pe", [lambda h, hh=hh: h.matmul(pK[0:64, hh * 64:(hh + 1) * 64], lhsT=a0[:, hh, :], rhs=b0[:, hh, :], start=True, stop=True) for hh in range(8)], R=[a0, b0], W=[pK])
                            yield
                            op("dve", lambda h: h.tensor_copy(out=b1[:].rearrange("p a b -> p (a b)"), in_=pK[0:64, :]), R=[pK], W=[b1])
                            yield
                        S.group("pe", [lambda h, hh=hh: h.matmul(pCh[0:64, hh * 64:(hh + 1) * 64], lhsT=a1[:, hh, :], rhs=m0[:, hh, :], start=True, stop=True) for hh in range(8)], R=[a1, m0], W=[pCh])
                        yield
                        op("dve", lambda h: h.tensor_tensor(out=m1[:].rearrange("p a b -> p (a b)"), in0=pCh[0:64, :], in1=m0[:].rearrange("p a b -> p (a b)"), op=ALU.add), R=[pCh, m0], W=[m1])
                        yield
                        cur = 1 - cur
                    op("pool", lambda h: h.tensor_copy(out=Mb[:], in_=Mk[cur][:]), R=[Mk[cur]], W=[Mb])
                    yield
                    M = Mb
                    op("pool", lambda h: h.tensor_tensor(out=bv[:], in0=vtok[:, c, :, :], in1=beta[:].unsqueeze(2).to_broadcast([64, 8, 128]), op=ALU.mult), R=[vtok, beta], W=[bv])
                    yield
                    op("pool", lambda h: h.tensor_tensor(out=bgk[:], in0=ktok[:, c, :, :], in1=bG[:].unsqueeze(2).to_broadcast([64, 8, 128]), op=ALU.mult), R=[ktok, bG], W=[bgk])
                    yield
                    op("pool", lambda h: h.tensor_tensor(out=kdec[:], in0=ktok[:, c, :, :], in1=dec[:].unsqueeze(2).to_broadcast([64, 8, 128]), op=ALU.mult), R=[ktok, dec], W=[kdec])
                    yield
                    S.group("pe", [lambda h, hh=hh: h.matmul(pX[:, hh * 64:(hh + 1) * 64], lhsT=bgk[:, hh, :], rhs=M[:, hh, :], start=True, stop=True) for hh in range(8)], R=[bgk, M], W=[pX])
                    yield
                    op("act", lambda h: h.activation(out=wTn[:].rearrange("p a b -> p (a b)"), in_=pX[:], func=AF.Copy, scale=-1.0), R=[pX], W=[wTn])
                    yield
                    if own:
                        op("dve", lambda h: h.tensor_tensor(out=dG[:], in0=i8f[:], in1=Gam[:].unsqueeze(2).to_broadcast([64, 8, 64]), op=ALU.mult), R=[i8f, Gam], W=[dG])
                        yield
                        op("pe", lambda h: h.matmul(pK[:], lhsT=ones_f[:], rhs=dG[:].rearrange("p a b -> p (a b)"), start=True, stop=True), R=[ones_f, dG], W=[pK])
                        yield
                        op("dve", lambda h: h.tensor_tensor(out=qg[:], in0=qT[:, :, cs], in1=pK[:].rearrange("p (a b) -> p a b", a=8), op=ALU.mult), R=[qT, pK], W=[qg])
                        yield
                    fns = []
                    for hh in range(8):
                        fns.append(lambda h, hh=hh: h.matmul(pV[0:64, hh * 128:(hh + 1) * 128], lhsT=M[:, hh, :], rhs=bv[:, hh, :], start=True, stop=False))
                        fns.append(lambda h, hh=hh: h.matmul(pV[0:64, hh * 128:(hh + 1) * 128], lhsT=wTn[:, hh, :], rhs=Sb[:, hh, :], start=False, stop=True))
                    S.group("pe", fns, R=[M, bv, wTn, Sb], W=[pV])
                    yield
                    op("act", lambda h: h.activation(out=vnew[:, 0:4, :].rearrange("p a b -> p (a b)"), in_=pV[0:64, 0:512], func=AF.Copy), R=[pV], W=[vnew])
                    yield
                    op("dve", lambda h: h.tensor_copy(out=vnew[:, 4:8, :].rearrange("p a b -> p (a b)"), in_=pV[0:64, 512:1024]), R=[pV], W=[vnew])
                    yield
                    if own:
                        fns = []
                        for hh in range(8):
                            fns.append(lambda h, hh=hh: h.matmul(pV[0:64, hh * 128:(hh + 1) * 128], lhsT=qg[:, hh, :], rhs=Sb[:, hh, :], start=True, stop=False))
                            fns.append(lambda h, hh=hh: h.matmul(pV[0:64, hh * 128:(hh + 1) * 128], lhsT=qkT_[:, hh, :], rhs=vnew[:, hh, :], start=False, stop=True))
                        S.group("pe", fns, R=[qg, Sb, qkT_, vnew], W=[pV])
                        yield
                        op("act", lambda h: h.activation(out=osb[:, 0:4, :].rearrange("p a b -> p (a b)"), in_=pV[0:64, 0:512], func=AF.Copy), R=[pV], W=[osb])
                        yield
                        op("dve", lambda h: h.tensor_copy(out=osb[:, 4:8, :].rearrange("p a b -> p (a b)"), in_=pV[0:64, 512:1024]), R=[pV], W=[osb])
                        yield
                        op("pool", lambda h: h.tensor_tensor(out=osq[:], in0=osb[:], in1=osb[:], op=ALU.mult), R=[osb], W=[osq])
                        yield
                        op("dve", lambda h: h.reduce_sum(out=oss[:], in_=osq[:], axis=AX.X), R=[osq], W=[oss])
                        yield
                        op("dve", lambda h: h.tensor_scalar(out=oss[:], in0=oss[:], scalar1=1.0 / 128, scalar2=EPS, op0=ALU.mult, op1=ALU.add), R=[oss], W=[oss])
                        yield
                        op("act", lambda h: h.activation(out=oss[:], in_=oss[:], func=AF.Sqrt), R=[oss], W=[oss])
                        yield
                        op("dve", lambda h: h.reciprocal(out=oss[:], in_=oss[:]), R=[oss], W=[oss])
                        yield
                        op("dve", lambda h: h.tensor_tensor(out=osb[:], in0=osb[:], in1=oss[:].unsqueeze(2).to_broadcast([64, 8, 128]), op=ALU.mult), R=[osb, oss], W=[osb])
                        yield
                        yd = ydst[st_i[0] % 2]; ssm = so[2 + st_i[0] % 2]; st_i[0] += 1
                        op("dve", lambda h: h.tensor_tensor(out=yd[:], in0=osb[:].rearrange("p a b -> p (a b)"), in1=zw[:, c, :], op=ALU.mult), R=[osb, zw], W=[yd])
                        yield
                        r0 = tok0 - NP + c * 64
                        dma(ydn_d[r0:r0 + 64, :], yd[:], ssm, R=[yd])
                        yield
                    S.group("pe", [lambda h, hh=hh: h.matmul(pV[:, hh * 128:(hh + 1) * 128], lhsT=kdec[:, hh, :], rhs=vnew[:, hh, :], start=True, stop=True) for hh in range(8)], R=[kdec, vnew], W=[pV])
                    yield
                    op("pool", lambda h: h.tensor_tensor(out=Sf[:], in0=Sf[:], in1=geT[:].unsqueeze(2).to_broadcast([128, 8, 128]), op=ALU.mult), R=[Sf, geT], W=[Sf])
                    yield
                    op("dve", lambda h: h.tensor_tensor(out=Sf[:, 0:4, :].rearrange("p a b -> p (a b)"), in0=Sf[:, 0:4, :].rearrange("p a b -> p (a b)"), in1=pV[:, 0:512], op=ALU.add), R=[Sf, pV], W=[Sf])
                    yield
                    op("dve", lambda h: h.tensor_tensor(out=Sf[:, 4:8, :].rearrange("p a b -> p (a b)"), in0=Sf[:, 4:8, :].rearrange("p a b -> p (a b)"), in1=pV[:, 512:1024], op=ALU.add), R=[Sf, pV], W=[Sf])
                    yield
                    op("act", lambda h: h.activation(out=Sb[:], in_=Sf[:], func=AF.Copy), R=[Sf], W=[Sb])
                    yield

            def emit_flag_dn():
                op("dve", lambda h: h.tensor_scalar_mul(out=carry[:, 8:32, :], in0=carry[:, 8:32, :], scalar1=flag[:, 0:1]), R=[carry, flag], W=[carry])
                op("dve", lambda h: h.tensor_scalar_mul(out=Sf[:], in0=Sf[:], scalar1=flag[:, 0:1]), R=[Sf, flag], W=[Sf])
                op("act", lambda h: h.activation(out=Sb[:], in_=Sf[:], func=AF.Copy), R=[Sf], W=[Sb])

            def interleave(gens):
                gens = list(gens)
                while gens:
                    for g_ in list(gens):
                        try:
                            next(g_)
                        except StopIteration:
                            gens.remove(g_)

            def chain(*gs):
                for g_ in gs:
                    yield from g_

            def gen_prep(ti):
                emit_prep(ti)
                yield

            emit_prep(0)
            interleave([gen_rg(0)])
            for ti in range(ntile):
                emit_dnt(ti)
                gs = [gen_ch(ti)]
                if ti + 1 < ntile:
                    gs.append(chain(gen_prep(ti + 1), gen_rg(ti + 1)))
                interleave(gs)
                if ti == npre - 1:
                    emit_flag_dn()
            S.barrier()

        NSUB = NO // 128
        wte = sb(st, "wte", [128, NSUB, NE])
        if phases >= 2:
          with ExitStack() as s2:
            wbrg = sb(s2, "wbrg", [128, 8, D], BF16); wbdn = sb(s2, "wbdn", [128, 8, D], BF16); wout = sb(s2, "wout", [128, 8, D], BF16)
            wgt = sb(s2, "wgt", [128, 8, 2048], BF16); wr = sb(s2, "wr", [128, 8, 36], BF16); br = sb(s2, "br", [128, 36])
            stgsem = S.newsem("stg")
            with ExitStack() as s2a:
                stg = sb(s2a, "stg", [128, 8, D]); wrf = sb(s2a, "wrf", [128, 8, 36])
                dma(wrf[:], w_r36.rearrange("(kc p) n -> p kc n", p=128), ld, W=[wrf])
                dma(br[:], b_r36_d, ld, W=[br])
                dma(wgt[:], w_in_bf[:, OFF_GRG:OFF_GRG + 2048].rearrange("(kc p) n -> p kc n", p=128), ld, W=[wgt])
                S.barrier()
                op("dve", lambda h: h.tensor_copy(out=wr[:], in_=wrf[:]), R=[wrf], W=[wr])
                for i_, (dst, src) in enumerate(((wbrg, w_brg), (wbdn, w_bdn), (wout, w_out))):
                    dma(stg[:], src.rearrange("(kc p) n -> p kc n", p=128), stgsem, W=[stg])
                    op(("dve", "pool", "dve")[i_], lambda h: h.tensor_copy(out=dst[:], in_=stg[:]), R=[stg], W=[dst])
                S.barrier()
            T2 = 512
            xt4 = sb(s2, "xt4", [128, 4, D]); x4sem = S.newsem("x4")
            junk = sb(s2, "junk2", [128, D], BF16); ss = sb(s2, "ss2", [128, 1]); xn = sb(s2, "xn2", [128, D], BF16); tmpf = sb(s2, "tmpf2", [128, 8, 128])
            hT = sb(s2, "hT2", [128, 8, T2], BF16); sgr = sb(s2, "sgr", [128, 8, T2], BF16); sgd = sb(s2, "sgd", [128, 8, T2], BF16)
            yrgT = sb(s2, "yrgT2", [128, 8, T2], BF16); ysem = S.newsem("yr2")
            ydn = sb(s2, "ydn2", [128, 4, D], BF16); ydsem = S.newsem("yd2"); ydnT = sb(s2, "ydnT", [128, 8, T2], BF16)
            mg = sb(s2, "mg", [128, 8, T2], BF16); m1 = sb(s2, "m1", [128, T2]); m2 = sb(s2, "m2", [128, T2])
            x2 = [sb(s2, "x2_%d" % i, [128, D]) for i in range(2)]; x2sem = [S.newsem("x2s%d" % i) for i in range(2)]
            h2T = sb(s2, "h2T", [128, 8, T2], BF16); h2sem = S.newsem("h2s")
            lg = sb(s2, "lg", [128, 36]); gmx = sb(s2, "gmx", [128, 1]); ngm = sb(s2, "ngm", [128, 1]); gex = sb(s2, "gex", [128, 4]); gsum = sb(s2, "gsum", [128, 1])
            oh = sb(s2, "oh", [128, 4]); elm = sb(s2, "elm", [128, 4, 8]); m8 = sb(s2, "m8", [128, 8]); dd = sb(s2, "dd", [128, 1]); w12 = sb(s2, "w12", [128, 2])
            mm1 = sb(s2, "mm1", [128, 32]); mm2 = sb(s2, "mm2", [128, 32])
            pT = ps(s2, "pT2", [128, 1024], BF16); pP = [ps(s2, "pP%d" % i, [128, T2]) for i in range(2)]
            pO = ps(s2, "pO", [128, D]); pR = ps(s2, "pR", [128, 64])
            pp_i = [0]
            for ti in range(NO // T2):
                c0 = ti * T2
                dma(xt4[:], xo[c0:c0 + T2, :].rearrange("(s p) d -> p s d", p=128), x4sem, W=[xt4])
                dma(yrgT[:], yrgT_d[:, c0:c0 + T2].rearrange("(c p) n -> p c n", p=128), ysem, W=[yrgT])
                dma(ydn[:], ydn_d[c0:c0 + T2, :].rearrange("(s p) d -> p s d", p=128), ydsem, W=[ydn])
                for sub in range(4):
                    xs = TV3(xt4, sub)
                    norm_to_hT(xs, junk, ss, xn, pT, hT, sub * 128, A1, cols[:, 0, :], tmpf)
                for sub in range(4):
                    S.group("pe", [lambda h, kc=kc: h.transpose(out=pT[:, kc * 128:(kc + 1) * 128], in_=ydn[:, sub, kc * 128:(kc + 1) * 128], identity=ident_b[:]) for kc in range(8)], R=[ydn, ident_b], W=[pT])
                    op("act", lambda h: h.activation(out=ydnT[:, :, sub * 128:(sub + 1) * 128], in_=pT[:].rearrange("p (a b) -> p a b", a=8), func=AF.Copy), R=[pT], W=[ydnT])
                for gi, sg in ((0, sgr), (1, sgd)):
                    for oc in range(8):
                        p = pP[pp_i[0] % 2]; pp_i[0] += 1
                        S.group("pe", [lambda h, kc=kc: h.matmul(p[:], lhsT=wgt[:, kc, gi * 1024 + oc * 128:gi * 1024 + (oc + 1) * 128], rhs=hT[:, kc, :], start=(kc == 0), stop=(kc == 7)) for kc in range(8)], R=[wgt, hT], W=[p])
                        op("act", lambda h: h.activation(out=sg[:, oc, :], in_=p[:], func=AF.Sigmoid), R=[p], W=[sg])
                for oc in range(8):
                    p = pP[pp_i[0] % 2]; pp_i[0] += 1
                    S.group("pe", [lambda h, kc=kc: h.matmul(p[:], lhsT=wbrg[:, kc, oc * 128:(oc + 1) * 128], rhs=yrgT[:, kc, :], start=(kc == 0), stop=(kc == 7)) for kc in range(8)], R=[wbrg, yrgT], W=[p])
                    op("dve", lambda h: h.tensor_tensor(out=m1[:], in0=p[:], in1=sgr[:, oc, :], op=ALU.mult), R=[p, sgr], W=[m1])
                    p = pP[pp_i[0] % 2]; pp_i[0] += 1
                    S.group("pe", [lambda h, kc=kc: h.matmul(p[:], lhsT=wbdn[:, kc, oc * 128:(oc + 1) * 128], rhs=ydnT[:, kc, :], start=(kc == 0), stop=(kc == 7)) for kc in range(8)], R=[wbdn, ydnT], W=[p])
                    op("dve", lambda h: h.tensor_tensor(out=m2[:], in0=p[:], in1=sgd[:, oc, :], op=ALU.mult), R=[p, sgd], W=[m2])
                    op("pool", lambda h: h.tensor_tensor(out=mg[:, oc, :], in0=m1[:], in1=m2[:], op=ALU.add), R=[m1, m2], W=[mg])
                for sub in range(4):
                    r0 = c0 + sub * 128
                    fns = []
                    for hf in range(2):
                        fns += [lambda h, kc=kc, hf=hf: h.matmul(pO[:, hf * 512:(hf + 1) * 512], lhsT=mg[:, kc, sub * 128:(sub + 1) * 128], rhs=wout[:, kc, hf * 512:(hf + 1) * 512], start=(kc == 0), stop=(kc == 7)) for kc in range(8)]
                    S.group("pe", fns, R=[mg, wout], W=[pO])
                    xx = x2[sub % 2]
                    for hf in range(2):
                        op("dve", lambda h: h.tensor_tensor(out=xx[:, hf * 512:(hf + 1) * 512], in0=pO[:, hf * 512:(hf + 1) * 512], in1=gate1[:, hf * 512:(hf + 1) * 512], op=ALU.mult), R=[pO, gate1], W=[xx])
                    op("pool", lambda h: h.tensor_tensor(out=xx[:], in0=xx[:], in1=xt4[:, sub, :], op=ALU.add), R=[xx, xt4], W=[xx])
                    dma(x2_d[r0:r0 + 128, :], xx[:], x2sem[sub % 2], R=[xx])
                    norm_to_hT(xx, junk, ss, xn, pT, h2T, sub * 128, A2, cols[:, 2, :], tmpf)
                    S.group("pe", [lambda h, kc=kc: h.matmul(pR[:, 0:36], lhsT=h2T[:, kc, sub * 128:(sub + 1) * 128], rhs=wr[:, kc, :], start=(kc == 0), stop=(kc == 7)) for kc in range(8)], R=[h2T, wr], W=[pR])
                    op("dve", lambda h: h.tensor_tensor(out=lg[:], in0=pR[:, 0:36], in1=br[:], op=ALU.add), R=[pR, br], W=[lg])
                    op("dve", lambda h: h.reduce_max(out=gmx[:], in_=lg[:, 0:4], axis=AX.X), R=[lg], W=[gmx])
                    op("dve", lambda h: h.tensor_scalar_mul(out=ngm[:], in0=gmx[:], scalar1=-1.0), R=[gmx], W=[ngm])
                    op("act", lambda h: h.activation(out=gex[:], in_=lg[:, 0:4], func=AF.Exp, bias=ngm[:], accum_out=gsum[:]), R=[lg, ngm], W=[gex, gsum])
                    op("dve", lambda h: h.reciprocal(out=gsum[:], in_=gsum[:]), R=[gsum], W=[gsum])
                    op("dve", lambda h: h.tensor_scalar(out=oh[:], in0=lg[:, 0:4], scalar1=gmx[:], scalar2=1.0e9, op0=ALU.is_equal, op1=ALU.mult), R=[lg, gmx], W=[oh])
                    op("dve", lambda h: h.tensor_scalar_add(out=oh[:], in0=oh[:], scalar1=-1.0e9), R=[oh], W=[oh])
                    op("dve", lambda h: h.tensor_tensor(out=elm[:], in0=lg[:, 4:36].rearrange("p (a b) -> p a b", a=4), in1=oh[:].unsqueeze(2).to_broadcast([128, 4, 8]), op=ALU.add), R=[lg, oh], W=[elm])
                    op("dve", lambda h: h.max(out=m8[:], in_=elm[:].rearrange("p a b -> p (a b)")), R=[elm], W=[m8])
                    op("dve", lambda h: h.tensor_tensor(out=dd[:], in0=m8[:, 1:2], in1=m8[:, 0:1], op=ALU.subtract), R=[m8], W=[dd])
                    op("act", lambda h: h.activation(out=dd[:], in_=dd[:], func=AF.Exp), R=[dd], W=[dd])
                    op("dve", lambda h: h.tensor_scalar_add(out=w12[:, 0:1], in0=dd[:], scalar1=1.0), R=[dd], W=[w12])
                    op("dve", lambda h: h.reciprocal(out=w12[:, 0:1], in_=w12[:, 0:1]), R=[w12], W=[w12])
                    op("dve", lambda h: h.tensor_tensor(out=w12[:, 0:1], in0=w12[:, 0:1], in1=gsum[:], op=ALU.mult), R=[w12, gsum], W=[w12])
                    op("dve", lambda h: h.tensor_tensor(out=w12[:, 1:2], in0=w12[:, 0:1], in1=dd[:], op=ALU.mult), R=[w12, dd], W=[w12])
                    op("dve", lambda h: h.tensor_scalar(out=mm1[:], in0=elm[:].rearrange("p a b -> p (a b)"), scalar1=m8[:, 0:1], scalar2=w12[:, 0:1], op0=ALU.is_equal, op1=ALU.mult), R=[elm, m8, w12], W=[mm1])
                    op("dve", lambda h: h.tensor_scalar(out=mm2[:], in0=elm[:].rearrange("p a b -> p (a b)"), scalar1=m8[:, 1:2], scalar2=w12[:, 1:2], op0=ALU.is_equal, op1=ALU.mult), R=[elm, m8, w12], W=[mm2])
                    op("dve", lambda h: h.tensor_tensor(out=wte[:, ti * 4 + sub, :], in0=mm1[:], in1=mm2[:], op=ALU.add), R=[mm1, mm2], W=[wte])
                dma(h2T_d[:, c0:c0 + T2].rearrange("(c p) n -> p c n", p=128), h2T[:], h2sem, R=[h2T])
            S.barrier()

        if phases >= 3:
          with ExitStack() as s3:
            Q = min(1024, NO); NQS = Q // 128
            h2q = sb(s3, "h2q", [128, 8, Q], BF16); hqsem = S.newsem("hq")
            acc = sb(s3, "acc", [128, NQS, D])
            wgf = sb(s3, "wgf", [128, 8, DE]); wuf = sb(s3, "wuf", [128, 8, DE]); wdf = sb(s3, "wdf", [128, 4, D])
            fsem = [S.newsem("mf%d" % i) for i in range(3)]
            wgb = [sb(s3, "wgb%d" % i, [128, 8, DE], BF16) for i in range(2)]; wub = [sb(s3, "wub%d" % i, [128, 8, DE], BF16) for i in range(2)]
            wdb = [sb(s3, "wdb%d" % i, [128, 4, D], BF16) for i in range(2)]
            sgt = sb(s3, "sgt", [128, 512]); AT = sb(s3, "AT", [128, 4, 512], BF16)
            xf = [sb(s3, "xf%d" % i, [128, D]) for i in range(2)]; xfsem = [S.newsem("xf%d" % i) for i in range(2)]
            ss3 = sb(s3, "ss3", [128, 1]); junk3 = sb(s3, "junk3", [128, D], BF16)
            ob = [sb(s3, "ob%d" % i, [128, D]) for i in range(2)]; osem = [S.newsem("ob%d" % i) for i in range(2)]
            pG = [ps(s3, "pG%d" % i, [128, 512]) for i in range(2)]; pU = [ps(s3, "pU%d" % i, [128, 512]) for i in range(2)]
            pY = [ps(s3, "pY%d" % i, [128, 512]) for i in range(2)]
            gi_ = [0]; yi_ = [0]
            for qi in range(NO // Q):
                q0 = qi * Q
                dma(h2q[:], h2T_d[:, q0:q0 + Q].rearrange("(c p) n -> p c n", p=128), hqsem, W=[h2q])
                op("pool", lambda h: h.memset(acc[:], 0.0), W=[acc])
                for e in range(NE):
                    b = e % 2
                    dma(wgf[:], moe_wg[e].rearrange("(kc p) n -> p kc n", p=128), fsem[0], W=[wgf])
                    dma(wuf[:], moe_wu[e].rearrange("(kc p) n -> p kc n", p=128), fsem[1], W=[wuf])
                    dma(wdf[:], moe_wd[e].rearrange("(kc p) n -> p kc n", p=128), fsem[2], W=[wdf])
                    op("act", lambda h: h.activation(out=wgb[b][:], in_=wgf[:], func=AF.Copy), R=[wgf], W=[wgb[b]])
                    op("act", lambda h: h.activation(out=wub[b][:], in_=wuf[:], func=AF.Copy), R=[wuf], W=[wub[b]])
                    op("pool", lambda h: h.tensor_copy(out=wdb[b][:, 0:2, :], in_=wdf[:, 0:2, :]), R=[wdf], W=[wdb[b]])
                    op("dve", lambda h: h.tensor_copy(out=wdb[b][:, 2:4, :], in_=wdf[:, 2:4, :]), R=[wdf], W=[wdb[b]])
                    for hf in range(Q // 512):
                        ts_ = slice(hf * 512, (hf + 1) * 512)
                        for oc in range(4):
                            g_ = pG[gi_[0] % 2]; u_ = pU[gi_[0] % 2]; gi_[0] += 1
                            S.group("pe", [lambda h, kc=kc: h.matmul(g_[:], lhsT=wgb[b][:, kc, oc * 128:(oc + 1) * 128], rhs=h2q[:, kc, ts_], start=(kc == 0), stop=(kc == 7)) for kc in range(8)], R=[wgb[b], h2q], W=[g_])
                            S.group("pe", [lambda h, kc=kc: h.matmul(u_[:], lhsT=wub[b][:, kc, oc * 128:(oc + 1) * 128], rhs=h2q[:, kc, ts_], start=(kc == 0), stop=(kc == 7)) for kc in range(8)], R=[wub[b], h2q], W=[u_])
                            op("act", lambda h: h.activation(out=sgt[:], in_=g_[:], func=AF.Silu), R=[g_], W=[sgt])
                            op("dve", lambda h: h.tensor_tensor(out=AT[:, oc, :], in0=u_[:], in1=sgt[:], op=ALU.mult), R=[u_, sgt], W=[AT])
                        for sub in range(4):
                            si = hf * 4 + sub
                            for ch in range(2):
                                y_ = pY[yi_[0] % 2]; yi_[0] += 1
                                S.group("pe", [lambda h, kc=kc: h.matmul(y_[:], lhsT=AT[:, kc, sub * 128:(sub + 1) * 128], rhs=wdb[b][:, kc, ch * 512:(ch + 1) * 512], start=(kc == 0), stop=(kc == 3)) for kc in range(4)], R=[AT, wdb[b]], W=[y_])
                                gs = qi * NQS + si
                                op("dve", lambda h: h.scalar_tensor_tensor(out=acc[:, si, ch * 512:(ch + 1) * 512], in0=y_[:], scalar=wte[:, gs, e:e + 1], in1=acc[:, si, ch * 512:(ch + 1) * 512], op0=ALU.mult, op1=ALU.add), R=[y_, wte, acc], W=[acc])
                for si in range(NQS):
                    r0 = q0 + si * 128
                    x_ = xf[si % 2]; o_ = ob[si % 2]
                    dma(x_[:], x2_d[r0:r0 + 128, :], xfsem[si % 2], W=[x_])
                    op("pool", lambda h: h.tensor_tensor(out=acc[:, si, :], in0=acc[:, si, :], in1=gate2[:], op=ALU.mult), R=[acc, gate2], W=[acc])
                    op("dve", lambda h: h.tensor_tensor(out=x_[:], in0=x_[:], in1=acc[:, si, :], op=ALU.add), R=[x_, acc], W=[x_])
                    op("act", lambda h: h.activation(out=junk3[:], in_=x_[:], func=AF.Square, scale=1.0 / 32, accum_out=ss3[:]), R=[x_], W=[junk3, ss3])
                    op("dve", lambda h: h.tensor_scalar_add(out=ss3[:], in0=ss3[:], scalar1=EPS), R=[ss3], W=[ss3])
                    op("act", lambda h: h.activation(out=ss3[:], in_=ss3[:], func=AF.Sqrt), R=[ss3], W=[ss3])
                    op("dve", lambda h: h.reciprocal(out=ss3[:], in_=ss3[:]), R=[ss3], W=[ss3])
                    op("dve", lambda h: h.scalar_tensor_tensor(out=o_[:], in0=x_[:], scalar=ss3[:, 0:1], in1=fnw[:], op0=ALU.mult, op1=ALU.mult), R=[x_, ss3, fnw], W=[o_])
                    dma(out_d[r0:r0 + 128, :], o_[:], osem[si % 2], R=[o_])
            S.barrier()

        if dbg and phases == 1:
            with ExitStack() as sd:
                a = sb(sd, "dba", [128, NO], BF16); b = sb(sd, "dbb", [128, D], BF16)
                for ch in range(8):
                    dma(a[:], yrgT_d[ch * 128:(ch + 1) * 128, :], ld, W=[a]); dma(dbg_out["d_yrgT"][ch * 128:(ch + 1) * 128, :], a[:], ld, R=[a])
                for r in range(NO // 128):
                    dma(b[:], ydn_d[r * 128:(r + 1) * 128, :], ld, W=[b]); dma(dbg_out["d_ydn"][r * 128:(r + 1) * 128, :], b[:], ld, R=[b])
                S.barrier()
        if dbg and phases >= 2:
            with ExitStack() as sd:
                b = sb(sd, "dbc", [128, D]); dsa = S.newsem("dsa"); dsb = S.newsem("dsb"); dsc = S.newsem("dsc")
                for r in range(NO // 128):
                    dma(b[:], x2_d[r * 128:(r + 1) * 128, :], dsa, W=[b]); dma(dbg_out["d_x2"][r * 128:(r + 1) * 128, :], b[:], dsb, R=[b])
                    dma(dbg_out["d_wte"][r * 128:(r + 1) * 128, :], wte[:, r, :], dsc, R=[wte])
                S.barrier()
        S.barrier()
    return nc


def _col(v, n=8):
    return np.ascontiguousarray(np.asarray(v, np.float32).reshape(n, 128).T)


def _rep(v, p=128):
    v = np.asarray(v, np.float32).reshape(1, -1)
    return np.ascontiguousarray(np.repeat(v, p, axis=0))


def shared_inputs(I):
    f = lambda a: np.ascontiguousarray(np.asarray(a, np.float32))
    d = {}
    d["w_ada"] = f(I["w_ada"][0]); d["b_ada_rep"] = _rep(I["b_ada"][0])
    d["n1w_col"] = _col(I["norm1_w"][0]); d["n2w_col"] = _col(I["norm2_w"][0]); d["fnw_rep"] = _rep(I["final_norm_w"])
    d["w_in"] = f(I["w_in"][0])
    d["rgcw"] = np.ascontiguousarray(f(I["rg_conv_w"][0]).reshape(4, 8, 128).transpose(2, 1, 0))
    d["rgcb"] = _col(I["rg_conv_b"][0])
    d["dncw"] = np.ascontiguousarray(f(I["dn_conv_w"][0]).reshape(4, 24, 128).transpose(2, 1, 0))
    ga = f(I["rg_gate_a_w"][0]).reshape(4, 2, 128, 256); gx = f(I["rg_gate_x_w"][0]).reshape(4, 2, 128, 256)
    d["rga_w"] = np.ascontiguousarray(ga.transpose(2, 0, 1, 3).reshape(128, 8, 256))
    d["rgx_w"] = np.ascontiguousarray(gx.transpose(2, 0, 1, 3).reshape(128, 8, 256))
    d["rga_b"] = _col(f(I["rg_gate_a_b"][0]).reshape(-1)); d["rgx_b"] = _col(f(I["rg_gate_x_b"][0]).reshape(-1))
    d["lam"] = _col(I["rg_lambda"][0])
    d["alog_rep"] = _rep(I["dn_a_log"][0], 64); d["dtb_rep"] = _rep(I["dn_dt_bias"][0], 64)
    d["dnw_rep"] = _rep(np.tile(f(I["dn_norm_w"][0]), 8), 64)
    d["w_brg"] = f(I["w_branch_rg"][0]); d["w_bdn"] = f(I["w_branch_dn"][0]); d["w_out"] = f(I["w_out"][0])
    d["w_r36"] = np.ascontiguousarray(np.concatenate([f(I["moe_w_group"][0]), f(I["moe_w_router"][0])], axis=1))
    d["b_r36_rep"] = _rep(np.concatenate([f(I["moe_b_group"][0]), f(I["moe_b_router"][0])]))
    d["moe_wg"] = f(I["moe_w_gate"][0]); d["moe_wu"] = f(I["moe_w_up"][0]); d["moe_wd"] = f(I["moe_w_down"][0])
    i = np.arange(64)
    d["c_ident"] = np.eye(128, dtype=np.float32)
    d["c_ut"] = (i[:, None] <= i[None, :]).astype(np.float32)
    d["c_maskgt"] = (i[:, None] > i[None, :]).astype(np.float32)
    d["c_neg"] = np.where(i[None, :] > i[:, None], -30000.0, 0.0).astype(np.float32)
    d["c_strict"] = (i[:, None] > i[None, :]).astype(np.float32)
    return d


def core_inputs(I, shared, b, half, NP, NO):
    x = np.asarray(I["x"], np.float32)
    d = dict(shared)
    own = x[b, half * NO:(half + 1) * NO]
    d["xo"] = np.ascontiguousarray(own)
    d["xp"] = np.ascontiguousarray(x[b, 0:NP]) if half == 1 else np.ascontiguousarray(own[0:NP])
    d["flag"] = np.full((128, 1), float(half), np.float32)
    d["ccol"] = _col(np.asarray(I["c"], np.float32)[b])
    return d


def kernel(**inputs):
    NP = NO = 4096
    nc = build(NP, NO)
    sh = shared_inputs(inputs)
    in_maps = [core_inputs(inputs, sh, b, half, NP, NO) for b in range(4) for half in range(2)]
    res = run_bass_kernel_spmd(nc, in_maps, core_ids=list(range(8)))
    out = np.empty((4, 2 * NO, D), np.float32)
    for i, r in enumerate(res.results):
        b, half = divmod(i, 2)
        out[b, half * NO:(half + 1) * NO] = np.asarray(r["out"], np.float32)
    return out
```

```python
import numpy as np
from contextlib import ExitStack
import concourse.bass as bass
import concourse.mybir as mybir
from concourse.bass_utils import run_bass_kernel_spmd

F32 = mybir.dt.float32
BF16 = mybir.dt.bfloat16
AF = mybir.ActivationFunctionType
ALU = mybir.AluOpType
AX = mybir.AxisListType

D = 1024
D_IN = 8208
OFF_RGX, OFF_RGY, OFF_Q, OFF_K, OFF_V, OFF_Z, OFF_BA, OFF_GRG, OFF_GDN = 0, 1024, 2048, 3072, 4096, 5120, 6144, 6160, 7184
NE = 32
DE = 512
EPS = 1e-6
TT = 256
CH = 64
NCH = TT // CH


class Buf:
    __slots__ = ("w", "r")

    def __init__(self):
        self.w = None
        self.r = {}


class T:
    def __init__(self, t):
        self.t = t
        self.b = Buf()

    def __getitem__(self, k):
        return self.t[k]


class TV:
    def __init__(self, t, lo, hi):
        self.t = t; self.lo = lo; self.hi = hi
        self.b = Buf()

    def __getitem__(self, k):
        assert k == slice(None)
        return self.t[:, self.lo:self.hi]


class TV3:
    def __init__(self, parent, sub):
        self.p = parent; self.sub = sub
        self.b = parent.b

    def __getitem__(self, k):
        assert k == slice(None)
        return self.p.t[:, self.sub, :]


class SemCounter:
    def __init__(self, nc, stack, name):
        self.h = stack.enter_context(nc.semaphore(name))
        self.n = 0


class Sync:
    def __init__(self, nc, stack):
        self.nc = nc
        self.eng = {"pe": nc.tensor, "act": nc.scalar, "dve": nc.vector, "pool": nc.gpsimd, "sp": nc.sync}
        self.sem = {k: stack.enter_context(nc.semaphore("s_" + k)) for k in ("pe", "act", "dve", "pool")}
        self.cnt = {k: 0 for k in self.sem}
        self.seen = {k: {} for k in self.eng}
        self.dsems = []
        self.stack = stack

    def newsem(self, name):
        s = SemCounter(self.nc, self.stack, name)
        self.dsems.append(s)
        return s

    def _need(self, e, tok, waits):
        if tok is None:
            return
        k, v = tok
        if k == e and e == "pe":
            return
        if self.seen[e].get(k, 0) < v:
            waits[k] = max(waits.get(k, 0), v)

    def _waits(self, e, reads, writes):
        waits = {}
        for b in reads:
            self._need(e, b.b.w, waits)
        for b in writes:
            self._need(e, b.b.w, waits)
            for k, v in b.b.r.items():
                self._need(e, (k, v), waits)
        h = self.eng[e]
        for k, v in waits.items():
            h.wait_ge(self.sem[k] if isinstance(k, str) else k, v)
            self.seen[e][k] = v
        return h

    def op(self, e, fn, R=(), W=()):
        h = self._waits(e, R, W)
        ins = fn(h)
        self.cnt[e] += 1
        ins.then_inc(self.sem[e], 1)
        tok = (e, self.cnt[e])
        for b in R:
            b.b.r[e] = self.cnt[e]
        for b in W:
            b.b.w = tok
            b.b.r = {}
        return tok

    def group(self, e, fns, R=(), W=()):
        h = self._waits(e, R, W)
        ins = None
        for fn in fns:
            ins = fn(h)
        self.cnt[e] += 1
        ins.then_inc(self.sem[e], 1)
        tok = (e, self.cnt[e])
        for b in R:
            b.b.r[e] = self.cnt[e]
        for b in W:
            b.b.w = tok
            b.b.r = {}
        return tok

    def dma(self, out, in_, sem, R=(), W=(), q="sp", **kw):
        h = self._waits(q, R, W)
        sem.n += 16
        h.dma_start(out=out, in_=in_, **kw).then_inc(sem.h, 16)
        tok = (sem.h, sem.n)
        for b in R:
            b.b.r[sem.h] = sem.n
        for b in W:
            b.b.w = tok
            b.b.r = {}
        return tok

    def barrier(self):
        for e, h in self.eng.items():
            for k in self.sem:
                if k != e and self.seen[e].get(k, 0) < self.cnt[k]:
                    h.wait_ge(self.sem[k], self.cnt[k])
                    self.seen[e][k] = self.cnt[k]
            for s in self.dsems:
                if s.n and self.seen[e].get(s.h, 0) < s.n:
                    h.wait_ge(s.h, s.n)
                    self.seen[e][s.h] = s.n


def build(NP, NO, phases=3, dbg=False, cut=99):
    assert NP % TT == 0 and NO % TT == 0
    nc = bass.Bass("TRN2", target_bir_lowering=False)

    def din(name, shape, dt=F32):
        return nc.dram_tensor(name, list(shape), dt, kind="ExternalInput").ap()

    def dscr(name, shape, dt):
        return nc.dram_tensor(name, list(shape), dt, kind="Internal").ap()

    xp = din("xp", [NP, D]); xo = din("xo", [NO, D]); flag_d = din("flag", [128, 1]); ccol_d = din("ccol", [128, 8])
    w_ada = din("w_ada", [D, 6 * D]); b_ada_rep = din("b_ada_rep", [128, 6 * D])
    n1w_d = din("n1w_col", [128, 8]); n2w_d = din("n2w_col", [128, 8]); fnw_d = din("fnw_rep", [128, D])
    w_in = din("w_in", [D, D_IN])
    rgcw_d = din("rgcw", [128, 8, 4]); rgcb_d = din("rgcb", [128, 8]); dncw_d = din("dncw", [128, 24, 4])
    rgaw_d = din("rga_w", [128, 8, 256]); rgxw_d = din("rgx_w", [128, 8, 256])
    rgab_d = din("rga_b", [128, 8]); rgxb_d = din("rgx_b", [128, 8]); lam_d = din("lam", [128, 8])
    alog_d = din("alog_rep", [64, 8]); dtb_d = din("dtb_rep", [64, 8]); dnw_d = din("dnw_rep", [64, D])
    w_brg = din("w_brg", [D, D]); w_bdn = din("w_bdn", [D, D]); w_out = din("w_out", [D, D])
    w_r36 = din("w_r36", [D, 36]); b_r36_d = din("b_r36_rep", [128, 36])
    moe_wg = din("moe_wg", [NE, D, DE]); moe_wu = din("moe_wu", [NE, D, DE]); moe_wd = din("moe_wd", [NE, DE, D])
    c_ident = din("c_ident", [128, 128]); c_ut = din("c_ut", [64, 64]); c_maskgt = din("c_maskgt", [64, 64])
    c_neg = din("c_neg", [64, 64]); c_strict = din("c_strict", [64, 64])
    out_d = nc.dram_tensor("out", [NO, D], F32, kind="ExternalOutput").ap()
    w_in_bf = dscr("w_in_bf", [D, D_IN], BF16)
    yrgT_d = dscr("yrgT", [D, NO], BF16)
    ydn_d = dscr("ydn", [NO, D], BF16)
    x2_d = dscr("x2", [NO, D], F32)
    h2T_d = dscr("h2T", [D, NO], BF16)
    wte_d = dscr("wte", [NO, NE], F32)
    dbg_out = {}
    if dbg:
        dbg_out["d_yrgT"] = nc.dram_tensor("d_yrgT", [D, NO], BF16, kind="ExternalOutput").ap()
        dbg_out["d_ydn"] = nc.dram_tensor("d_ydn", [NO, D], BF16, kind="ExternalOutput").ap()
        dbg_out["d_x2"] = nc.dram_tensor("d_x2", [NO, D], F32, kind="ExternalOutput").ap()
        dbg_out["d_wte"] = nc.dram_tensor("d_wte", [NO, NE], F32, kind="ExternalOutput").ap()

    with ExitStack() as st:
        S = Sync(nc, st)
        op, dma = S.op, S.dma

        def act_sigmoid(out_ap, outT, in_ap, inR, scale=1.0, nbias=None, nbR=()):
            if nbias is not None:
                op("act", lambda h: h.activation(out=out_ap, in_=in_ap, func=AF.Exp, scale=-scale, bias=nbias), R=list(inR) + list(nbR), W=[outT])
            else:
                op("act", lambda h: h.activation(out=out_ap, in_=in_ap, func=AF.Exp, scale=-scale), R=list(inR), W=[outT])
            op("act", lambda h: h.activation(out=out_ap, in_=out_ap, func=AF.Ln, bias=1.0), R=[outT], W=[outT])
            op("act", lambda h: h.activation(out=out_ap, in_=out_ap, func=AF.Exp, scale=-1.0), R=[outT], W=[outT])

        def act_rsqrt(out_ap, outT):
            op("act", lambda h: h.activation(out=out_ap, in_=out_ap, func=AF.Ln), R=[outT], W=[outT])
            op("act", lambda h: h.activation(out=out_ap, in_=out_ap, func=AF.Exp, scale=-0.5), R=[outT], W=[outT])

        def sb(stk, name, shape, dt=F32):
            return T(stk.enter_context(nc.sbuf_tensor("s_" + name, list(shape), dt)))

        def ps(stk, name, shape, dt=F32):
            return T(stk.enter_context(nc.psum_tensor("p_" + name, list(shape), dt)))

        st.enter_context(nc.Block())
        ld = S.newsem("ld")
        so = [S.newsem("so%d" % i) for i in range(4)]

        ident_f = sb(st, "ident_f", [128, 128]); ident_b = sb(st, "ident_b", [128, 128], BF16)
        ones_b = sb(st, "ones_b", [128, 128], BF16); ones_f = sb(st, "ones_f", [64, 128])
        flag = sb(st, "flag", [128, 1])
        cols = sb(st, "cols", [128, 4, 8])
        A1 = sb(st, "A1", [128, 8]); A2 = sb(st, "A2", [128, 8]); n1w = sb(st, "n1w", [128, 8]); n2w = sb(st, "n2w", [128, 8])
        gate1 = sb(st, "gate1", [128, D]); gate2 = sb(st, "gate2", [128, D]); fnw = sb(st, "fnw", [128, D])
        for (t_, d_) in ((ident_f, c_ident), (flag, flag_d), (n1w, n1w_d), (n2w, n2w_d), (fnw, fnw_d)):
            dma(t_[:], d_, ld, W=[t_])
        S.barrier()
        op("dve", lambda h: h.tensor_copy(out=ident_b[:], in_=ident_f[:]), R=[ident_f], W=[ident_b])
        op("pool", lambda h: h.memset(ones_b[:], 1.0), W=[ones_b])
        op("pool", lambda h: h.memset(ones_f[:], 1.0), W=[ones_f])

        with ExitStack() as s0:
            ccol = sb(s0, "ccol", [128, 8]); crep = sb(s0, "crep", [128, 8, 128])
            wsl = [sb(s0, "wsl%d" % i, [128, 8, 512]) for i in range(2)]
            wsem = [S.newsem("wsl%d" % i) for i in range(2)]
            bsl = sb(s0, "bsl", [128, 512]); mtmp = sb(s0, "mtmp", [128, 4, 128]); mt2 = sb(s0, "mt2", [128, 4, 128])
            pm = ps(s0, "pm", [128, 512])
            bsem = S.newsem("bsl")
            dma(ccol[:], ccol_d, ld, W=[ccol])
            S.barrier()
            op("act", lambda h: h.activation(out=ccol[:], in_=ccol[:], func=AF.Silu), R=[ccol], W=[ccol])
            op("dve", lambda h: h.tensor_copy(out=crep[:], in_=ccol[:].unsqueeze(2).to_broadcast([128, 8, 128])), R=[ccol], W=[crep])
            for s in range(12):
                w_ = wsl[s % 2]
                dma(w_[:], w_ada[:, s * 512:(s + 1) * 512].rearrange("(kc p) n -> p kc n", p=128), wsem[s % 2], W=[w_])
                dma(bsl[:], b_ada_rep[:, s * 512:(s + 1) * 512], bsem, W=[bsl])
                S.group("pe", [lambda h, kc=kc: h.matmul(pm[:], lhsT=crep[:, kc, :], rhs=w_[:, kc, :], start=(kc == 0), stop=(kc == 7)) for kc in range(8)], R=[crep, w_], W=[pm])
                v, half = s // 2, s % 2
                if v in (2, 5):
                    g_ = gate1 if v == 2 else gate2
                    op("dve", lambda h: h.tensor_tensor(out=g_[:, half * 512:(half + 1) * 512], in0=pm[:], in1=bsl[:], op=ALU.add), R=[pm, bsl], W=[g_])
                else:
                    ci = {0: 0, 1: 1, 3: 2, 4: 3}[v]
                    op("dve", lambda h: h.tensor_tensor(out=mtmp[:].rearrange("p a b -> p (a b)"), in0=pm[:], in1=bsl[:], op=ALU.add), R=[pm, bsl], W=[mtmp])
                    op("pool", lambda h: h.tensor_tensor(out=mt2[:], in0=mtmp[:], in1=ident_f[:].unsqueeze(1).to_broadcast([128, 4, 128]), op=ALU.mult), R=[mtmp, ident_f], W=[mt2])
                    op("dve", lambda h: h.reduce_sum(out=cols[:, ci, half * 4:(half + 1) * 4], in_=mt2[:], axis=AX.X), R=[mt2], W=[cols])
            op("dve", lambda h: h.scalar_tensor_tensor(out=A1[:], in0=cols[:, 1, :], scalar=1.0, in1=n1w[:], op0=ALU.add, op1=ALU.mult), R=[cols, n1w], W=[A1])
            op("dve", lambda h: h.scalar_tensor_tensor(out=A2[:], in0=cols[:, 3, :], scalar=1.0, in1=n2w[:], op0=ALU.add, op1=ALU.mult), R=[cols, n2w], W=[A2])
            S.barrier()

        with ExitStack() as s0:
          if cut >= 2:
            wf = [sb(s0, "wf%d" % i, [128, 8, 256]) for i in range(4)]
            wb = [sb(s0, "wb%d" % i, [128, 8, 256], BF16) for i in range(4)]
            lsem = [S.newsem("wfl%d" % i) for i in range(4)]
            nsl = (D_IN + 255) // 256
            for s in range(nsl):
                c0 = s * 256; cw = min(256, D_IN - c0)
                f_, b_ = wf[s % 4], wb[s % 4]
                dma(f_[:, :, 0:cw], w_in[:, c0:c0 + cw].rearrange("(kc p) n -> p kc n", p=128), lsem[s % 4], W=[f_])
                op(("dve", "pool", "dve", "act")[s % 4], (lambda h: h.activation(out=b_[:, :, 0:cw], in_=f_[:, :, 0:cw], func=AF.Copy)) if s % 4 == 3 else (lambda h: h.tensor_copy(out=b_[:, :, 0:cw], in_=f_[:, :, 0:cw])), R=[f_], W=[b_])
                dma(w_in_bf[:, c0:c0 + cw].rearrange("(kc p) n -> p kc n", p=128), b_[:, :, 0:cw], so[s % 4], R=[b_])
            S.barrier()

        def norm_to_hT(xt, junk, ss, xn, pT, hT, col0, Acol, Bcol_ap, tmpf):
            op("act", lambda h: h.activation(out=junk[:], in_=xt[:], func=AF.Square, scale=1.0 / 32, accum_out=ss[:]), R=[xt], W=[junk, ss])
            op("dve", lambda h: h.tensor_scalar_add(out=ss[:], in0=ss[:], scalar1=EPS), R=[ss], W=[ss])
            act_rsqrt(ss[:], ss)
            op("dve", lambda h: h.tensor_scalar_mul(out=xn[:], in0=xt[:], scalar1=ss[:]), R=[xt, ss], W=[xn])
            S.group("pe", [lambda h, kc=kc: h.transpose(out=pT[:, kc * 128:(kc + 1) * 128], in_=xn[:, kc * 128:(kc + 1) * 128], identity=ident_b[:]) for kc in range(8)], R=[xn, ident_b], W=[pT])
            op("dve", lambda h: h.tensor_tensor(out=tmpf[:], in0=pT[:].rearrange("p (a b) -> p a b", a=8), in1=Acol[:].unsqueeze(2).to_broadcast([128, 8, 128]), op=ALU.mult), R=[pT, Acol], W=[tmpf])
            op("pool", lambda h: h.tensor_tensor(out=hT[:, :, col0:col0 + 128], in0=tmpf[:], in1=Bcol_ap.unsqueeze(2).to_broadcast([128, 8, 128]), op=ALU.add), R=[tmpf, cols], W=[hT])

        with ExitStack() as s1:
            rgcw = sb(s1, "rgcw", [128, 8, 4]); rgcb = sb(s1, "rgcb", [128, 8]); dncw = sb(s1, "dncw", [128, 24, 4])
            rgab = sb(s1, "rgab", [128, 8]); rgxb = sb(s1, "rgxb", [128, 8]); lamc = sb(s1, "lamc", [128, 8])
            rgw_f = sb(s1, "rgw_f", [128, 8, 256])
            rgaw = sb(s1, "rgaw", [128, 8, 256], BF16); rgxw = sb(s1, "rgxw", [128, 8, 256], BF16)
            alog = sb(s1, "alog", [64, 8]); dtb = sb(s1, "dtb", [64, 8]); dnw = sb(s1, "dnw", [64, D])
            ut = sb(s1, "ut", [64, 64]); maskgt = sb(s1, "maskgt", [64, 64]); negm = sb(s1, "negm", [64, 64]); strict = sb(s1, "strict", [64, 64])
            for (t_, d_) in ((rgcw, rgcw_d), (rgcb, rgcb_d), (dncw, dncw_d), (rgab, rgab_d), (rgxb, rgxb_d), (lamc, lam_d),
                             (alog, alog_d), (dtb, dtb_d), (dnw, dnw_d), (ut, c_ut), (maskgt, c_maskgt), (negm, c_neg), (strict, c_strict)):
                dma(t_[:], d_, ld, W=[t_])
            rgw_f2 = sb(s1, "rgw_f2", [128, 8, 256])
            dma(rgw_f[:], rgaw_d, ld, W=[rgw_f])
            dma(rgw_f2[:], rgxw_d, ld, W=[rgw_f2])
            S.barrier()
            op("dve", lambda h: h.tensor_copy(out=rgaw[:], in_=rgw_f[:]), R=[rgw_f], W=[rgaw])
            op("dve", lambda h: h.tensor_copy(out=rgxw[:], in_=rgw_f2[:]), R=[rgw_f2], W=[rgxw])
            nrgab = sb(s1, "nrgab", [128, 8]); nrgxb = sb(s1, "nrgxb", [128, 8])
            op("dve", lambda h: h.tensor_scalar_mul(out=nrgab[:], in0=rgab[:], scalar1=-1.0), R=[rgab], W=[nrgab])
            op("dve", lambda h: h.tensor_scalar_mul(out=nrgxb[:], in0=rgxb[:], scalar1=-1.0), R=[rgxb], W=[nrgxb])
            op("act", lambda h: h.activation(out=lamc[:], in_=lamc[:], func=AF.Exp, scale=-1.0), R=[lamc], W=[lamc])
            op("act", lambda h: h.activation(out=lamc[:], in_=lamc[:], func=AF.Ln, bias=1.0), R=[lamc], W=[lamc])
            op("dve", lambda h: h.tensor_scalar_mul(out=lamc[:], in0=lamc[:], scalar1=-8.0), R=[lamc], W=[lamc])
            op("act", lambda h: h.activation(out=alog[:], in_=alog[:], func=AF.Exp), R=[alog], W=[alog])
            op("dve", lambda h: h.tensor_scalar_mul(out=alog[:], in0=alog[:], scalar1=-1.0), R=[alog], W=[alog])
            i8b = sb(s1, "i8b", [64, 8, 64], BF16); i8f = sb(s1, "i8f", [64, 8, 64])
            strict8 = sb(s1, "strict8", [64, 8, 64]); maskgt8 = sb(s1, "maskgt8", [64, 8, 64]); neg8 = sb(s1, "neg8", [64, 8, 64])
            op("dve", lambda h: h.tensor_copy(out=i8f[:], in_=ident_f[0:64, 0:64].unsqueeze(1).to_broadcast([64, 8, 64])), R=[ident_f], W=[i8f])
            op("dve", lambda h: h.tensor_copy(out=i8b[:], in_=i8f[:]), R=[i8f], W=[i8b])
            op("dve", lambda h: h.tensor_copy(out=strict8[:], in_=strict[:].unsqueeze(1).to_broadcast([64, 8, 64])), R=[strict], W=[strict8])
            op("dve", lambda h: h.tensor_copy(out=maskgt8[:], in_=maskgt[:].unsqueeze(1).to_broadcast([64, 8, 64])), R=[maskgt], W=[maskgt8])
            op("dve", lambda h: h.tensor_copy(out=neg8[:], in_=negm[:].unsqueeze(1).to_broadcast([64, 8, 64])), R=[negm], W=[neg8])

            carry = sb(s1, "carry", [128, 32, 3]); hst = sb(s1, "hst", [128, 8])
            Sf = sb(s1, "Sf", [128, 8, 128]); Sb = sb(s1, "Sb", [128, 8, 128], BF16)
            for t_ in (carry, hst, Sf, Sb):
                op("pool", lambda h: h.memset(t_[:], 0.0), W=[t_])

            xt = [sb(s1, "xt%d" % i, [128, D]) for i in range(2)]; xsem = [S.newsem("xs%d" % i) for i in range(2)]
            junk = sb(s1, "junk", [128, D], BF16); ss = sb(s1, "ss", [128, 1]); xn = sb(s1, "xn", [128, D], BF16)
            tmpf = sb(s1, "tmpf", [128, 8, 128])
            hT = sb(s1, "hT", [128, 8, TT], BF16); hT_b = sb(s1, "hT_b", [128, 8, TT], BF16)
            NSLAB = 4
            slabs = [sb(s1, "slab%d" % i, [128, 8, 128], BF16) for i in range(NSLAB)]; slsem = [S.newsem("sl%d" % i) for i in range(NSLAB)]
            pre_r = sb(s1, "pre", [128, TT + 3]); preD = [sb(s1, "preD%d" % i, [128, TT + 3]) for i in range(2)]; d1 = [sb(s1, "d1_%d" % i, [128, TT]) for i in range(2)]; d2 = [sb(s1, "d2_%d" % i, [128, TT]) for i in range(2)]; d3 = [sb(s1, "d3_%d" % i, [128, TT]) for i in range(2)]; sqbD = [sb(s1, "sqbD%d" % i, [128, TT], BF16) for i in range(2)]; vTD = [sb(s1, "vTD%d" % i, [128, TT], BF16) for i in range(2)]; xc = sb(s1, "xc", [128, 2, TT]); xcb = sb(s1, "xcb", [128, 2, TT], BF16)
            t1 = sb(s1, "t1", [128, TT]); t2 = sb(s1, "t2", [128, TT]); t3 = sb(s1, "t3", [128, TT]); t4 = sb(s1, "t4", [128, TT])
            sqb = sb(s1, "sqb", [128, TT], BF16)
            yst = [sb(s1, "yst%d" % i, [128, TT], BF16) for i in range(2)]
            qT = sb(s1, "qT", [128, 8, TT], BF16); kT = sb(s1, "kT", [128, 8, TT], BF16); vT = sb(s1, "vT", [128, TT], BF16)
            vtok = sb(s1, "vtok", [64, NCH, 8, 128], BF16); ktok = sb(s1, "ktok", [64, NCH, 8, 128], BF16)
            zw = sb(s1, "zw", [64, NCH, D], BF16); ztmp = sb(s1, "ztmp", [64, NCH, 128])
            ba = sb(s1, "ba", [64, NCH, 16])
            beta = sb(s1, "beta", [64, 8]); gstep = sb(s1, "gstep", [64, 8]); gsb = sb(s1, "gsb", [64, 16]); Gam = sb(s1, "Gam", [64, 8])
            dec = sb(s1, "dec", [64, 8]); bG = sb(s1, "bG", [64, 8]); geT = sb(s1, "geT", [128, 8])
            Rm = sb(s1, "Rm", [64, 8, 64]); E = sb(s1, "E", [64, 8, 64]); Es = sb(s1, "Es", [64, 8, 64]); tA = sb(s1, "tA", [64, 8, 64])
            Ak = [sb(s1, "Ak%d" % i, [64, 8, 64]) for i in range(2)]; Bk = [sb(s1, "Bk%d" % i, [64, 8, 64]) for i in range(2)]
            Mk = [sb(s1, "Mk%d" % i, [64, 8, 64]) for i in range(2)]; Mb = sb(s1, "Mb", [64, 8, 64], BF16)
            qk = sb(s1, "qk", [64, 8, 64], BF16); qkT_ = sb(s1, "qkT", [64, 8, 64], BF16)
            bv = sb(s1, "bv", [64, 8, 128], BF16); bgk = sb(s1, "bgk", [64, 8, 128], BF16); kdec = sb(s1, "kdec", [64, 8, 128], BF16)
            wTn = sb(s1, "wTn", [128, 8, 64], BF16); dG = sb(s1, "dG", [64, 8, 64]); qg = sb(s1, "qg", [128, 8, 64], BF16)
            vnew = sb(s1, "vnew", [64, 8, 128], BF16); osb = sb(s1, "osb", [64, 8, 128]); osq = sb(s1, "osq", [64, 8, 128])
            oss = sb(s1, "oss", [64, 8]); ydst = [sb(s1, "ydst%d" % i, [64, D], BF16) for i in range(2)]
            pA = [ps(s1, "pA%d" % i, [128, TT]) for i in range(2)]
            pT = ps(s1, "pT", [128, 1024], BF16)
            pX = ps(s1, "pX", [128, 512]); pK = ps(s1, "pK", [128, 512]); pCh = ps(s1, "pCh", [128, 512]); pV = ps(s1, "pV", [128, 1024])
            S.barrier()

            ntile = (NP + NO) // TT
            npre = NP // TT

            def rg_slabs(own):
                l = []
                for g in range(4):
                    l += [OFF_RGX + (2 * g) * 128, OFF_RGX + (2 * g + 1) * 128]
                    if own:
                        l += [OFF_RGY + (2 * g) * 128, OFF_RGY + (2 * g + 1) * 128]
                return l

            def dnt_slabs(own):
                l = []
                for pp in range(4):
                    for o_ in (OFF_Q, OFF_K, OFF_V):
                        l += [o_ + (2 * pp) * 128, o_ + (2 * pp + 1) * 128]
                l += [OFF_BA]
                if own:
                    l += [OFF_Z + j * 128 for j in range(8)]
                return l

            sched = rg_slabs(0 >= npre)
            for ti in range(ntile):
                sched += dnt_slabs(ti >= npre)
                if ti + 1 < ntile:
                    sched += rg_slabs(ti + 1 >= npre)
            state = {"issued": 0, "used": 0}

            def issue_slab():
                i = state["issued"]
                if i >= len(sched):
                    return
                c0 = sched[i]; cw = min(128, D_IN - c0)
                sl = slabs[i % NSLAB]
                dma(sl[:, :, 0:cw], w_in_bf[:, c0:c0 + cw].rearrange("(kc p) n -> p kc n", p=128), slsem[i % NSLAB], W=[sl])
                state["issued"] += 1

            def next_slab(expect=None):
                assert expect is None or sched[state["used"]] == expect, (expect, sched[state["used"]], state["used"])
                while state["issued"] < min(len(sched), state["used"] + NSLAB - 1) or state["issued"] <= state["used"]:
                    issue_slab()
                sl = slabs[state["used"] % NSLAB]
                state["used"] += 1
                return sl

            pa_i = [0]

            def proj_fm(sl, hT, ptile=None):
                if ptile is not None:
                    p = ptile
                else:
                    p = pA[pa_i[0] % 2]; pa_i[0] += 1
                S.group("pe", [lambda h, kc=kc: h.matmul(p[:], lhsT=sl[:, kc, :], rhs=hT[:, kc, :], start=(kc == 0), stop=(kc == 7)) for kc in range(8)], R=[sl, hT], W=[p])
                return p

            def conv(p, cidx, wcol, bias_ap, dst_ap, dstT, pre=None):
                pre = pre if pre is not None else pre_r
                op("pool", lambda h: h.tensor_copy(out=pre[:, 0:3], in_=carry[:, cidx, :]), R=[carry], W=[pre])
                op("act", lambda h: h.activation(out=pre[:, 3:3 + TT], in_=p[:], func=AF.Copy), R=[p], W=[pre])
                if bias_ap is not None:
                    op("act", lambda h: h.activation(out=dst_ap, in_=p[:], func=AF.Identity, scale=wcol[:, 3:4], bias=bias_ap), R=[p], W=[dstT])
                else:
                    op("act", lambda h: h.activation(out=dst_ap, in_=p[:], func=AF.Identity, scale=wcol[:, 3:4]), R=[p], W=[dstT])
                op("pool", lambda h: h.tensor_copy(out=carry[:, cidx, :], in_=pre[:, TT:TT + 3]), R=[pre], W=[carry])
                for j in range(0, 3):
                    op("dve", lambda h: h.scalar_tensor_tensor(out=dst_ap, in0=pre[:, j:j + TT], scalar=wcol[:, j:j + 1], in1=dst_ap, op0=ALU.mult, op1=ALU.add), R=[pre, dstT], W=[dstT])

            st_i = [0]

            hTs = [hT, hT_b]

            def emit_prep(ti):
                hTt = hTs[ti % 2]
                own = ti >= npre
                tok0 = ti * TT
                for sub in range(TT // 128):
                    r0 = tok0 + sub * 128
                    src = xp[r0:r0 + 128, :] if r0 < NP else xo[r0 - NP:r0 - NP + 128, :]
                    xi = (ti * (TT // 128) + sub) % 2
                    dma(xt[xi][:], src, xsem[xi], W=[xt[xi]])
                    norm_to_hT(xt[xi], junk, ss, xn, pT, hTt, sub * 128, A1, cols[:, 0, :], tmpf)

            def gen_rg(ti):
                own = ti >= npre; tok0 = ti * TT; hTt = hTs[ti % 2]
                if ti == npre:
                    op("dve", lambda h: h.tensor_scalar_mul(out=carry[:, 0:8, :], in0=carry[:, 0:8, :], scalar1=flag[:, 0:1]), R=[carry, flag], W=[carry])
                    op("dve", lambda h: h.tensor_scalar_mul(out=hst[:], in0=hst[:], scalar1=flag[:, 0:1]), R=[hst, flag], W=[hst])
                for g in range(4):
                    for jc in range(2):
                        ch = 2 * g + jc
                        p = proj_fm(next_slab(), hTt)
                        yield
                        conv(p, ch, rgcw[:, ch, :], rgcb[:, ch:ch + 1], xc[:, jc, :], xc)
                        yield
                    ypp = []
                    if own:
                        for jc in range(2):
                            ypp.append(next_slab())
                    op("pool", lambda h: h.tensor_copy(out=xcb[:], in_=xc[:]), R=[xc], W=[xcb])
                    yield
                    for oc in range(2):
                        ch = 2 * g + oc
                        p = pA[pa_i[0] % 2]; pa_i[0] += 1
                        S.group("pe", [lambda h, ic=ic: h.matmul(p[:], lhsT=rgaw[:, 2 * g + ic, oc * 128:(oc + 1) * 128], rhs=xcb[:, ic, :], start=(ic == 0), stop=(ic == 1)) for ic in range(2)], R=[rgaw, xcb], W=[p])
                        yield
                        act_sigmoid(t1[:], t1, p[:], [p], nbias=nrgab[:, ch:ch + 1], nbR=[nrgab])
                        yield
                        p = pA[pa_i[0] % 2]; pa_i[0] += 1
                        S.group("pe", [lambda h, ic=ic: h.matmul(p[:], lhsT=rgxw[:, 2 * g + ic, oc * 128:(oc + 1) * 128], rhs=xcb[:, ic, :], start=(ic == 0), stop=(ic == 1)) for ic in range(2)], R=[rgxw, xcb], W=[p])
                        yield
                        act_sigmoid(t2[:], t2, p[:], [p], nbias=nrgxb[:, ch:ch + 1], nbR=[nrgxb])
                        yield
                        op("act", lambda h: h.activation(out=t1[:], in_=t1[:], func=AF.Exp, scale=lamc[:, ch:ch + 1]), R=[t1, lamc], W=[t1])
                        yield
                        op("pool", lambda h: h.tensor_tensor(out=t3[:], in0=t1[:], in1=t1[:], op=ALU.mult), R=[t1], W=[t3])
                        yield
                        op("act", lambda h: h.activation(out=t3[:], in_=t3[:], func=AF.Ln, scale=-1.0, bias=1.0), R=[t3], W=[t3])
                        yield
                        op("act", lambda h: h.activation(out=t3[:], in_=t3[:], func=AF.Exp, scale=0.5), R=[t3], W=[t3])
                        yield
                        op("dve", lambda h: h.tensor_tensor(out=t2[:], in0=t2[:], in1=xc[:, oc, :], op=ALU.mult), R=[t2, xc], W=[t2])
                        yield
                        op("dve", lambda h: h.tensor_tensor(out=t2[:], in0=t2[:], in1=t3[:], op=ALU.mult), R=[t2, t3], W=[t2])
                        yield
                        op("dve", lambda h: h.tensor_tensor_scan(out=t4[:], data0=t1[:], data1=t2[:], initial=hst[:, ch:ch + 1], op0=ALU.mult, op1=ALU.add), R=[t1, t2, hst], W=[t4])
                        yield
                        op("pool", lambda h: h.tensor_copy(out=hst[:, ch:ch + 1], in_=t4[:, TT - 1:TT]), R=[t4], W=[hst])
                        yield
                        if own:
                            p = proj_fm(ypp[oc], hTt)
                            yield
                            op("act", lambda h: h.activation(out=t1[:], in_=p[:], func=AF.Square), R=[p], W=[t1])
                            yield
                            op("dve", lambda h: h.tensor_scalar(out=t1[:], in0=t1[:], scalar1=0.044715, scalar2=1.0, op0=ALU.mult, op1=ALU.add), R=[t1], W=[t1])
                            yield
                            op("dve", lambda h: h.tensor_tensor(out=t1[:], in0=t1[:], in1=p[:], op=ALU.mult), R=[t1, p], W=[t1])
                            yield
                            act_sigmoid(t1[:], t1, t1[:], [t1], scale=1.5957691216057308)
                            yield
                            op("dve", lambda h: h.tensor_tensor(out=t1[:], in0=t1[:], in1=p[:], op=ALU.mult), R=[t1, p], W=[t1])
                            yield
                            ys = yst[st_i[0] % 2]; ssm = so[st_i[0] % 2]; st_i[0] += 1
                            op("dve", lambda h: h.tensor_tensor(out=ys[:], in0=t1[:], in1=t4[:], op=ALU.mult), R=[t1, t4], W=[ys])
                            yield
                            c0 = tok0 - NP
                            dma(yrgT_d[ch * 128:(ch + 1) * 128, c0:c0 + TT], ys[:], ssm, R=[ys])
                            yield

            def emit_dnt(ti):
                own = ti >= npre; tok0 = ti * TT; hTt = hTs[ti % 2]
                def gen_head(hh, k):
                    t1 = d1[k]; t2 = d2[k]; t3 = d3[k]; sqb = sqbD[k]; vT = vTD[k]
                    for which, dstT in ((0, qT), (1, kT), (2, vT)):
                        cid = which * 8 + hh
                        p = proj_fm(next_slab((OFF_Q, OFF_K, OFF_V)[which] + hh * 128), hTt, ptile=pA[k])
                        yield
                        conv(p, 8 + cid, dncw[:, cid, :], None, t1[:], t1, pre=preD[k])
                        yield
                        if which == 2:
                            act_sigmoid(t2[:], t2, t1[:], [t1])
                            yield
                            op("dve", lambda h: h.tensor_tensor(out=vT[:], in0=t1[:], in1=t2[:], op=ALU.mult), R=[t1, t2], W=[vT])
                            yield
                            S.group("pe", [lambda h, c=c: h.transpose(out=pT[0:64, c * 128:(c + 1) * 128], in_=vT[:, c * 64:(c + 1) * 64], identity=ident_b[:]) for c in range(NCH)], R=[vT, ident_b], W=[pT])
                            op("dve", lambda h: h.tensor_copy(out=vtok[:, :, hh, :], in_=pT[0:64, 0:NCH * 128].rearrange("p (c d) -> p c d", c=NCH)), R=[pT], W=[vtok])
                            yield
                        else:
                            act_sigmoid(t2[:], t2, t1[:], [t1])
                            yield
                            op("dve", lambda h: h.tensor_tensor(out=t2[:], in0=t1[:], in1=t2[:], op=ALU.mult), R=[t1, t2], W=[t2])
                            yield
                            op("pool", lambda h: h.tensor_tensor(out=sqb[:], in0=t2[:], in1=t2[:], op=ALU.mult), R=[t2], W=[sqb])
                            yield
                            pn = pA[k]
                            op("pe", lambda h: h.matmul(pn[:], lhsT=ones_b[:], rhs=sqb[:], start=True, stop=True), R=[ones_b, sqb], W=[pn])
                            yield
                            op("dve", lambda h: h.tensor_scalar_add(out=t3[:], in0=pn[:], scalar1=EPS), R=[pn], W=[t3])
                            yield
                            act_rsqrt(t3[:], t3)
                            yield
                            sc = (128.0 ** -0.5) if which == 0 else 1.0
                            op("dve", lambda h: h.scalar_tensor_tensor(out=dstT[:, hh, :], in0=t2[:], scalar=sc, in1=t3[:], op0=ALU.mult, op1=ALU.mult), R=[t2, t3], W=[dstT])
                            yield
                            if which == 1:
                                S.group("pe", [lambda h, c=c: h.transpose(out=pT[0:64, c * 128:(c + 1) * 128], in_=kT[:, hh, c * 64:(c + 1) * 64], identity=ident_b[:]) for c in range(NCH)], R=[kT, ident_b], W=[pT])
                                op("dve", lambda h: h.tensor_copy(out=ktok[:, :, hh, :], in_=pT[0:64, 0:NCH * 128].rearrange("p (c d) -> p c d", c=NCH)), R=[pT], W=[ktok])
                                yield
                interleave([chain(*[gen_head(hh, 0) for hh in (0, 2, 4, 6)]), chain(*[gen_head(hh, 1) for hh in (1, 3, 5, 7)])])
                sl = next_slab()
                for c in range(NCH):
                    S.group("pe", [lambda h, kc=kc: h.matmul(pV[0:64, 0:16], lhsT=hTt[:, kc, c * 64:(c + 1) * 64], rhs=sl[:, kc, 0:16], start=(kc == 0), stop=(kc == 7)) for kc in range(8)], R=[hTt, sl], W=[pV])
                    op("act", lambda h: h.activation(out=ba[:, c, :], in_=pV[0:64, 0:16], func=AF.Copy), R=[pV], W=[ba])
                if own:
                    for j in range(8):
                        sl = next_slab()
                        for c in range(NCH):
                            S.group("pe", [lambda h, kc=kc: h.matmul(pX[0:64, c * 128:(c + 1) * 128], lhsT=hTt[:, kc, c * 64:(c + 1) * 64], rhs=sl[:, kc, :], start=(kc == 0), stop=(kc == 7)) for kc in range(8)], R=[hTt, sl], W=[pX])
                        act_sigmoid(ztmp[:], ztmp, pX[0:64, 0:NCH * 128].rearrange("p (c d) -> p c d", c=NCH), [pX])
                        op("dve", lambda h: h.tensor_tensor(out=ztmp[:], in0=ztmp[:], in1=pX[0:64, 0:NCH * 128].rearrange("p (c d) -> p c d", c=NCH), op=ALU.mult), R=[ztmp, pX], W=[ztmp])
                        op("pool", lambda h: h.tensor_tensor(out=zw[:, :, j * 128:(j + 1) * 128], in0=ztmp[:], in1=dnw[:, j * 128:(j + 1) * 128].unsqueeze(1).to_broadcast([64, NCH, 128]), op=ALU.mult), R=[ztmp, dnw], W=[zw])

            def gen_ch(ti):
                own = ti >= npre; tok0 = ti * TT
                for c in range(NCH):
                    cs = slice(c * 64, (c + 1) * 64)
                    act_sigmoid(beta[:], beta, ba[:, c, 0:8], [ba])
                    yield
                    op("dve", lambda h: h.tensor_tensor(out=gstep[:], in0=ba[:, c, 8:16], in1=dtb[:], op=ALU.add), R=[ba, dtb], W=[gstep])
                    yield
                    op("act", lambda h: h.activation(out=gstep[:], in_=gstep[:], func=AF.Exp), R=[gstep], W=[gstep])
                    yield
                    op("act", lambda h: h.activation(out=gstep[:], in_=gstep[:], func=AF.Ln, bias=1.0), R=[gstep], W=[gstep])
                    yield
                    op("dve", lambda h: h.tensor_tensor(out=gstep[:], in0=gstep[:], in1=alog[:], op=ALU.mult), R=[gstep, alog], W=[gstep])
                    yield
                    op("dve", lambda h: h.tensor_tensor(out=Rm[:], in0=maskgt8[:], in1=gstep[:].unsqueeze(2).to_broadcast([64, 8, 64]), op=ALU.mult), R=[maskgt8, gstep], W=[Rm])
                    yield
                    S.group("pe", [lambda h: h.matmul(pX[0:64, :], lhsT=ut[:], rhs=Rm[:].rearrange("p a b -> p (a b)"), start=True, stop=False),
                                   lambda h: h.matmul(pX[0:64, :], lhsT=ident_f[0:64, 0:64], rhs=neg8[:].rearrange("p a b -> p (a b)"), start=False, stop=True),
                                   lambda h: h.matmul(pV[0:64, 0:8], lhsT=ut[:], rhs=gstep[:], start=True, stop=True),
                                   lambda h: h.matmul(pV[:, 8:16], lhsT=ones_f[:], rhs=gstep[:], start=True, stop=True)],
                            R=[ut, Rm, ident_f, neg8, gstep, ones_f], W=[pX, pV])
                    yield
                    op("act", lambda h: h.activation(out=E[:].rearrange("p a b -> p (a b)"), in_=pX[0:64, :], func=AF.Exp), R=[pX], W=[E])
                    yield
                    op("act", lambda h: h.activation(out=Gam[:], in_=pV[0:64, 0:8], func=AF.Exp), R=[pV], W=[Gam])
                    yield
                    op("act", lambda h: h.activation(out=geT[:], in_=pV[:, 8:16], func=AF.Exp), R=[pV], W=[geT])
                    yield
                    op("dve", lambda h: h.tensor_copy(out=gsb[:], in_=pV[0:64, 0:16]), R=[pV], W=[gsb])
                    yield
                    op("dve", lambda h: h.tensor_tensor(out=dec[:], in0=gsb[:, 8:16], in1=gsb[:, 0:8], op=ALU.subtract), R=[gsb], W=[dec])
                    yield
                    op("act", lambda h: h.activation(out=dec[:], in_=dec[:], func=AF.Exp), R=[dec], W=[dec])
                    yield
                    op("dve", lambda h: h.tensor_tensor(out=bG[:], in0=beta[:], in1=Gam[:], op=ALU.mult), R=[beta, Gam], W=[bG])
                    yield
                    op("dve", lambda h: h.tensor_tensor(out=Es[:], in0=E[:], in1=strict8[:], op=ALU.mult), R=[E, strict8], W=[Es])
                    yield
                    op("pool", lambda h: h.tensor_tensor(out=bv[:], in0=vtok[:, c, :, :], in1=beta[:].unsqueeze(2).to_broadcast([64, 8, 128]), op=ALU.mult), R=[vtok, beta], W=[bv])
                    yield
                    op("pool", lambda h: h.tensor_tensor(out=bgk[:], in0=ktok[:, c, :, :], in1=bG[:].unsqueeze(2).to_broadcast([64, 8, 128]), op=ALU.mult), R=[ktok, bG], W=[bgk])
                    yield
                    op("pool", lambda h: h.tensor_tensor(out=kdec[:], in0=ktok[:, c, :, :], in1=dec[:].unsqueeze(2).to_broadcast([64, 8, 128]), op=ALU.mult), R=[ktok, dec], W=[kdec])
                    yield
                    op("pool", lambda h: h.tensor_tensor(out=Sf[:], in0=Sf[:], in1=geT[:].unsqueeze(2).to_broadcast([128, 8, 128]), op=ALU.mult), R=[Sf, geT], W=[Sf])
                    yield
                    S.group("pe", [lambda h, hh=hh: h.matmul(pK[0:64, hh * 64:(hh + 1) * 64], lhsT=kT[:, hh, cs], rhs=kT[:, hh, cs], start=True, stop=True) for hh in range(8)], R=[kT], W=[pK])
                    yield
                    op("dve", lambda h: h.tensor_tensor(out=tA[:].rearrange("p a b -> p (a b)"), in0=pK[0:64, :], in1=Es[:].rearrange("p a b -> p (a b)"), op=ALU.mult), R=[pK, Es], W=[tA])
                    yield
                    op("dve", lambda h: h.tensor_tensor(out=Ak[0][:], in0=tA[:], in1=beta[:].unsqueeze(2).to_broadcast([64, 8, 64]), op=ALU.mult), R=[tA, beta], W=[Ak[0]])
                    yield
                    if own:
                        S.group("pe", [lambda h, hh=hh: h.matmul(pK[0:64, hh * 64:(hh + 1) * 64], lhsT=qT[:, hh, cs], rhs=kT[:, hh, cs], start=True, stop=True) for hh in range(8)], R=[qT, kT], W=[pK])
                        yield
                        op("dve", lambda h: h.tensor_tensor(out=qk[:].rearrange("p a b -> p (a b)"), in0=pK[0:64, :], in1=E[:].rearrange("p a b -> p (a b)"), op=ALU.mult), R=[pK, E], W=[qk])
                        yield
                    S.group("pe", [lambda h, hh=hh: h.transpose(out=pCh[0:64, hh * 64:(hh + 1) * 64], in_=Ak[0][:, hh, :], identity=ident_f[0:64, 0:64]) for hh in range(8)], R=[Ak[0], ident_f], W=[pCh])
                    yield
                    op("act", lambda h: h.activation(out=Bk[0][:].rearrange("p a b -> p (a b)"), in_=pCh[0:64, :], func=AF.Copy), R=[pCh], W=[Bk[0]])
                    yield
                    if own:
                        S.group("pe", [lambda h, hh=hh: h.transpose(out=pX[0:64, 0:256].bitcast(BF16)[:, hh * 64:(hh + 1) * 64], in_=qk[:, hh, :], identity=ident_b[0:64, 0:64]) for hh in range(8)], R=[qk, ident_b], W=[pX])
                        yield
                        op("act", lambda h: h.activation(out=qkT_[:].rearrange("p a b -> p (a b)"), in_=pX[0:64, 0:256].bitcast(BF16), func=AF.Copy), R=[pX], W=[qkT_])
                        yield
                    op("dve", lambda h: h.tensor_tensor(out=Mk[0][:], in0=i8f[:], in1=Bk[0][:], op=ALU.subtract), R=[i8f, Bk[0]], W=[Mk[0]])
                    yield
                    cur = 0
                    for lvl in range(1, 6):
                        a0, b0, m0 = Ak[cur], Bk[cur], Mk[cur]
                        a1, b1, m1 = Ak[1 - cur], Bk[1 - cur], Mk[1 - cur]
                        S.group("pe", [lambda h, hh=hh: h.matmul(pCh[0:64, hh * 64:(hh + 1) * 64], lhsT=b0[:, hh, :], rhs=a0[:, hh, :], start=True, stop=True) for hh in range(8)], R=[a0, b0], W=[pCh])
                        yield
                        op("act", lambda h: h.activation(out=a1[:].rearrange("p a b -> p (a b)"), in_=pCh[0:64, :], func=AF.Copy), R=[pCh], W=[a1])
                        yield
                        if lvl < 5:
                            S.group("pe", [lambda h, hh=hh: h.matmul(pK[0:64, hh * 64:(hh + 1) * 64], lhsT=a0[:, hh, :], rhs=b0[:, hh, :], start=True, stop=True) for hh in range(8)], R=[a0, b0], W=[pK])
                            yield
                            op("dve", lambda h: h.tensor_copy(out=b1[:].rearrange("p a b -> p (a b)"), in_=pK[0:64, :]), R=[pK], W=[b1])
                            yield
                        S.group("pe", [lambda h, hh=hh: h.matmul(pX[0:64, hh * 64:(hh + 1) * 64], lhsT=a1[:, hh, :], rhs=m0[:, hh, :], start=True, stop=True) for hh in range(8)], R=[a1, m0], W=[pX])
                        yield
                        mo = Mb if lvl == 5 else m1
                        op("dve", lambda h: h.tensor_tensor(out=mo[:].rearrange("p a b -> p (a b)"), in0=pX[0:64, :], in1=m0[:].rearrange("p a b -> p (a b)"), op=ALU.add), R=[pX, m0], W=[mo])
                        yield
                        cur = 1 - cur
                    M = Mb
                    S.group("pe", [lambda h, hh=hh: h.matmul(pX[:, hh * 64:(hh + 1) * 64], lhsT=bgk[:, hh, :], rhs=M[:, hh, :], start=True, stop=True) for hh in range(8)], R=[bgk, M], W=[pX])
                    yield
                    op("act", lambda h: h.activation(out=wTn[:].rearrange("p a b -> p (a b)"), in_=pX[:], func=AF.Copy, scale=-1.0), R=[pX], W=[wTn])
                    yield
                    if own:
                        op("dve", lambda h: h.tensor_tensor(out=dG[:], in0=i8f[:], in1=Gam[:].unsqueeze(2).to_broadcast([64, 8, 64]), op=ALU.mult), R=[i8f, Gam], W=[dG])
                        yield
                        op("pe", lambda h: h.matmul(pK[:], lhsT=ones_f[:], rhs=dG[:].rearrange("p a b -> p (a b)"), start=True, stop=True), R=[ones_f, dG], W=[pK])
                        yield
                        op("dve", lambda h: h.tensor_tensor(out=qg[:], in0=qT[:, :, cs], in1=pK[:].rearrange("p (a b) -> p a b", a=8), op=ALU.mult), R=[qT, pK], W=[qg])
                        yield
                    fns = []
                    for hh in range(8):
                        fns.append(lambda h, hh=hh: h.matmul(pV[0:64, hh * 128:(hh + 1) * 128], lhsT=M[:, hh, :], rhs=bv[:, hh, :], start=True, stop=False))
                        fns.append(lambda h, hh=hh: h.matmul(pV[0:64, hh * 128:(hh + 1) * 128], lhsT=wTn[:, hh, :], rhs=Sb[:, hh, :], start=False, stop=True))
                    S.group("pe", fns, R=[M, bv, wTn, Sb], W=[pV])
                    yield
                    op("act", lambda h: h.activation(out=vnew[:, 0:4, :].rearrange("p a b -> p (a b)"), in_=pV[0:64, 0:512], func=AF.Copy), R=[pV], W=[vnew])
                    yield
                    op("dve", lambda h: h.tensor_copy(out=vnew[:, 4:8, :].rearrange("p a b -> p (a b)"), in_=pV[0:64, 512:1024]), R=[pV], W=[vnew])
                    yield
                    if own:
                        fns = []
                        for hh in range(8):
                            fns.append(lambda h, hh=hh: h.matmul(pV[0:64, hh * 128:(hh + 1) * 128], lhsT=qg[:, hh, :], rhs=Sb[:, hh, :], start=True, stop=False))
                            fns.append(lambda h, hh=hh: h.matmul(pV[0:64, hh * 128:(hh + 1) * 128], lhsT=qkT_[:, hh, :], rhs=vnew[:, hh, :], start=False, stop=True))
                        S.group("pe", fns, R=[qg, Sb, qkT_, vnew], W=[pV])
                        yield
                        op("act", lambda h: h.activation(out=osb[:, 0:4, :].rearrange("p a b -> p (a b)"), in_=pV[0:64, 0:512], func=AF.Copy), R=[pV], W=[osb])
                        yield
                        op("dve", lambda h: h.tensor_copy(out=osb[:, 4:8, :].rearrange("p a b -> p (a b)"), in_=pV[0:64, 512:1024]), R=[pV], W=[osb])
                        yield
                        op("pool", lambda h: h.tensor_tensor(out=osq[:], in0=osb[:], in1=osb[:], op=ALU.mult), R=[osb], W=[osq])
                        yield
                        op("dve", lambda h: h.reduce_sum(out=oss[:], in_=osq[:], axis=AX.X), R=[osq], W=[oss])
                        yield
                        op("dve", lambda h: h.tensor_scalar(out=oss[:], in0=oss[:], scalar1=1.0 / 128, scalar2=EPS, op0=ALU.mult, op1=ALU.add), R=[oss], W=[oss])
                        yield
                        act_rsqrt(oss[:], oss)
                        yield
                        op("dve", lambda h: h.tensor_tensor(out=osb[:], in0=osb[:], in1=oss[:].unsqueeze(2).to_broadcast([64, 8, 128]), op=ALU.mult), R=[osb, oss], W=[osb])
                        yield
                        yd = ydst[st_i[0] % 2]; ssm = so[2 + st_i[0] % 2]; st_i[0] += 1
                        op("dve", lambda h: h.tensor_tensor(out=yd[:], in0=osb[:].rearrange("p a b -> p (a b)"), in1=zw[:, c, :], op=ALU.mult), R=[osb, zw], W=[yd])
                        yield
                        r0 = tok0 - NP + c * 64
                        dma(ydn_d[r0:r0 + 64, :], yd[:], ssm, R=[yd])
                        yield
                    S.group("pe", [lambda h, hh=hh: h.matmul(pV[:, hh * 128:(hh + 1) * 128], lhsT=kdec[:, hh, :], rhs=vnew[:, hh, :], start=True, stop=True) for hh in range(8)], R=[kdec, vnew], W=[pV])
                    yield
                    op("dve", lambda h: h.tensor_tensor(out=Sf[:, 0:4, :].rearrange("p a b -> p (a b)"), in0=Sf[:, 0:4, :].rearrange("p a b -> p (a b)"), in1=pV[:, 0:512], op=ALU.add), R=[Sf, pV], W=[Sf])
                    yield
                    op("dve", lambda h: h.tensor_tensor(out=Sf[:, 4:8, :].rearrange("p a b -> p (a b)"), in0=Sf[:, 4:8, :].rearrange("p a b -> p (a b)"), in1=pV[:, 512:1024], op=ALU.add), R=[Sf, pV], W=[Sf])
                    yield
                    op("act", lambda h: h.activation(out=Sb[:], in_=Sf[:], func=AF.Copy), R=[Sf], W=[Sb])
                    yield

            def emit_flag_dn():
                op("dve", lambda h: h.tensor_scalar_mul(out=carry[:, 8:32, :], in0=carry[:, 8:32, :], scalar1=flag[:, 0:1]), R=[carry, flag], W=[carry])
                op("dve", lambda h: h.tensor_scalar_mul(out=Sf[:], in0=Sf[:], scalar1=flag[:, 0:1]), R=[Sf, flag], W=[Sf])
                op("act", lambda h: h.activation(out=Sb[:], in_=Sf[:], func=AF.Copy), R=[Sf], W=[Sb])

            def interleave(gens):
                gens = list(gens)
                while gens:
                    for g_ in list(gens):
                        try:
                            next(g_)
                        except StopIteration:
                            gens.remove(g_)

            def chain(*gs):
                for g_ in gs:
                    yield from g_

            def gen_prep(ti):
                emit_prep(ti)
                yield

            emit_prep(0)
            interleave([gen_rg(0)])
            for ti in range(ntile):
                emit_dnt(ti)
                gs = [gen_ch(ti)]
                if ti + 1 < ntile:
                    gs.append(chain(gen_prep(ti + 1), gen_rg(ti + 1)))
                interleave(gs)
                if ti == npre - 1:
                    emit_flag_dn()
            S.barrier()

        NSUB = NO // 128
        wte = sb(st, "wte", [128, NSUB, NE])
        if phases >= 2:
          with ExitStack() as s2:
            wbrg = sb(s2, "wbrg", [128, 8, D], BF16); wbdn = sb(s2, "wbdn", [128, 8, D], BF16); wout = sb(s2, "wout", [128, 8, D], BF16)
            wgt = sb(s2, "wgt", [128, 8, 2048], BF16); wr = sb(s2, "wr", [128, 8, 36], BF16); br = sb(s2, "br", [128, 36])
            stgsem = S.newsem("stg")
            with ExitStack() as s2a:
                stg = sb(s2a, "stg", [128, 8, D]); wrf = sb(s2a, "wrf", [128, 8, 36])
                dma(wrf[:], w_r36.rearrange("(kc p) n -> p kc n", p=128), ld, W=[wrf])
                dma(br[:], b_r36_d, ld, W=[br])
                dma(wgt[:], w_in_bf[:, OFF_GRG:OFF_GRG + 2048].rearrange("(kc p) n -> p kc n", p=128), ld, W=[wgt])
                S.barrier()
                op("dve", lambda h: h.tensor_copy(out=wr[:], in_=wrf[:]), R=[wrf], W=[wr])
                for i_, (dst, src) in enumerate(((wbrg, w_brg), (wbdn, w_bdn), (wout, w_out))):
                    dma(stg[:], src.rearrange("(kc p) n -> p kc n", p=128), stgsem, W=[stg])
                    op(("dve", "pool", "dve")[i_], lambda h: h.tensor_copy(out=dst[:], in_=stg[:]), R=[stg], W=[dst])
                S.barrier()
            T2 = 512
            xt4 = sb(s2, "xt4", [128, 4, D]); x4sem = S.newsem("x4")
            junk = sb(s2, "junk2", [128, D], BF16); ss = sb(s2, "ss2", [128, 1]); xn = sb(s2, "xn2", [128, D], BF16); tmpf = sb(s2, "tmpf2", [128, 8, 128])
            hT = sb(s2, "hT2", [128, 8, T2], BF16); sgr = sb(s2, "sgr", [128, 8, T2], BF16); sgd = sb(s2, "sgd", [128, 8, T2], BF16)
            yrgT = sb(s2, "yrgT2", [128, 8, T2], BF16); ysem = S.newsem("yr2")
            ydn = sb(s2, "ydn2", [128, 4, D], BF16); ydsem = S.newsem("yd2"); ydnT = sb(s2, "ydnT", [128, 8, T2], BF16)
            mg = sb(s2, "mg", [128, 8, T2], BF16); m1 = sb(s2, "m1", [128, T2]); m2 = sb(s2, "m2", [128, T2])
            x2 = [sb(s2, "x2_%d" % i, [128, D]) for i in range(2)]; x2sem = [S.newsem("x2s%d" % i) for i in range(2)]
            h2T = sb(s2, "h2T", [128, 8, T2], BF16); h2sem = S.newsem("h2s")
            lg = sb(s2, "lg", [128, 36]); gmx = sb(s2, "gmx", [128, 1]); ngm = sb(s2, "ngm", [128, 1]); gex = sb(s2, "gex", [128, 4]); gsum = sb(s2, "gsum", [128, 1])
            oh = sb(s2, "oh", [128, 4]); elm = sb(s2, "elm", [128, 4, 8]); m8 = sb(s2, "m8", [128, 8]); dd = sb(s2, "dd", [128, 1]); w12 = sb(s2, "w12", [128, 2])
            mm1 = sb(s2, "mm1", [128, 32]); mm2 = sb(s2, "mm2", [128, 32])
            pT = ps(s2, "pT2", [128, 1024], BF16); pP = [ps(s2, "pP%d" % i, [128, T2]) for i in range(2)]
            pO = ps(s2, "pO", [128, D]); pR = ps(s2, "pR", [128, 64])
            pp_i = [0]
            for ti in range(NO // T2):
                c0 = ti * T2
                dma(xt4[:], xo[c0:c0 + T2, :].rearrange("(s p) d -> p s d", p=128), x4sem, W=[xt4])
                dma(yrgT[:], yrgT_d[:, c0:c0 + T2].rearrange("(c p) n -> p c n", p=128), ysem, W=[yrgT])
                dma(ydn[:], ydn_d[c0:c0 + T2, :].rearrange("(s p) d -> p s d", p=128), ydsem, W=[ydn])
                for sub in range(4):
                    xs = TV3(xt4, sub)
                    norm_to_hT(xs, junk, ss, xn, pT, hT, sub * 128, A1, cols[:, 0, :], tmpf)
                for sub in range(4):
                    S.group("pe", [lambda h, kc=kc: h.transpose(out=pT[:, kc * 128:(kc + 1) * 128], in_=ydn[:, sub, kc * 128:(kc + 1) * 128], identity=ident_b[:]) for kc in range(8)], R=[ydn, ident_b], W=[pT])
                    op("act", lambda h: h.activation(out=ydnT[:, :, sub * 128:(sub + 1) * 128], in_=pT[:].rearrange("p (a b) -> p a b", a=8), func=AF.Copy), R=[pT], W=[ydnT])
                for gi, sg in ((0, sgr), (1, sgd)):
                    for oc in range(8):
                        p = pP[pp_i[0] % 2]; pp_i[0] += 1
                        S.group("pe", [lambda h, kc=kc: h.matmul(p[:], lhsT=wgt[:, kc, gi * 1024 + oc * 128:gi * 1024 + (oc + 1) * 128], rhs=hT[:, kc, :], start=(kc == 0), stop=(kc == 7)) for kc in range(8)], R=[wgt, hT], W=[p])
                        op("act", lambda h: h.activation(out=sg[:, oc, :], in_=p[:], func=AF.Sigmoid), R=[p], W=[sg])
                for oc in range(8):
                    p = pP[pp_i[0] % 2]; pp_i[0] += 1
                    S.group("pe", [lambda h, kc=kc: h.matmul(p[:], lhsT=wbrg[:, kc, oc * 128:(oc + 1) * 128], rhs=yrgT[:, kc, :], start=(kc == 0), stop=(kc == 7)) for kc in range(8)], R=[wbrg, yrgT], W=[p])
                    op("dve", lambda h: h.tensor_tensor(out=m1[:], in0=p[:], in1=sgr[:, oc, :], op=ALU.mult), R=[p, sgr], W=[m1])
                    p = pP[pp_i[0] % 2]; pp_i[0] += 1
                    S.group("pe", [lambda h, kc=kc: h.matmul(p[:], lhsT=wbdn[:, kc, oc * 128:(oc + 1) * 128], rhs=ydnT[:, kc, :], start=(kc == 0), stop=(kc == 7)) for kc in range(8)], R=[wbdn, ydnT], W=[p])
                    op("dve", lambda h: h.tensor_tensor(out=m2[:], in0=p[:], in1=sgd[:, oc, :], op=ALU.mult), R=[p, sgd], W=[m2])
                    op("pool", lambda h: h.tensor_tensor(out=mg[:, oc, :], in0=m1[:], in1=m2[:], op=ALU.add), R=[m1, m2], W=[mg])
                for sub in range(4):
                    r0 = c0 + sub * 128
                    fns = []
                    for hf in range(2):
                        fns += [lambda h, kc=kc, hf=hf: h.matmul(pO[:, hf * 512:(hf + 1) * 512], lhsT=mg[:, kc, sub * 128:(sub + 1) * 128], rhs=wout[:, kc, hf * 512:(hf + 1) * 512], start=(kc == 0), stop=(kc == 7)) for kc in range(8)]
                    S.group("pe", fns, R=[mg, wout], W=[pO])
                    xx = x2[sub % 2]
                    for hf in range(2):
                        op("dve", lambda h: h.tensor_tensor(out=xx[:, hf * 512:(hf + 1) * 512], in0=pO[:, hf * 512:(hf + 1) * 512], in1=gate1[:, hf * 512:(hf + 1) * 512], op=ALU.mult), R=[pO, gate1], W=[xx])
                    op("pool", lambda h: h.tensor_tensor(out=xx[:], in0=xx[:], in1=xt4[:, sub, :], op=ALU.add), R=[xx, xt4], W=[xx])
                    dma(x2_d[r0:r0 + 128, :], xx[:], x2sem[sub % 2], R=[xx])
                    norm_to_hT(xx, junk, ss, xn, pT, h2T, sub * 128, A2, cols[:, 2, :], tmpf)
                    S.group("pe", [lambda h, kc=kc: h.matmul(pR[:, 0:36], lhsT=h2T[:, kc, sub * 128:(sub + 1) * 128], rhs=wr[:, kc, :], start=(kc == 0), stop=(kc == 7)) for kc in range(8)], R=[h2T, wr], W=[pR])
                    op("dve", lambda h: h.tensor_tensor(out=lg[:], in0=pR[:, 0:36], in1=br[:], op=ALU.add), R=[pR, br], W=[lg])
                    op("dve", lambda h: h.reduce_max(out=gmx[:], in_=lg[:, 0:4], axis=AX.X), R=[lg], W=[gmx])
                    op("dve", lambda h: h.tensor_scalar_mul(out=ngm[:], in0=gmx[:], scalar1=-1.0), R=[gmx], W=[ngm])
                    op("act", lambda h: h.activation(out=gex[:], in_=lg[:, 0:4], func=AF.Exp, bias=ngm[:], accum_out=gsum[:]), R=[lg, ngm], W=[gex, gsum])
                    op("dve", lambda h: h.reciprocal(out=gsum[:], in_=gsum[:]), R=[gsum], W=[gsum])
                    op("dve", lambda h: h.tensor_scalar(out=oh[:], in0=lg[:, 0:4], scalar1=gmx[:], scalar2=1.0e9, op0=ALU.is_equal, op1=ALU.mult), R=[lg, gmx], W=[oh])
                    op("dve", lambda h: h.tensor_scalar_add(out=oh[:], in0=oh[:], scalar1=-1.0e9), R=[oh], W=[oh])
                    op("dve", lambda h: h.tensor_tensor(out=elm[:], in0=lg[:, 4:36].rearrange("p (a b) -> p a b", a=4), in1=oh[:].unsqueeze(2).to_broadcast([128, 4, 8]), op=ALU.add), R=[lg, oh], W=[elm])
                    op("dve", lambda h: h.max(out=m8[:], in_=elm[:].rearrange("p a b -> p (a b)")), R=[elm], W=[m8])
                    op("dve", lambda h: h.tensor_tensor(out=dd[:], in0=m8[:, 1:2], in1=m8[:, 0:1], op=ALU.subtract), R=[m8], W=[dd])
                    op("act", lambda h: h.activation(out=dd[:], in_=dd[:], func=AF.Exp), R=[dd], W=[dd])
                    op("dve", lambda h: h.tensor_scalar_add(out=w12[:, 0:1], in0=dd[:], scalar1=1.0), R=[dd], W=[w12])
                    op("dve", lambda h: h.reciprocal(out=w12[:, 0:1], in_=w12[:, 0:1]), R=[w12], W=[w12])
                    op("dve", lambda h: h.tensor_tensor(out=w12[:, 0:1], in0=w12[:, 0:1], in1=gsum[:], op=ALU.mult), R=[w12, gsum], W=[w12])
                    op("dve", lambda h: h.tensor_tensor(out=w12[:, 1:2], in0=w12[:, 0:1], in1=dd[:], op=ALU.mult), R=[w12, dd], W=[w12])
                    op("dve", lambda h: h.tensor_scalar(out=mm1[:], in0=elm[:].rearrange("p a b -> p (a b)"), scalar1=m8[:, 0:1], scalar2=w12[:, 0:1], op0=ALU.is_equal, op1=ALU.mult), R=[elm, m8, w12], W=[mm1])
                    op("dve", lambda h: h.tensor_scalar(out=mm2[:], in0=elm[:].rearrange("p a b -> p (a b)"), scalar1=m8[:, 1:2], scalar2=w12[:, 1:2], op0=ALU.is_equal, op1=ALU.mult), R=[elm, m8, w12], W=[mm2])
                    op("dve", lambda h: h.tensor_tensor(out=wte[:, ti * 4 + sub, :], in0=mm1[:], in1=mm2[:], op=ALU.add), R=[mm1, mm2], W=[wte])
                dma(h2T_d[:, c0:c0 + T2].rearrange("(c p) n -> p c n", p=128), h2T[:], h2sem, R=[h2T])
            S.barrier()

        if phases >= 3:
          with ExitStack() as s3:
            Q = min(1024, NO); NQS = Q // 128
            h2q = sb(s3, "h2q", [128, 8, Q], BF16); hqsem = S.newsem("hq")
            acc = sb(s3, "acc", [128, NQS, D])
            wgf = sb(s3, "wgf", [128, 8, DE]); wuf = sb(s3, "wuf", [128, 8, DE]); wdf = sb(s3, "wdf", [128, 4, D])
            fsem = [S.newsem("mf%d" % i) for i in range(3)]
            wgb = [sb(s3, "wgb%d" % i, [128, 8, DE], BF16) for i in range(2)]; wub = [sb(s3, "wub%d" % i, [128, 8, DE], BF16) for i in range(2)]
            wdb = [sb(s3, "wdb%d" % i, [128, 4, D], BF16) for i in range(2)]
            sgt = sb(s3, "sgt", [128, 512]); AT = sb(s3, "AT", [128, 4, 512], BF16)
            xf = [sb(s3, "xf%d" % i, [128, D]) for i in range(2)]; xfsem = [S.newsem("xf%d" % i) for i in range(2)]
            ss3 = sb(s3, "ss3", [128, 1]); junk3 = sb(s3, "junk3", [128, D], BF16)
            ob = [sb(s3, "ob%d" % i, [128, D]) for i in range(2)]; osem = [S.newsem("ob%d" % i) for i in range(2)]
            pG = [ps(s3, "pG%d" % i, [128, 512]) for i in range(2)]; pU = [ps(s3, "pU%d" % i, [128, 512]) for i in range(2)]
            pY = [ps(s3, "pY%d" % i, [128, 512]) for i in range(2)]
            gi_ = [0]; yi_ = [0]
            for qi in range(NO // Q):
                q0 = qi * Q
                dma(h2q[:], h2T_d[:, q0:q0 + Q].rearrange("(c p) n -> p c n", p=128), hqsem, W=[h2q])
                op("pool", lambda h: h.memset(acc[:], 0.0), W=[acc])
                for e in range(NE):
                    b = e % 2
                    dma(wgf[:], moe_wg[e].rearrange("(kc p) n -> p kc n", p=128), fsem[0], W=[wgf])
                    dma(wuf[:], moe_wu[e].rearrange("(kc p) n -> p kc n", p=128), fsem[1], W=[wuf])
                    dma(wdf[:], moe_wd[e].rearrange("(kc p) n -> p kc n", p=128), fsem[2], W=[wdf])
                    op("act", lambda h: h.activation(out=wgb[b][:], in_=wgf[:], func=AF.Copy), R=[wgf], W=[wgb[b]])
                    op("act", lambda h: h.activation(out=wub[b][:], in_=wuf[:], func=AF.Copy), R=[wuf], W=[wub[b]])
                    op("pool", lambda h: h.tensor_copy(out=wdb[b][:, 0:2, :], in_=wdf[:, 0:2, :]), R=[wdf], W=[wdb[b]])
                    op("dve", lambda h: h.tensor_copy(out=wdb[b][:, 2:4, :], in_=wdf[:, 2:4, :]), R=[wdf], W=[wdb[b]])
                    for hf in range(Q // 512):
                        ts_ = slice(hf * 512, (hf + 1) * 512)
                        for oc in range(4):
                            g_ = pG[gi_[0] % 2]; u_ = pU[gi_[0] % 2]; gi_[0] += 1
                            S.group("pe", [lambda h, kc=kc: h.matmul(g_[:], lhsT=wgb[b][:, kc, oc * 128:(oc + 1) * 128], rhs=h2q[:, kc, ts_], start=(kc == 0), stop=(kc == 7)) for kc in range(8)], R=[wgb[b], h2q], W=[g_])
                            S.group("pe", [lambda h, kc=kc: h.matmul(u_[:], lhsT=wub[b][:, kc, oc * 128:(oc + 1) * 128], rhs=h2q[:, kc, ts_], start=(kc == 0), stop=(kc == 7)) for kc in range(8)], R=[wub[b], h2q], W=[u_])
                            op("act", lambda h: h.activation(out=sgt[:], in_=g_[:], func=AF.Silu), R=[g_], W=[sgt])
                            op("dve", lambda h: h.tensor_tensor(out=AT[:, oc, :], in0=u_[:], in1=sgt[:], op=ALU.mult), R=[u_, sgt], W=[AT])
                        for sub in range(4):
                            si = hf * 4 + sub
                            for ch in range(2):
                                y_ = pY[yi_[0] % 2]; yi_[0] += 1
                                S.group("pe", [lambda h, kc=kc: h.matmul(y_[:], lhsT=AT[:, kc, sub * 128:(sub + 1) * 128], rhs=wdb[b][:, kc, ch * 512:(ch + 1) * 512], start=(kc == 0), stop=(kc == 3)) for kc in range(4)], R=[AT, wdb[b]], W=[y_])
                                gs = qi * NQS + si
                                op("dve", lambda h: h.scalar_tensor_tensor(out=acc[:, si, ch * 512:(ch + 1) * 512], in0=y_[:], scalar=wte[:, gs, e:e + 1], in1=acc[:, si, ch * 512:(ch + 1) * 512], op0=ALU.mult, op1=ALU.add), R=[y_, wte, acc], W=[acc])
                for si in range(NQS):
                    r0 = q0 + si * 128
                    x_ = xf[si % 2]; o_ = ob[si % 2]
                    dma(x_[:], x2_d[r0:r0 + 128, :], xfsem[si % 2], W=[x_])
                    op("pool", lambda h: h.tensor_tensor(out=acc[:, si, :], in0=acc[:, si, :], in1=gate2[:], op=ALU.mult), R=[acc, gate2], W=[acc])
                    op("dve", lambda h: h.tensor_tensor(out=x_[:], in0=x_[:], in1=acc[:, si, :], op=ALU.add), R=[x_, acc], W=[x_])
                    op("act", lambda h: h.activation(out=junk3[:], in_=x_[:], func=AF.Square, scale=1.0 / 32, accum_out=ss3[:]), R=[x_], W=[junk3, ss3])
                    op("dve", lambda h: h.tensor_scalar_add(out=ss3[:], in0=ss3[:], scalar1=EPS), R=[ss3], W=[ss3])
                    op("act", lambda h: h.activation(out=ss3[:], in_=ss3[:], func=AF.Sqrt), R=[ss3], W=[ss3])
                    op("dve", lambda h: h.reciprocal(out=ss3[:], in_=ss3[:]), R=[ss3], W=[ss3])
                    op("dve", lambda h: h.scalar_tensor_tensor(out=o_[:], in0=x_[:], scalar=ss3[:, 0:1], in1=fnw[:], op0=ALU.mult, op1=ALU.mult), R=[x_, ss3, fnw], W=[o_])
                    dma(out_d[r0:r0 + 128, :], o_[:], osem[si % 2], R=[o_])
            S.barrier()

        if dbg and phases == 1:
            with ExitStack() as sd:
                a = sb(sd, "dba", [128, NO], BF16); b = sb(sd, "dbb", [128, D], BF16)
                for ch in range(8):
                    dma(a[:], yrgT_d[ch * 128:(ch + 1) * 128, :], ld, W=[a]); dma(dbg_out["d_yrgT"][ch * 128:(ch + 1) * 128, :], a[:], ld, R=[a])
                for r in range(NO // 128):
                    dma(b[:], ydn_d[r * 128:(r + 1) * 128, :], ld, W=[b]); dma(dbg_out["d_ydn"][r * 128:(r + 1) * 128, :], b[:], ld, R=[b])
                S.barrier()
        if dbg and phases >= 2:
            with ExitStack() as sd:
                b = sb(sd, "dbc", [128, D]); dsa = S.newsem("dsa"); dsb = S.newsem("dsb"); dsc = S.newsem("dsc")
                for r in range(NO // 128):
                    dma(b[:], x2_d[r * 128:(r + 1) * 128, :], dsa, W=[b]); dma(dbg_out["d_x2"][r * 128:(r + 1) * 128, :], b[:], dsb, R=[b])
                    dma(dbg_out["d_wte"][r * 128:(r + 1) * 128, :], wte[:, r, :], dsc, R=[wte])
                S.barrier()
        S.barrier()
    return nc


def _col(v, n=8):
    return np.ascontiguousarray(np.asarray(v, np.float32).reshape(n, 128).T)


def _rep(v, p=128):
    v = np.asarray(v, np.float32).reshape(1, -1)
    return np.ascontiguousarray(np.repeat(v, p, axis=0))


def shared_inputs(I):
    f = lambda a: np.ascontiguousarray(np.asarray(a, np.float32))
    d = {}
    d["w_ada"] = f(I["w_ada"][0]); d["b_ada_rep"] = _rep(I["b_ada"][0])
    d["n1w_col"] = _col(I["norm1_w"][0]); d["n2w_col"] = _col(I["norm2_w"][0]); d["fnw_rep"] = _rep(I["final_norm_w"])
    d["w_in"] = f(I["w_in"][0])
    d["rgcw"] = np.ascontiguousarray(f(I["rg_conv_w"][0]).reshape(4, 8, 128).transpose(2, 1, 0))
    d["rgcb"] = _col(I["rg_conv_b"][0])
    d["dncw"] = np.ascontiguousarray(f(I["dn_conv_w"][0]).reshape(4, 24, 128).transpose(2, 1, 0))
    ga = f(I["rg_gate_a_w"][0]).reshape(4, 2, 128, 256); gx = f(I["rg_gate_x_w"][0]).reshape(4, 2, 128, 256)
    d["rga_w"] = np.ascontiguousarray(ga.transpose(2, 0, 1, 3).reshape(128, 8, 256))
    d["rgx_w"] = np.ascontiguousarray(gx.transpose(2, 0, 1, 3).reshape(128, 8, 256))
    d["rga_b"] = _col(f(I["rg_gate_a_b"][0]).reshape(-1)); d["rgx_b"] = _col(f(I["rg_gate_x_b"][0]).reshape(-1))
    d["lam"] = _col(I["rg_lambda"][0])
    d["alog_rep"] = _rep(I["dn_a_log"][0], 64); d["dtb_rep"] = _rep(I["dn_dt_bias"][0], 64)
    d["dnw_rep"] = _rep(np.tile(f(I["dn_norm_w"][0]), 8), 64)
    d["w_brg"] = f(I["w_branch_rg"][0]); d["w_bdn"] = f(I["w_branch_dn"][0]); d["w_out"] = f(I["w_out"][0])
    d["w_r36"] = np.ascontiguousarray(np.concatenate([f(I["moe_w_group"][0]), f(I["moe_w_router"][0])], axis=1))
    d["b_r36_rep"] = _rep(np.concatenate([f(I["moe_b_group"][0]), f(I["moe_b_router"][0])]))
    d["moe_wg"] = f(I["moe_w_gate"][0]); d["moe_wu"] = f(I["moe_w_up"][0]); d["moe_wd"] = f(I["moe_w_down"][0])
    i = np.arange(64)
    d["c_ident"] = np.eye(128, dtype=np.float32)
    d["c_ut"] = (i[:, None] <= i[None, :]).astype(np.float32)
    d["c_maskgt"] = (i[:, None] > i[None, :]).astype(np.float32)
    d["c_neg"] = np.where(i[None, :] > i[:, None], -30000.0, 0.0).astype(np.float32)
    d["c_strict"] = (i[:, None] > i[None, :]).astype(np.float32)
    return d


def core_inputs(I, shared, b, half, NP, NO):
    x = np.asarray(I["x"], np.float32)
    d = dict(shared)
    own = x[b, half * NO:(half + 1) * NO]
    d["xo"] = np.ascontiguousarray(own)
    d["xp"] = np.ascontiguousarray(x[b, 0:NP]) if half == 1 else np.ascontiguousarray(own[0:NP])
    d["flag"] = np.full((128, 1), float(half), np.float32)
    d["ccol"] = _col(np.asarray(I["c"], np.float32)[b])
    return d


def kernel(**inputs):
    NP = NO = 4096
    nc = build(NP, NO)
    sh = shared_inputs(inputs)
    in_maps = [core_inputs(inputs, sh, b, half, NP, NO) for b in range(4) for half in range(2)]
    res = run_bass_kernel_spmd(nc, in_maps, core_ids=list(range(8)))
    out = np.empty((4, 2 * NO, D), np.float32)
    for i, r in enumerate(res.results):
        b, half = divmod(i, 2)
        out[b, half * NO:(half + 1) * NO] = np.asarray(r["out"], np.float32)
    return out
```

```python
import numpy as np
from contextlib import ExitStack
import concourse.bass as bass
import concourse.mybir as mybir
from concourse.bass_utils import run_bass_kernel_spmd

F32 = mybir.dt.float32
BF16 = mybir.dt.bfloat16
AF = mybir.ActivationFunctionType
ALU = mybir.AluOpType
AX = mybir.AxisListType

D = 1024
D_IN = 8208
OFF_RGX, OFF_RGY, OFF_Q, OFF_K, OFF_V, OFF_Z, OFF_BA, OFF_GRG, OFF_GDN = 0, 1024, 2048, 3072, 4096, 5120, 6144, 6160, 7184
NE = 32
DE = 512
EPS = 1e-6
TT = 256
CH = 64
NCH = TT // CH


class Buf:
    __slots__ = ("w", "r")

    def __init__(self):
        self.w = None
        self.r = {}


class T:
    def __init__(self, t):
        self.t = t
        self.b = Buf()

    def __getitem__(self, k):
        return self.t[k]


class TV:
    def __init__(self, t, lo, hi):
        self.t = t; self.lo = lo; self.hi = hi
        self.b = Buf()

    def __getitem__(self, k):
        assert k == slice(None)
        return self.t[:, self.lo:self.hi]


class TV3:
    def __init__(self, parent, sub):
        self.p = parent; self.sub = sub
        self.b = parent.b

    def __getitem__(self, k):
        assert k == slice(None)
        return self.p.t[:, self.sub, :]


class SemCounter:
    def __init__(self, nc, stack, name):
        self.h = stack.enter_context(nc.semaphore(name))
        self.n = 0


class Sync:
    def __init__(self, nc, stack):
        self.nc = nc
        self.eng = {"pe": nc.tensor, "act": nc.scalar, "dve": nc.vector, "pool": nc.gpsimd, "sp": nc.sync}
        self.sem = {k: stack.enter_context(nc.semaphore("s_" + k)) for k in ("pe", "act", "dve", "pool")}
        self.cnt = {k: 0 for k in self.sem}
        self.seen = {k: {} for k in self.eng}
        self.dsems = []
        self.stack = stack

    def newsem(self, name):
        s = SemCounter(self.nc, self.stack, name)
        self.dsems.append(s)
        return s

    def _need(self, e, tok, waits):
        if tok is None:
            return
        k, v = tok
        if k == e and e == "pe":
            return
        if self.seen[e].get(k, 0) < v:
            waits[k] = max(waits.get(k, 0), v)

    def _waits(self, e, reads, writes):
        waits = {}
        for b in reads:
            self._need(e, b.b.w, waits)
        for b in writes:
            self._need(e, b.b.w, waits)
            for k, v in b.b.r.items():
                self._need(e, (k, v), waits)
        h = self.eng[e]
        for k, v in waits.items():
            h.wait_ge(self.sem[k] if isinstance(k, str) else k, v)
            self.seen[e][k] = v
        return h

    def op(self, e, fn, R=(), W=()):
        h = self._waits(e, R, W)
        ins = fn(h)
        self.cnt[e] += 1
        ins.then_inc(self.sem[e], 1)
        tok = (e, self.cnt[e])
        for b in R:
            b.b.r[e] = self.cnt[e]
        for b in W:
            b.b.w = tok
            b.b.r = {}
        return tok

    def group(self, e, fns, R=(), W=()):
        h = self._waits(e, R, W)
        ins = None
        for fn in fns:
            ins = fn(h)
        self.cnt[e] += 1
        ins.then_inc(self.sem[e], 1)
        tok = (e, self.cnt[e])
        for b in R:
            b.b.r[e] = self.cnt[e]
        for b in W:
            b.b.w = tok
            b.b.r = {}
        return tok

    def dma(self, out, in_, sem, R=(), W=(), q="sp", **kw):
        h = self._waits(q, R, W)
        sem.n += 16
        h.dma_start(out=out, in_=in_, **kw).then_inc(sem.h, 16)
        tok = (sem.h, sem.n)
        for b in R:
            b.b.r[sem.h] = sem.n
        for b in W:
            b.b.w = tok
            b.b.r = {}
        return tok

    def barrier(self):
        for e, h in self.eng.items():
            for k in self.sem:
                if not (k == e and e == "pe") and self.seen[e].get(k, 0) < self.cnt[k]:
                    h.wait_ge(self.sem[k], self.cnt[k])
                    self.seen[e][k] = self.cnt[k]
            for s in self.dsems:
                if s.n and self.seen[e].get(s.h, 0) < s.n:
                    h.wait_ge(s.h, s.n)
                    self.seen[e][s.h] = s.n


def build(NP, NO, phases=3, dbg=False, cut=99):
    assert NP % TT == 0 and NO % TT == 0
    nc = bass.Bass("TRN2", target_bir_lowering=False)

    def din(name, shape, dt=F32):
        return nc.dram_tensor(name, list(shape), dt, kind="ExternalInput").ap()

    def dscr(name, shape, dt):
        return nc.dram_tensor(name, list(shape), dt, kind="Internal").ap()

    xp = din("xp", [NP, D]); xo = din("xo", [NO, D]); flag_d = din("flag", [128, 1]); ccol_d = din("ccol", [128, 8])
    w_ada = din("w_ada", [D, 6 * D]); b_ada_rep = din("b_ada_rep", [128, 6 * D])
    n1w_d = din("n1w_col", [128, 8]); n2w_d = din("n2w_col", [128, 8]); fnw_d = din("fnw_rep", [128, D])
    w_in = din("w_in", [D, D_IN])
    rgcw_d = din("rgcw", [128, 8, 4]); rgcb_d = din("rgcb", [128, 8]); dncw_d = din("dncw", [128, 24, 4])
    rgaw_d = din("rga_w", [128, 8, 256]); rgxw_d = din("rgx_w", [128, 8, 256])
    rgab_d = din("rga_b", [128, 8]); rgxb_d = din("rgx_b", [128, 8]); lam_d = din("lam", [128, 8])
    alog_d = din("alog_rep", [64, 8]); dtb_d = din("dtb_rep", [64, 8]); dnw_d = din("dnw_rep", [64, D])
    w_brg = din("w_brg", [D, D]); w_bdn = din("w_bdn", [D, D]); w_out = din("w_out", [D, D])
    w_r36 = din("w_r36", [D, 36]); b_r36_d = din("b_r36_rep", [128, 36])
    moe_wg = din("moe_wg", [NE, D, DE]); moe_wu = din("moe_wu", [NE, D, DE]); moe_wd = din("moe_wd", [NE, DE, D])
    c_ident = din("c_ident", [128, 128]); c_ut = din("c_ut", [64, 64]); c_maskgt = din("c_maskgt", [64, 64])
    c_neg = din("c_neg", [64, 64]); c_strict = din("c_strict", [64, 64])
    out_d = nc.dram_tensor("out", [NO, D], F32, kind="ExternalOutput").ap()
    w_in_bf = dscr("w_in_bf", [D, D_IN], BF16)
    yrgT_d = dscr("yrgT", [D, NO], BF16)
    ydn_d = dscr("ydn", [NO, D], BF16)
    x2_d = dscr("x2", [NO, D], F32)
    h2T_d = dscr("h2T", [D, NO], BF16)
    wte_d = dscr("wte", [NO, NE], F32)
    g12_d = dscr("g12", [2, 128, D], F32)
    dbg_out = {}
    if dbg:
        dbg_out["d_yrgT"] = nc.dram_tensor("d_yrgT", [D, NO], BF16, kind="ExternalOutput").ap()
        dbg_out["d_ydn"] = nc.dram_tensor("d_ydn", [NO, D], BF16, kind="ExternalOutput").ap()
        dbg_out["d_x2"] = nc.dram_tensor("d_x2", [NO, D], F32, kind="ExternalOutput").ap()
        dbg_out["d_wte"] = nc.dram_tensor("d_wte", [NO, NE], F32, kind="ExternalOutput").ap()

    with ExitStack() as st:
        S = Sync(nc, st)
        op, dma = S.op, S.dma

        def act_sigmoid(out_ap, outT, in_ap, inR, scale=1.0, nbias=None, nbR=()):
            if nbias is not None:
                op("act", lambda h: h.activation(out=out_ap, in_=in_ap, func=AF.Exp, scale=-scale, bias=nbias), R=list(inR) + list(nbR), W=[outT])
            else:
                op("act", lambda h: h.activation(out=out_ap, in_=in_ap, func=AF.Exp, scale=-scale), R=list(inR), W=[outT])
            op("act", lambda h: h.activation(out=out_ap, in_=out_ap, func=AF.Ln, bias=1.0), R=[outT], W=[outT])
            op("act", lambda h: h.activation(out=out_ap, in_=out_ap, func=AF.Exp, scale=-1.0), R=[outT], W=[outT])

        def act_rsqrt(out_ap, outT):
            op("act", lambda h: h.activation(out=out_ap, in_=out_ap, func=AF.Ln), R=[outT], W=[outT])
            op("act", lambda h: h.activation(out=out_ap, in_=out_ap, func=AF.Exp, scale=-0.5), R=[outT], W=[outT])

        def sb(stk, name, shape, dt=F32):
            return T(stk.enter_context(nc.sbuf_tensor("s_" + name, list(shape), dt)))

        def ps(stk, name, shape, dt=F32):
            return T(stk.enter_context(nc.psum_tensor("p_" + name, list(shape), dt)))

        st.enter_context(nc.Block())
        ld = S.newsem("ld")
        so = [S.newsem("so%d" % i) for i in range(4)]

        ident_f = sb(st, "ident_f", [128, 128]); ident_b = sb(st, "ident_b", [128, 128], BF16)
        ones_b = sb(st, "ones_b", [128, 128], BF16); ones_f = sb(st, "ones_f", [64, 128])
        flag = sb(st, "flag", [128, 1])
        cols = sb(st, "cols", [128, 4, 8])
        A1 = sb(st, "A1", [128, 8]); A2 = sb(st, "A2", [128, 8]); n1w = sb(st, "n1w", [128, 8]); n2w = sb(st, "n2w", [128, 8])
        gsem = S.newsem("gsem")
        for (t_, d_) in ((ident_f, c_ident), (flag, flag_d), (n1w, n1w_d), (n2w, n2w_d)):
            dma(t_[:], d_, ld, W=[t_])
        S.barrier()
        op("dve", lambda h: h.tensor_copy(out=ident_b[:], in_=ident_f[:]), R=[ident_f], W=[ident_b])
        op("pool", lambda h: h.memset(ones_b[:], 1.0), W=[ones_b])
        op("pool", lambda h: h.memset(ones_f[:], 1.0), W=[ones_f])

        with ExitStack() as s0:
            ccol = sb(s0, "ccol", [128, 8]); crep = sb(s0, "crep", [128, 8, 128])
            wsl = [sb(s0, "wsl%d" % i, [128, 8, 512]) for i in range(2)]
            wsem = [S.newsem("wsl%d" % i) for i in range(2)]
            bsl = sb(s0, "bsl", [128, 512]); mtmp = sb(s0, "mtmp", [128, 4, 128]); mt2 = sb(s0, "mt2", [128, 4, 128])
            pm = ps(s0, "pm", [128, 512])
            bsem = S.newsem("bsl")
            dma(ccol[:], ccol_d, ld, W=[ccol])
            S.barrier()
            op("act", lambda h: h.activation(out=ccol[:], in_=ccol[:], func=AF.Silu), R=[ccol], W=[ccol])
            op("dve", lambda h: h.tensor_copy(out=crep[:], in_=ccol[:].unsqueeze(2).to_broadcast([128, 8, 128])), R=[ccol], W=[crep])
            for s in range(12):
                w_ = wsl[s % 2]
                dma(w_[:], w_ada[:, s * 512:(s + 1) * 512].rearrange("(kc p) n -> p kc n", p=128), wsem[s % 2], W=[w_])
                dma(bsl[:], b_ada_rep[:, s * 512:(s + 1) * 512], bsem, W=[bsl])
                S.group("pe", [lambda h, kc=kc: h.matmul(pm[:], lhsT=crep[:, kc, :], rhs=w_[:, kc, :], start=(kc == 0), stop=(kc == 7)) for kc in range(8)], R=[crep, w_], W=[pm])
                v, half = s // 2, s % 2
                if v in (2, 5):
                    op("dve", lambda h: h.tensor_tensor(out=mtmp[:].rearrange("p a b -> p (a b)"), in0=pm[:], in1=bsl[:], op=ALU.add), R=[pm, bsl], W=[mtmp])
                    dma(g12_d[0 if v == 2 else 1][:, half * 512:(half + 1) * 512], mtmp[:].rearrange("p a b -> p (a b)"), gsem, R=[mtmp])
                else:
                    ci = {0: 0, 1: 1, 3: 2, 4: 3}[v]
                    op("dve", lambda h: h.tensor_tensor(out=mtmp[:].rearrange("p a b -> p (a b)"), in0=pm[:], in1=bsl[:], op=ALU.add), R=[pm, bsl], W=[mtmp])
                    op("pool", lambda h: h.tensor_tensor(out=mt2[:], in0=mtmp[:], in1=ident_f[:].unsqueeze(1).to_broadcast([128, 4, 128]), op=ALU.mult), R=[mtmp, ident_f], W=[mt2])
                    op("dve", lambda h: h.reduce_sum(out=cols[:, ci, half * 4:(half + 1) * 4], in_=mt2[:], axis=AX.X), R=[mt2], W=[cols])
            op("dve", lambda h: h.scalar_tensor_tensor(out=A1[:], in0=cols[:, 1, :], scalar=1.0, in1=n1w[:], op0=ALU.add, op1=ALU.mult), R=[cols, n1w], W=[A1])
            op("dve", lambda h: h.scalar_tensor_tensor(out=A2[:], in0=cols[:, 3, :], scalar=1.0, in1=n2w[:], op0=ALU.add, op1=ALU.mult), R=[cols, n2w], W=[A2])
            S.barrier()

        with ExitStack() as s0:
          if cut >= 2:
            wf = [sb(s0, "wf%d" % i, [128, 8, 256]) for i in range(4)]
            wb = [sb(s0, "wb%d" % i, [128, 8, 256], BF16) for i in range(4)]
            lsem = [S.newsem("wfl%d" % i) for i in range(4)]
            nsl = (D_IN + 255) // 256
            for s in range(nsl):
                c0 = s * 256; cw = min(256, D_IN - c0)
                f_, b_ = wf[s % 4], wb[s % 4]
                dma(f_[:, :, 0:cw], w_in[:, c0:c0 + cw].rearrange("(kc p) n -> p kc n", p=128), lsem[s % 4], W=[f_])
                op(("dve", "pool", "dve", "act")[s % 4], (lambda h: h.activation(out=b_[:, :, 0:cw], in_=f_[:, :, 0:cw], func=AF.Copy)) if s % 4 == 3 else (lambda h: h.tensor_copy(out=b_[:, :, 0:cw], in_=f_[:, :, 0:cw])), R=[f_], W=[b_])
                dma(w_in_bf[:, c0:c0 + cw].rearrange("(kc p) n -> p kc n", p=128), b_[:, :, 0:cw], so[s % 4], R=[b_], q=("sp" if s % 4 == 3 else "act"))
            S.barrier()

        def norm_to_hT(xt, junk, ss, xn, pT, hT, col0, Acol, Bcol_ap, tmpf):
            op("act", lambda h: h.activation(out=junk[:], in_=xt[:], func=AF.Square, scale=1.0 / 32, accum_out=ss[:]), R=[xt], W=[junk, ss])
            op("dve", lambda h: h.tensor_scalar_add(out=ss[:], in0=ss[:], scalar1=EPS), R=[ss], W=[ss])
            act_rsqrt(ss[:], ss)
            op("dve", lambda h: h.tensor_scalar_mul(out=xn[:], in0=xt[:], scalar1=ss[:]), R=[xt, ss], W=[xn])
            S.group("pe", [lambda h, kc=kc: h.transpose(out=pT[:, kc * 128:(kc + 1) * 128], in_=xn[:, kc * 128:(kc + 1) * 128], identity=ident_b[:]) for kc in range(8)], R=[xn, ident_b], W=[pT])
            op("dve", lambda h: h.tensor_tensor(out=tmpf[:], in0=pT[:].rearrange("p (a b) -> p a b", a=8), in1=Acol[:].unsqueeze(2).to_broadcast([128, 8, 128]), op=ALU.mult), R=[pT, Acol], W=[tmpf])
            op("pool", lambda h: h.tensor_tensor(out=hT[:, :, col0:col0 + 128], in0=tmpf[:], in1=Bcol_ap.unsqueeze(2).to_broadcast([128, 8, 128]), op=ALU.add), R=[tmpf, cols], W=[hT])

        with ExitStack() as s1:
            rgcw = sb(s1, "rgcw", [128, 8, 4]); rgcb = sb(s1, "rgcb", [128, 8]); dncw = sb(s1, "dncw", [128, 24, 4])
            rgab = sb(s1, "rgab", [128, 8]); rgxb = sb(s1, "rgxb", [128, 8]); lamc = sb(s1, "lamc", [128, 8])
            rgaw = sb(s1, "rgaw", [128, 8, 256], BF16); rgxw = sb(s1, "rgxw", [128, 8, 256], BF16)
            alog = sb(s1, "alog", [64, 8]); dtb = sb(s1, "dtb", [64, 8]); dnw = sb(s1, "dnw", [64, D])
            ut = sb(s1, "ut", [64, 64]); maskgt = sb(s1, "maskgt", [64, 64]); negm = sb(s1, "negm", [64, 64]); strict = sb(s1, "strict", [64, 64])
            for (t_, d_) in ((rgcw, rgcw_d), (rgcb, rgcb_d), (dncw, dncw_d), (rgab, rgab_d), (rgxb, rgxb_d), (lamc, lam_d),
                             (alog, alog_d), (dtb, dtb_d), (dnw, dnw_d), (ut, c_ut), (maskgt, c_maskgt), (negm, c_neg), (strict, c_strict)):
                dma(t_[:], d_, ld, W=[t_])
            with ExitStack() as s1a:
                rgw_f = sb(s1a, "rgw_f", [128, 8, 256]); rgw_f2 = sb(s1a, "rgw_f2", [128, 8, 256])
                dma(rgw_f[:], rgaw_d, ld, W=[rgw_f])
                dma(rgw_f2[:], rgxw_d, ld, W=[rgw_f2])
                S.barrier()
                op("dve", lambda h: h.tensor_copy(out=rgaw[:], in_=rgw_f[:]), R=[rgw_f], W=[rgaw])
                op("dve", lambda h: h.tensor_copy(out=rgxw[:], in_=rgw_f2[:]), R=[rgw_f2], W=[rgxw])
                S.barrier()
            nrgab = sb(s1, "nrgab", [128, 8]); nrgxb = sb(s1, "nrgxb", [128, 8])
            op("dve", lambda h: h.tensor_scalar_mul(out=nrgab[:], in0=rgab[:], scalar1=-1.0), R=[rgab], W=[nrgab])
            op("dve", lambda h: h.tensor_scalar_mul(out=nrgxb[:], in0=rgxb[:], scalar1=-1.0), R=[rgxb], W=[nrgxb])
            op("act", lambda h: h.activation(out=lamc[:], in_=lamc[:], func=AF.Exp, scale=-1.0), R=[lamc], W=[lamc])
            op("act", lambda h: h.activation(out=lamc[:], in_=lamc[:], func=AF.Ln, bias=1.0), R=[lamc], W=[lamc])
            op("dve", lambda h: h.tensor_scalar_mul(out=lamc[:], in0=lamc[:], scalar1=-8.0), R=[lamc], W=[lamc])
            op("act", lambda h: h.activation(out=alog[:], in_=alog[:], func=AF.Exp), R=[alog], W=[alog])
            op("dve", lambda h: h.tensor_scalar_mul(out=alog[:], in0=alog[:], scalar1=-1.0), R=[alog], W=[alog])
            i8f = sb(s1, "i8f", [64, 8, 64])
            strict8 = sb(s1, "strict8", [64, 8, 64]); maskgt8 = strict8; neg8 = sb(s1, "neg8", [64, 8, 64])
            op("dve", lambda h: h.tensor_copy(out=i8f[:], in_=ident_f[0:64, 0:64].unsqueeze(1).to_broadcast([64, 8, 64])), R=[ident_f], W=[i8f])
            op("dve", lambda h: h.tensor_copy(out=strict8[:], in_=strict[:].unsqueeze(1).to_broadcast([64, 8, 64])), R=[strict], W=[strict8])
            op("dve", lambda h: h.tensor_copy(out=neg8[:], in_=negm[:].unsqueeze(1).to_broadcast([64, 8, 64])), R=[negm], W=[neg8])

            carry = sb(s1, "carry", [128, 32, 3]); hst = sb(s1, "hst", [128, 8])
            Sf = sb(s1, "Sf", [128, 8, 128]); Sb = sb(s1, "Sb", [128, 8, 128], BF16)
            for t_ in (carry, hst, Sf, Sb):
                op("pool", lambda h: h.memset(t_[:], 0.0), W=[t_])

            xt = [sb(s1, "xt%d" % i, [128, D]) for i in range(2)]; xsem = [S.newsem("xs%d" % i) for i in range(2)]
            junk = sb(s1, "junk", [128, D], BF16); ss = sb(s1, "ss", [128, 1]); xn = sb(s1, "xn", [128, D], BF16)
            tmpf = sb(s1, "tmpf", [128, 8, 128])
            hT = sb(s1, "hT", [128, 8, TT], BF16); hT_b = sb(s1, "hT_b", [128, 8, TT], BF16)
            NSLAB = 4
            slabs = [sb(s1, "slab%d" % i, [128, 8, 128], BF16) for i in range(NSLAB)]; slsem = [S.newsem("sl%d" % i) for i in range(NSLAB)]
            pre_r = sb(s1, "pre", [128, TT + 3]); preD = [sb(s1, "preD%d" % i, [128, TT + 3]) for i in range(2)]; d1 = [sb(s1, "d1_%d" % i, [128, TT]) for i in range(2)]; d2 = [sb(s1, "d2_%d" % i, [128, TT]) for i in range(2)]; d3 = [sb(s1, "d3_%d" % i, [128, TT]) for i in range(2)]; sqbD = [sb(s1, "sqbD%d" % i, [128, TT], BF16) for i in range(2)]; vTD = [sb(s1, "vTD%d" % i, [128, TT], BF16) for i in range(2)]; xc = sb(s1, "xc", [128, 2, TT]); xcb = sb(s1, "xcb", [128, 2, TT], BF16)
            t1 = sb(s1, "t1", [128, TT]); t2 = sb(s1, "t2", [128, TT]); t3 = sb(s1, "t3", [128, TT]); t4 = sb(s1, "t4", [128, TT])
            sqb = sb(s1, "sqb", [128, TT], BF16)
            yst = [sb(s1, "yst%d" % i, [128, TT], BF16) for i in range(2)]
            qTs = [sb(s1, "qT%d" % i, [128, 8, TT], BF16) for i in range(2)]; kTs = [sb(s1, "kT%d" % i, [128, 8, TT], BF16) for i in range(2)]
            vtoks = [sb(s1, "vtok%d" % i, [64, NCH, 8, 128], BF16) for i in range(2)]; ktoks = [sb(s1, "ktok%d" % i, [64, NCH, 8, 128], BF16) for i in range(2)]
            zws = [sb(s1, "zw%d" % i, [64, NCH, D], BF16) for i in range(2)]; ztmp = sb(s1, "ztmp", [64, NCH, 128])
            bas = [sb(s1, "ba%d" % i, [64, NCH, 16]) for i in range(2)]
            beta = sb(s1, "beta", [64, 8]); gstep = sb(s1, "gstep", [64, 8]); gsb = sb(s1, "gsb", [64, 16]); Gam = sb(s1, "Gam", [64, 8])
            dec = sb(s1, "dec", [64, 8]); bG = sb(s1, "bG", [64, 8]); geT = sb(s1, "geT", [128, 8])
            Rm = sb(s1, "Rm", [64, 8, 64]); E = sb(s1, "E", [64, 8, 64]); Es = sb(s1, "Es", [64, 8, 64]); tA = sb(s1, "tA", [64, 8, 64])
            Ak = [sb(s1, "Ak%d" % i, [64, 8, 64]) for i in range(2)]; Bk = [sb(s1, "Bk%d" % i, [64, 8, 64]) for i in range(2)]
            Mk = [sb(s1, "Mk%d" % i, [64, 8, 64]) for i in range(2)]; Mb = sb(s1, "Mb", [64, 8, 64], BF16)
            qk = sb(s1, "qk", [64, 8, 64], BF16); qkT_ = sb(s1, "qkT", [64, 8, 64], BF16)
            bv = sb(s1, "bv", [64, 8, 128], BF16); bgk = sb(s1, "bgk", [64, 8, 128], BF16); kdec = sb(s1, "kdec", [64, 8, 128], BF16)
            wTn = sb(s1, "wTn", [128, 8, 64], BF16); dG = sb(s1, "dG", [64, 8, 64]); qg = sb(s1, "qg", [128, 8, 64], BF16)
            vnew = sb(s1, "vnew", [64, 8, 128], BF16); osb = sb(s1, "osb", [64, 8, 128]); osq = sb(s1, "osq", [64, 8, 128])
            oss = sb(s1, "oss", [64, 8]); ydst = [sb(s1, "ydst%d" % i, [64, D], BF16) for i in range(2)]
            pA = [ps(s1, "pA%d" % i, [128, TT]) for i in range(2)]
            pT = ps(s1, "pT", [128, 1024], BF16)
            pX = ps(s1, "pX", [128, 512]); pK = ps(s1, "pK", [128, 512]); pCh = ps(s1, "pCh", [128, 512]); pV = ps(s1, "pV", [128, 1024])
            S.barrier()

            ntile = (NP + NO) // TT
            npre = NP // TT

            def rg_slabs(own):
                l = []
                for g in range(4):
                    l += [OFF_RGX + (2 * g) * 128, OFF_RGX + (2 * g + 1) * 128]
                    if own:
                        l += [OFF_RGY + (2 * g) * 128, OFF_RGY + (2 * g + 1) * 128]
                return l

            def dnt_slabs(own):
                l = []
                for pp in range(4):
                    for o_ in (OFF_Q, OFF_K, OFF_V):
                        l += [o_ + (2 * pp) * 128, o_ + (2 * pp + 1) * 128]
                l += [OFF_BA]
                if own:
                    l += [OFF_Z + j * 128 for j in range(8)]
                return l

            sched = []
            for ti in range(ntile):
                sched += rg_slabs(ti >= npre) + dnt_slabs(ti >= npre)
            state = {"issued": 0, "used": 0}

            def issue_slab():
                i = state["issued"]
                if i >= len(sched):
                    return
                c0 = sched[i]; cw = min(128, D_IN - c0)
                sl = slabs[i % NSLAB]
                dma(sl[:, :, 0:cw], w_in_bf[:, c0:c0 + cw].rearrange("(kc p) n -> p kc n", p=128), slsem[i % NSLAB], W=[sl])
                state["issued"] += 1

            def next_slab(expect=None):
                assert expect is None or sched[state["used"]] == expect, (expect, sched[state["used"]], state["used"])
                while state["issued"] < min(len(sched), state["used"] + NSLAB - 1) or state["issued"] <= state["used"]:
                    issue_slab()
                sl = slabs[state["used"] % NSLAB]
                state["used"] += 1
                return sl

            pa_i = [0]

            def proj_fm(sl, hT, ptile=None):
                if ptile is not None:
                    p = ptile
                else:
                    p = pA[pa_i[0] % 2]; pa_i[0] += 1
                S.group("pe", [lambda h, kc=kc: h.matmul(p[:], lhsT=sl[:, kc, :], rhs=hT[:, kc, :], start=(kc == 0), stop=(kc == 7)) for kc in range(8)], R=[sl, hT], W=[p])
                return p

            def conv(p, cidx, wcol, bias_ap, dst_ap, dstT, pre=None):
                pre = pre if pre is not None else pre_r
                op("pool", lambda h: h.tensor_copy(out=pre[:, 0:3], in_=carry[:, cidx, :]), R=[carry], W=[pre])
                op("act", lambda h: h.activation(out=pre[:, 3:3 + TT], in_=p[:], func=AF.Copy), R=[p], W=[pre])
                if bias_ap is not None:
                    op("act", lambda h: h.activation(out=dst_ap, in_=p[:], func=AF.Identity, scale=wcol[:, 3:4], bias=bias_ap), R=[p], W=[dstT])
                else:
                    op("act", lambda h: h.activation(out=dst_ap, in_=p[:], func=AF.Identity, scale=wcol[:, 3:4]), R=[p], W=[dstT])
                op("pool", lambda h: h.tensor_copy(out=carry[:, cidx, :], in_=pre[:, TT:TT + 3]), R=[pre], W=[carry])
                for j in range(0, 3):
                    op("dve", lambda h: h.scalar_tensor_tensor(out=dst_ap, in0=pre[:, j:j + TT], scalar=wcol[:, j:j + 1], in1=dst_ap, op0=ALU.mult, op1=ALU.add), R=[pre, dstT], W=[dstT])

            st_i = [0]

            hTs = [hT, hT_b]

            def emit_prep(ti):
                hTt = hTs[ti % 2]
                own = ti >= npre
                tok0 = ti * TT
                for sub in range(TT // 128):
                    r0 = tok0 + sub * 128
                    src = xp[r0:r0 + 128, :] if r0 < NP else xo[r0 - NP:r0 - NP + 128, :]
                    xi = (ti * (TT // 128) + sub) % 2
                    dma(xt[xi][:], src, xsem[xi], W=[xt[xi]])
                    norm_to_hT(xt[xi], junk, ss, xn, pT, hTt, sub * 128, A1, cols[:, 0, :], tmpf)

            def gen_rg(ti):
                own = ti >= npre; tok0 = ti * TT; hTt = hTs[ti % 2]
                if ti == npre:
                    op("dve", lambda h: h.tensor_scalar_mul(out=carry[:, 0:8, :], in0=carry[:, 0:8, :], scalar1=flag[:, 0:1]), R=[carry, flag], W=[carry])
                    op("dve", lambda h: h.tensor_scalar_mul(out=hst[:], in0=hst[:], scalar1=flag[:, 0:1]), R=[hst, flag], W=[hst])
                for g in range(4):
                    for jc in range(2):
                        ch = 2 * g + jc
                        p = proj_fm(next_slab(), hTt)
                        yield
                        conv(p, ch, rgcw[:, ch, :], rgcb[:, ch:ch + 1], xc[:, jc, :], xc)
                        yield
                    ypp = []
                    if own:
                        for jc in range(2):
                            ypp.append(next_slab())
                    op("pool", lambda h: h.tensor_copy(out=xcb[:], in_=xc[:]), R=[xc], W=[xcb])
                    yield
                    for oc in range(2):
                        ch = 2 * g + oc
                        p = pA[pa_i[0] % 2]; pa_i[0] += 1
                        S.group("pe", [lambda h, ic=ic: h.matmul(p[:], lhsT=rgaw[:, 2 * g + ic, oc * 128:(oc + 1) * 128], rhs=xcb[:, ic, :], start=(ic == 0), stop=(ic == 1)) for ic in range(2)], R=[rgaw, xcb], W=[p])
                        yield
                        act_sigmoid(t1[:], t1, p[:], [p], nbias=nrgab[:, ch:ch + 1], nbR=[nrgab])
                        yield
                        p = pA[pa_i[0] % 2]; pa_i[0] += 1
                        S.group("pe", [lambda h, ic=ic: h.matmul(p[:], lhsT=rgxw[:, 2 * g + ic, oc * 128:(oc + 1) * 128], rhs=xcb[:, ic, :], start=(ic == 0), stop=(ic == 1)) for ic in range(2)], R=[rgxw, xcb], W=[p])
                        yield
                        act_sigmoid(t2[:], t2, p[:], [p], nbias=nrgxb[:, ch:ch + 1], nbR=[nrgxb])
                        yield
                        op("act", lambda h: h.activation(out=t1[:], in_=t1[:], func=AF.Exp, scale=lamc[:, ch:ch + 1]), R=[t1, lamc], W=[t1])
                        yield
                        op("pool", lambda h: h.tensor_tensor(out=t3[:], in0=t1[:], in1=t1[:], op=ALU.mult), R=[t1], W=[t3])
                        yield
                        op("act", lambda h: h.activation(out=t3[:], in_=t3[:], func=AF.Ln, scale=-1.0, bias=1.0), R=[t3], W=[t3])
                        yield
                        op("act", lambda h: h.activation(out=t3[:], in_=t3[:], func=AF.Exp, scale=0.5), R=[t3], W=[t3])
                        yield
                        op("dve", lambda h: h.tensor_tensor(out=t2[:], in0=t2[:], in1=xc[:, oc, :], op=ALU.mult), R=[t2, xc], W=[t2])
                        yield
                        op("dve", lambda h: h.tensor_tensor(out=t2[:], in0=t2[:], in1=t3[:], op=ALU.mult), R=[t2, t3], W=[t2])
                        yield
                        op("dve", lambda h: h.tensor_tensor_scan(out=t4[:], data0=t1[:], data1=t2[:], initial=hst[:, ch:ch + 1], op0=ALU.mult, op1=ALU.add), R=[t1, t2, hst], W=[t4])
                        yield
                        op("pool", lambda h: h.tensor_copy(out=hst[:, ch:ch + 1], in_=t4[:, TT - 1:TT]), R=[t4], W=[hst])
                        yield
                        if own:
                            p = proj_fm(ypp[oc], hTt)
                            yield
                            op("act", lambda h: h.activation(out=t1[:], in_=p[:], func=AF.Square), R=[p], W=[t1])
                            yield
                            op("dve", lambda h: h.tensor_scalar(out=t1[:], in0=t1[:], scalar1=0.044715, scalar2=1.0, op0=ALU.mult, op1=ALU.add), R=[t1], W=[t1])
                            yield
                            op("dve", lambda h: h.tensor_tensor(out=t1[:], in0=t1[:], in1=p[:], op=ALU.mult), R=[t1, p], W=[t1])
                            yield
                            act_sigmoid(t1[:], t1, t1[:], [t1], scale=1.5957691216057308)
                            yield
                            op("dve", lambda h: h.tensor_tensor(out=t1[:], in0=t1[:], in1=p[:], op=ALU.mult), R=[t1, p], W=[t1])
                            yield
                            ys = yst[st_i[0] % 2]; ssm = so[st_i[0] % 2]; st_i[0] += 1
                            op("dve", lambda h: h.tensor_tensor(out=ys[:], in0=t1[:], in1=t4[:], op=ALU.mult), R=[t1, t4], W=[ys])
                            yield
                            c0 = tok0 - NP
                            dma(yrgT_d[ch * 128:(ch + 1) * 128, c0:c0 + TT], ys[:], ssm, R=[ys])
                            yield

            def gen_dnt(ti):
                own = ti >= npre; tok0 = ti * TT; hTt = hTs[ti % 2]
                qT = qTs[ti % 2]; kT = kTs[ti % 2]; vtok = vtoks[ti % 2]; ktok = ktoks[ti % 2]; zw = zws[ti % 2]; ba = bas[ti % 2]
                if ti == npre:
                    op("dve", lambda h: h.tensor_scalar_mul(out=carry[:, 8:32, :], in0=carry[:, 8:32, :], scalar1=flag[:, 0:1]), R=[carry, flag], W=[carry])
                def gen_head(hh, k):
                    t1 = d1[k]; t2 = d2[k]; t3 = d3[k]; sqb = sqbD[k]; vT = vTD[k]
                    for which, dstT in ((0, qT), (1, kT), (2, vT)):
                        cid = which * 8 + hh
                        p = proj_fm(next_slab((OFF_Q, OFF_K, OFF_V)[which] + hh * 128), hTt, ptile=pA[k])
                        yield
                        conv(p, 8 + cid, dncw[:, cid, :], None, t1[:], t1, pre=preD[k])
                        yield
                        if which == 2:
                            act_sigmoid(t2[:], t2, t1[:], [t1])
                            yield
                            op("dve", lambda h: h.tensor_tensor(out=vT[:], in0=t1[:], in1=t2[:], op=ALU.mult), R=[t1, t2], W=[vT])
                            yield
                            S.group("pe", [lambda h, c=c: h.transpose(out=pT[0:64, c * 128:(c + 1) * 128], in_=vT[:, c * 64:(c + 1) * 64], identity=ident_b[:]) for c in range(NCH)], R=[vT, ident_b], W=[pT])
                            op("dve", lambda h: h.tensor_copy(out=vtok[:, :, hh, :], in_=pT[0:64, 0:NCH * 128].rearrange("p (c d) -> p c d", c=NCH)), R=[pT], W=[vtok])
                            yield
                        else:
                            act_sigmoid(t2[:], t2, t1[:], [t1])
                            yield
                            op("dve", lambda h: h.tensor_tensor(out=t2[:], in0=t1[:], in1=t2[:], op=ALU.mult), R=[t1, t2], W=[t2])
                            yield
                            op("pool", lambda h: h.tensor_tensor(out=sqb[:], in0=t2[:], in1=t2[:], op=ALU.mult), R=[t2], W=[sqb])
                            yield
                            pn = pA[k]
                            op("pe", lambda h: h.matmul(pn[:], lhsT=ones_b[:], rhs=sqb[:], start=True, stop=True), R=[ones_b, sqb], W=[pn])
                            yield
                            op("dve", lambda h: h.tensor_scalar_add(out=t3[:], in0=pn[:], scalar1=EPS), R=[pn], W=[t3])
                            yield
                            act_rsqrt(t3[:], t3)
                            yield
                            sc = (128.0 ** -0.5) if which == 0 else 1.0
                            op("dve", lambda h: h.scalar_tensor_tensor(out=dstT[:, hh, :], in0=t2[:], scalar=sc, in1=t3[:], op0=ALU.mult, op1=ALU.mult), R=[t2, t3], W=[dstT])
                            yield
                            if which == 1:
                                S.group("pe", [lambda h, c=c: h.transpose(out=pT[0:64, c * 128:(c + 1) * 128], in_=kT[:, hh, c * 64:(c + 1) * 64], identity=ident_b[:]) for c in range(NCH)], R=[kT, ident_b], W=[pT])
                                op("dve", lambda h: h.tensor_copy(out=ktok[:, :, hh, :], in_=pT[0:64, 0:NCH * 128].rearrange("p (c d) -> p c d", c=NCH)), R=[pT], W=[ktok])
                                yield
                yield from interleave_gen([chain(*[gen_head(hh, 0) for hh in (0, 2, 4, 6)]), chain(*[gen_head(hh, 1) for hh in (1, 3, 5, 7)])])
                sl = next_slab()
                for c in range(NCH):
                    S.group("pe", [lambda h, kc=kc: h.matmul(pA[0][:][0:64, 0:16], lhsT=hTt[:, kc, c * 64:(c + 1) * 64], rhs=sl[:, kc, 0:16], start=(kc == 0), stop=(kc == 7)) for kc in range(8)], R=[hTt, sl], W=[pA[0]])
                    op("act", lambda h: h.activation(out=ba[:, c, :], in_=pA[0][:][0:64, 0:16], func=AF.Copy), R=[pA[0]], W=[ba])
                    yield
                if own:
                    for j in range(8):
                        sl = next_slab()
                        for c in range(NCH):
                            S.group("pe", [lambda h, kc=kc: h.matmul(pT[0:64, :].bitcast(F32)[:, c * 128:(c + 1) * 128], lhsT=hTt[:, kc, c * 64:(c + 1) * 64], rhs=sl[:, kc, :], start=(kc == 0), stop=(kc == 7)) for kc in range(8)], R=[hTt, sl], W=[pT])
                            yield
                        act_sigmoid(ztmp[:], ztmp, pT[0:64, :].bitcast(F32)[:, 0:NCH * 128].rearrange("p (c d) -> p c d", c=NCH), [pT])
                        yield
                        op("dve", lambda h: h.tensor_tensor(out=ztmp[:], in0=ztmp[:], in1=pT[0:64, :].bitcast(F32)[:, 0:NCH * 128].rearrange("p (c d) -> p c d", c=NCH), op=ALU.mult), R=[ztmp, pT], W=[ztmp])
                        yield
                        op("pool", lambda h: h.tensor_tensor(out=zw[:, :, j * 128:(j + 1) * 128], in0=ztmp[:], in1=dnw[:, j * 128:(j + 1) * 128].unsqueeze(1).to_broadcast([64, NCH, 128]), op=ALU.mult), R=[ztmp, dnw], W=[zw])

            def gen_ch(ti):
                own = ti >= npre; tok0 = ti * TT
                qT = qTs[ti % 2]; kT = kTs[ti % 2]; vtok = vtoks[ti % 2]; ktok = ktoks[ti % 2]; zw = zws[ti % 2]; ba = bas[ti % 2]
                if ti == npre:
                    op("dve", lambda h: h.tensor_scalar_mul(out=Sf[:], in0=Sf[:], scalar1=flag[:, 0:1]), R=[Sf, flag], W=[Sf])
                    op("act", lambda h: h.activation(out=Sb[:], in_=Sf[:], func=AF.Copy), R=[Sf], W=[Sb])
                for c in range(NCH):
                    cs = slice(c * 64, (c + 1) * 64)
                    act_sigmoid(beta[:], beta, ba[:, c, 0:8], [ba])
                    yield
                    op("dve", lambda h: h.tensor_tensor(out=gstep[:], in0=ba[:, c, 8:16], in1=dtb[:], op=ALU.add), R=[ba, dtb], W=[gstep])
                    yield
                    op("act", lambda h: h.activation(out=gstep[:], in_=gstep[:], func=AF.Exp), R=[gstep], W=[gstep])
                    yield
                    op("act", lambda h: h.activation(out=gstep[:], in_=gstep[:], func=AF.Ln, bias=1.0), R=[gstep], W=[gstep])
                    yield
                    op("dve", lambda h: h.tensor_tensor(out=gstep[:], in0=gstep[:], in1=alog[:], op=ALU.mult), R=[gstep, alog], W=[gstep])
                    yield
                    op("dve", lambda h: h.tensor_tensor(out=Rm[:], in0=maskgt8[:], in1=gstep[:].unsqueeze(2).to_broadcast([64, 8, 64]), op=ALU.mult), R=[maskgt8, gstep], W=[Rm])
                    yield
                    S.group("pe", [lambda h: h.matmul(pX[0:64, :], lhsT=ut[:], rhs=Rm[:].rearrange("p a b -> p (a b)"), start=True, stop=False),
                                   lambda h: h.matmul(pX[0:64, :], lhsT=ident_f[0:64, 0:64], rhs=neg8[:].rearrange("p a b -> p (a b)"), start=False, stop=True),
                                   lambda h: h.matmul(pV[0:64, 0:8], lhsT=ut[:], rhs=gstep[:], start=True, stop=True),
                                   lambda h: h.matmul(pV[:, 8:16], lhsT=ones_f[:], rhs=gstep[:], start=True, stop=True)],
                            R=[ut, Rm, ident_f, neg8, gstep, ones_f], W=[pX, pV])
                    yield
                    op("act", lambda h: h.activation(out=E[:].rearrange("p a b -> p (a b)"), in_=pX[0:64, :], func=AF.Exp), R=[pX], W=[E])
                    yield
                    op("act", lambda h: h.activation(out=Gam[:], in_=pV[0:64, 0:8], func=AF.Exp), R=[pV], W=[Gam])
                    yield
                    op("act", lambda h: h.activation(out=geT[:], in_=pV[:, 8:16], func=AF.Exp), R=[pV], W=[geT])
                    yield
                    op("dve", lambda h: h.tensor_copy(out=gsb[:], in_=pV[0:64, 0:16]), R=[pV], W=[gsb])
                    yield
                    op("dve", lambda h: h.tensor_tensor(out=dec[:], in0=gsb[:, 8:16], in1=gsb[:, 0:8], op=ALU.subtract), R=[gsb], W=[dec])
                    yield
                    op("act", lambda h: h.activation(out=dec[:], in_=dec[:], func=AF.Exp), R=[dec], W=[dec])
                    yield
                    op("dve", lambda h: h.tensor_tensor(out=bG[:], in0=beta[:], in1=Gam[:], op=ALU.mult), R=[beta, Gam], W=[bG])
                    yield
                    op("dve", lambda h: h.tensor_tensor(out=Es[:], in0=E[:], in1=strict8[:], op=ALU.mult), R=[E, strict8], W=[Es])
                    yield
                    op("pool", lambda h: h.tensor_tensor(out=bv[:], in0=vtok[:, c, :, :], in1=beta[:].unsqueeze(2).to_broadcast([64, 8, 128]), op=ALU.mult), R=[vtok, beta], W=[bv])
                    yield
                    op("pool", lambda h: h.tensor_tensor(out=bgk[:], in0=ktok[:, c, :, :], in1=bG[:].unsqueeze(2).to_broadcast([64, 8, 128]), op=ALU.mult), R=[ktok, bG], W=[bgk])
                    yield
                    op("pool", lambda h: h.tensor_tensor(out=kdec[:], in0=ktok[:, c, :, :], in1=dec[:].unsqueeze(2).to_broadcast([64, 8, 128]), op=ALU.mult), R=[ktok, dec], W=[kdec])
                    yield
                    op("pool", lambda h: h.tensor_tensor(out=Sf[:], in0=Sf[:], in1=geT[:].unsqueeze(2).to_broadcast([128, 8, 128]), op=ALU.mult), R=[Sf, geT], W=[Sf])
                    yield
                    S.group("pe", [lambda h, hh=hh: h.matmul(pK[0:64, hh * 64:(hh + 1) * 64], lhsT=kT[:, hh, cs], rhs=kT[:, hh, cs], start=True, stop=True) for hh in range(8)], R=[kT], W=[pK])
                    yield
                    op("dve", lambda h: h.tensor_tensor(out=tA[:].rearrange("p a b -> p (a b)"), in0=pK[0:64, :], in1=Es[:].rearrange("p a b -> p (a b)"), op=ALU.mult), R=[pK, Es], W=[tA])
                    yield
                    op("dve", lambda h: h.tensor_tensor(out=Ak[0][:], in0=tA[:], in1=beta[:].unsqueeze(2).to_broadcast([64, 8, 64]), op=ALU.mult), R=[tA, beta], W=[Ak[0]])
                    yield
                    if own:
                        S.group("pe", [lambda h, hh=hh: h.matmul(pK[0:64, hh * 64:(hh + 1) * 64], lhsT=qT[:, hh, cs], rhs=kT[:, hh, cs], start=True, stop=True) for hh in range(8)], R=[qT, kT], W=[pK])
                        yield
                        op("dve", lambda h: h.tensor_tensor(out=qk[:].rearrange("p a b -> p (a b)"), in0=pK[0:64, :], in1=E[:].rearrange("p a b -> p (a b)"), op=ALU.mult), R=[pK, E], W=[qk])
                        yield
                    S.group("pe", [lambda h, hh=hh: h.transpose(out=pCh[0:64, hh * 64:(hh + 1) * 64], in_=Ak[0][:, hh, :], identity=ident_f[0:64, 0:64]) for hh in range(8)], R=[Ak[0], ident_f], W=[pCh])
                    yield
                    op("act", lambda h: h.activation(out=Bk[0][:].rearrange("p a b -> p (a b)"), in_=pCh[0:64, :], func=AF.Copy), R=[pCh], W=[Bk[0]])
                    yield
                    if own:
                        S.group("pe", [lambda h, hh=hh: h.transpose(out=pX[0:64, 0:256].bitcast(BF16)[:, hh * 64:(hh + 1) * 64], in_=qk[:, hh, :], identity=ident_b[0:64, 0:64]) for hh in range(8)], R=[qk, ident_b], W=[pX])
                        yield
                        op("act", lambda h: h.activation(out=qkT_[:].rearrange("p a b -> p (a b)"), in_=pX[0:64, 0:256].bitcast(BF16), func=AF.Copy), R=[pX], W=[qkT_])
                        yield
                    op("dve", lambda h: h.tensor_tensor(out=Mk[0][:], in0=i8f[:], in1=Bk[0][:], op=ALU.subtract), R=[i8f, Bk[0]], W=[Mk[0]])
                    yield
                    cur = 0
                    for lvl in range(1, 6):
                        a0, b0, m0 = Ak[cur], Bk[cur], Mk[cur]
                        a1, b1, m1 = Ak[1 - cur], Bk[1 - cur], Mk[1 - cur]
                        S.group("pe", [lambda h, hh=hh: h.matmul(pCh[0:64, hh * 64:(hh + 1) * 64], lhsT=b0[:, hh, :], rhs=a0[:, hh, :], start=True, stop=True) for hh in range(8)], R=[a0, b0], W=[pCh])
                        yield
                        op("act", lambda h: h.activation(out=a1[:].rearrange("p a b -> p (a b)"), in_=pCh[0:64, :], func=AF.Copy), R=[pCh], W=[a1])
                        yield
                        if lvl < 5:
                            S.group("pe", [lambda h, hh=hh: h.matmul(pK[0:64, hh * 64:(hh + 1) * 64], lhsT=a0[:, hh, :], rhs=b0[:, hh, :], start=True, stop=True) for hh in range(8)], R=[a0, b0], W=[pK])
                            yield
                            op("dve", lambda h: h.tensor_copy(out=b1[:].rearrange("p a b -> p (a b)"), in_=pK[0:64, :]), R=[pK], W=[b1])
                            yield
                        S.group("pe", [lambda h, hh=hh: h.matmul(pX[0:64, hh * 64:(hh + 1) * 64], lhsT=a1[:, hh, :], rhs=m0[:, hh, :], start=True, stop=True) for hh in range(8)], R=[a1, m0], W=[pX])
                        yield
                        mo = Mb if lvl == 5 else m1
                        op("dve", lambda h: h.tensor_tensor(out=mo[:].rearrange("p a b -> p (a b)"), in0=pX[0:64, :], in1=m0[:].rearrange("p a b -> p (a b)"), op=ALU.add), R=[pX, m0], W=[mo])
                        yield
                        cur = 1 - cur
                    M = Mb
                    S.group("pe", [lambda h, hh=hh: h.matmul(pX[:, hh * 64:(hh + 1) * 64], lhsT=bgk[:, hh, :], rhs=M[:, hh, :], start=True, stop=True) for hh in range(8)], R=[bgk, M], W=[pX])
                    yield
                    op("act", lambda h: h.activation(out=wTn[:].rearrange("p a b -> p (a b)"), in_=pX[:], func=AF.Copy, scale=-1.0), R=[pX], W=[wTn])
                    yield
                    if own:
                        op("dve", lambda h: h.tensor_tensor(out=dG[:], in0=i8f[:], in1=Gam[:].unsqueeze(2).to_broadcast([64, 8, 64]), op=ALU.mult), R=[i8f, Gam], W=[dG])
                        yield
                        op("pe", lambda h: h.matmul(pK[:], lhsT=ones_f[:], rhs=dG[:].rearrange("p a b -> p (a b)"), start=True, stop=True), R=[ones_f, dG], W=[pK])
                        yield
                        op("dve", lambda h: h.tensor_tensor(out=qg[:], in0=qT[:, :, cs], in1=pK[:].rearrange("p (a b) -> p a b", a=8), op=ALU.mult), R=[qT, pK], W=[qg])
                        yield
                    fns = []
                    for hh in range(8):
                        fns.append(lambda h, hh=hh: h.matmul(pV[0:64, hh * 128:(hh + 1) * 128], lhsT=M[:, hh, :], rhs=bv[:, hh, :], start=True, stop=False))
                        fns.append(lambda h, hh=hh: h.matmul(pV[0:64, hh * 128:(hh + 1) * 128], lhsT=wTn[:, hh, :], rhs=Sb[:, hh, :], start=False, stop=True))
                    S.group("pe", fns, R=[M, bv, wTn, Sb], W=[pV])
                    yield
                    op("act", lambda h: h.activation(out=vnew[:, 0:4, :].rearrange("p a b -> p (a b)"), in_=pV[0:64, 0:512], func=AF.Copy), R=[pV], W=[vnew])
                    yield
                    op("dve", lambda h: h.tensor_copy(out=vnew[:, 4:8, :].rearrange("p a b -> p (a b)"), in_=pV[0:64, 512:1024]), R=[pV], W=[vnew])
                    yield
                    if own:
                        fns = []
                        for hh in range(8):
                            fns.append(lambda h, hh=hh: h.matmul(pV[0:64, hh * 128:(hh + 1) * 128], lhsT=qg[:, hh, :], rhs=Sb[:, hh, :], start=True, stop=False))
                            fns.append(lambda h, hh=hh: h.matmul(pV[0:64, hh * 128:(hh + 1) * 128], lhsT=qkT_[:, hh, :], rhs=vnew[:, hh, :], start=False, stop=True))
                        S.group("pe", fns, R=[qg, Sb, qkT_, vnew], W=[pV])
                        yield
                        op("act", lambda h: h.activation(out=osb[:, 0:4, :].rearrange("p a b -> p (a b)"), in_=pV[0:64, 0:512], func=AF.Copy), R=[pV], W=[osb])
                        yield
                        op("dve", lambda h: h.tensor_copy(out=osb[:, 4:8, :].rearrange("p a b -> p (a b)"), in_=pV[0:64, 512:1024]), R=[pV], W=[osb])
                        yield
                        op("pool", lambda h: h.tensor_tensor(out=osq[:], in0=osb[:], in1=osb[:], op=ALU.mult), R=[osb], W=[osq])
                        yield
                        op("dve", lambda h: h.reduce_sum(out=oss[:], in_=osq[:], axis=AX.X), R=[osq], W=[oss])
                        yield
                        op("dve", lambda h: h.tensor_scalar(out=oss[:], in0=oss[:], scalar1=1.0 / 128, scalar2=EPS, op0=ALU.mult, op1=ALU.add), R=[oss], W=[oss])
                        yield
                        act_rsqrt(oss[:], oss)
                        yield
                        op("dve", lambda h: h.tensor_tensor(out=osb[:], in0=osb[:], in1=oss[:].unsqueeze(2).to_broadcast([64, 8, 128]), op=ALU.mult), R=[osb, oss], W=[osb])
                        yield
                        yd = ydst[st_i[0] % 2]; ssm = so[2 + st_i[0] % 2]; st_i[0] += 1
                        op("dve", lambda h: h.tensor_tensor(out=yd[:], in0=osb[:].rearrange("p a b -> p (a b)"), in1=zw[:, c, :], op=ALU.mult), R=[osb, zw], W=[yd])
                        yield
                        r0 = tok0 - NP + c * 64
                        dma(ydn_d[r0:r0 + 64, :], yd[:], ssm, R=[yd])
                        yield
                    S.group("pe", [lambda h, hh=hh: h.matmul(pV[:, hh * 128:(hh + 1) * 128], lhsT=kdec[:, hh, :], rhs=vnew[:, hh, :], start=True, stop=True) for hh in range(8)], R=[kdec, vnew], W=[pV])
                    yield
                    op("dve", lambda h: h.tensor_tensor(out=Sf[:, 0:4, :].rearrange("p a b -> p (a b)"), in0=Sf[:, 0:4, :].rearrange("p a b -> p (a b)"), in1=pV[:, 0:512], op=ALU.add), R=[Sf, pV], W=[Sf])
                    yield
                    op("dve", lambda h: h.tensor_tensor(out=Sf[:, 4:8, :].rearrange("p a b -> p (a b)"), in0=Sf[:, 4:8, :].rearrange("p a b -> p (a b)"), in1=pV[:, 512:1024], op=ALU.add), R=[Sf, pV], W=[Sf])
                    yield
                    op("act", lambda h: h.activation(out=Sb[:], in_=Sf[:], func=AF.Copy), R=[Sf], W=[Sb])
                    yield

            def interleave(gens):
                gens = list(gens)
                while gens:
                    for g_ in list(gens):
                        try:
                            next(g_)
                        except StopIteration:
                            gens.remove(g_)

            def chain(*gs):
                for g_ in gs:
                    yield from g_

            def gen_prep(ti):
                emit_prep(ti)
                yield

            def interleave_gen(gens):
                gens = list(gens)
                while gens:
                    for g_ in list(gens):
                        try:
                            next(g_)
                            yield
                        except StopIteration:
                            gens.remove(g_)

            emit_prep(0)
            interleave([chain(gen_rg(0), gen_dnt(0))])
            for ti in range(ntile):
                gs = [gen_ch(ti)]
                if ti + 1 < ntile:
                    gs.append(chain(gen_prep(ti + 1), gen_rg(ti + 1), gen_dnt(ti + 1)))
                interleave(gs)
            S.barrier()

        NSUB = NO // 128
        wte = sb(st, "wte", [128, NSUB, NE])
        if phases >= 2:
          with ExitStack() as s2:
            wbrg = sb(s2, "wbrg", [128, 8, D], BF16); wbdn = sb(s2, "wbdn", [128, 8, D], BF16); wout = sb(s2, "wout", [128, 8, D], BF16)
            wgt = sb(s2, "wgt", [128, 8, 2048], BF16); wr = sb(s2, "wr", [128, 8, 36], BF16); br = sb(s2, "br", [128, 36])
            stgsem = S.newsem("stg")
            with ExitStack() as s2a:
                stg = sb(s2a, "stg", [128, 8, D]); wrf = sb(s2a, "wrf", [128, 8, 36])
                dma(wrf[:], w_r36.rearrange("(kc p) n -> p kc n", p=128), ld, W=[wrf])
                dma(br[:], b_r36_d, ld, W=[br])
                dma(wgt[:], w_in_bf[:, OFF_GRG:OFF_GRG + 2048].rearrange("(kc p) n -> p kc n", p=128), ld, W=[wgt])
                S.barrier()
                op("dve", lambda h: h.tensor_copy(out=wr[:], in_=wrf[:]), R=[wrf], W=[wr])
                for i_, (dst, src) in enumerate(((wbrg, w_brg), (wbdn, w_bdn), (wout, w_out))):
                    dma(stg[:], src.rearrange("(kc p) n -> p kc n", p=128), stgsem, W=[stg])
                    op(("dve", "pool", "dve")[i_], lambda h: h.tensor_copy(out=dst[:], in_=stg[:]), R=[stg], W=[dst])
                S.barrier()
            T2 = 512
            gate1 = sb(s2, "gate1", [128, D]); dma(gate1[:], g12_d[0], gsem, W=[gate1])
            xt4 = sb(s2, "xt4", [128, 4, D]); x4sem = S.newsem("x4")
            junk = sb(s2, "junk2", [128, D], BF16); ss = sb(s2, "ss2", [128, 1]); xn = sb(s2, "xn2", [128, D], BF16); tmpf = sb(s2, "tmpf2", [128, 8, 128])
            hT = sb(s2, "hT2", [128, 8, T2], BF16); sgr = sb(s2, "sgr", [128, 8, T2], BF16); sgd = sb(s2, "sgd", [128, 8, T2], BF16)
            yrgT = sb(s2, "yrgT2", [128, 8, T2], BF16); ysem = S.newsem("yr2")
            ydn = sb(s2, "ydn2", [128, 4, D], BF16); ydsem = S.newsem("yd2"); ydnT = sb(s2, "ydnT", [128, 8, T2], BF16)
            mg = sb(s2, "mg", [128, 8, T2], BF16); m1 = sb(s2, "m1", [128, T2]); m2 = sb(s2, "m2", [128, T2])
            x2 = [sb(s2, "x2_%d" % i, [128, D]) for i in range(2)]; x2sem = [S.newsem("x2s%d" % i) for i in range(2)]
            h2T = sb(s2, "h2T", [128, 8, T2], BF16); h2sem = S.newsem("h2s")
            rt = []
            for i_ in range(4):
                rt.append((sb(s2, "lg%d" % i_, [128, 36]), sb(s2, "gmx%d" % i_, [128, 1]), sb(s2, "ngm%d" % i_, [128, 1]), sb(s2, "gex%d" % i_, [128, 4]), sb(s2, "gsum%d" % i_, [128, 1]),
                           sb(s2, "oh%d" % i_, [128, 4]), sb(s2, "elm%d" % i_, [128, 4, 8]), sb(s2, "m8%d" % i_, [128, 8]), sb(s2, "dd%d" % i_, [128, 1]), sb(s2, "w12%d" % i_, [128, 2]),
                           sb(s2, "mm1%d" % i_, [128, 32]), sb(s2, "mm2%d" % i_, [128, 32])))

            def interleave2(gens):
                gens = list(gens)
                while gens:
                    for g_ in list(gens):
                        try:
                            next(g_)
                        except StopIteration:
                            gens.remove(g_)
            pT = ps(s2, "pT2", [128, 1024], BF16); pP = [ps(s2, "pP%d" % i, [128, T2]) for i in range(2)]
            pO = ps(s2, "pO", [128, D]); pR = ps(s2, "pR", [128, 64])
            pp_i = [0]
            for ti in range(NO // T2):
                c0 = ti * T2
                dma(xt4[:], xo[c0:c0 + T2, :].rearrange("(s p) d -> p s d", p=128), x4sem, W=[xt4])
                dma(yrgT[:], yrgT_d[:, c0:c0 + T2].rearrange("(c p) n -> p c n", p=128), ysem, W=[yrgT])
                dma(ydn[:], ydn_d[c0:c0 + T2, :].rearrange("(s p) d -> p s d", p=128), ydsem, W=[ydn])
                for sub in range(4):
                    xs = TV3(xt4, sub)
                    norm_to_hT(xs, junk, ss, xn, pT, hT, sub * 128, A1, cols[:, 0, :], tmpf)
                for sub in range(4):
                    S.group("pe", [lambda h, kc=kc: h.transpose(out=pT[:, kc * 128:(kc + 1) * 128], in_=ydn[:, sub, kc * 128:(kc + 1) * 128], identity=ident_b[:]) for kc in range(8)], R=[ydn, ident_b], W=[pT])
                    op("act", lambda h: h.activation(out=ydnT[:, :, sub * 128:(sub + 1) * 128], in_=pT[:].rearrange("p (a b) -> p a b", a=8), func=AF.Copy), R=[pT], W=[ydnT])
                for gi, sg in ((0, sgr), (1, sgd)):
                    for oc in range(8):
                        p = pP[pp_i[0] % 2]; pp_i[0] += 1
                        S.group("pe", [lambda h, kc=kc: h.matmul(p[:], lhsT=wgt[:, kc, gi * 1024 + oc * 128:gi * 1024 + (oc + 1) * 128], rhs=hT[:, kc, :], start=(kc == 0), stop=(kc == 7)) for kc in range(8)], R=[wgt, hT], W=[p])
                        op("act", lambda h: h.activation(out=sg[:, oc, :], in_=p[:], func=AF.Sigmoid), R=[p], W=[sg])
                for oc in range(8):
                    p = pP[pp_i[0] % 2]; pp_i[0] += 1
                    S.group("pe", [lambda h, kc=kc: h.matmul(p[:], lhsT=wbrg[:, kc, oc * 128:(oc + 1) * 128], rhs=yrgT[:, kc, :], start=(kc == 0), stop=(kc == 7)) for kc in range(8)], R=[wbrg, yrgT], W=[p])
                    op("dve", lambda h: h.tensor_tensor(out=m1[:], in0=p[:], in1=sgr[:, oc, :], op=ALU.mult), R=[p, sgr], W=[m1])
                    p = pP[pp_i[0] % 2]; pp_i[0] += 1
                    S.group("pe", [lambda h, kc=kc: h.matmul(p[:], lhsT=wbdn[:, kc, oc * 128:(oc + 1) * 128], rhs=ydnT[:, kc, :], start=(kc == 0), stop=(kc == 7)) for kc in range(8)], R=[wbdn, ydnT], W=[p])
                    op("dve", lambda h: h.tensor_tensor(out=m2[:], in0=p[:], in1=sgd[:, oc, :], op=ALU.mult), R=[p, sgd], W=[m2])
                    op("pool", lambda h: h.tensor_tensor(out=mg[:, oc, :], in0=m1[:], in1=m2[:], op=ALU.add), R=[m1, m2], W=[mg])
                routes = []
                for sub in range(4):
                    r0 = c0 + sub * 128
                    lg, gmx, ngm, gex, gsum, oh, elm, m8, dd, w12, mm1, mm2 = rt[sub]
                    fns = []
                    for hf in range(2):
                        fns += [lambda h, kc=kc, hf=hf: h.matmul(pO[:, hf * 512:(hf + 1) * 512], lhsT=mg[:, kc, sub * 128:(sub + 1) * 128], rhs=wout[:, kc, hf * 512:(hf + 1) * 512], start=(kc == 0), stop=(kc == 7)) for kc in range(8)]
                    S.group("pe", fns, R=[mg, wout], W=[pO])
                    xx = x2[sub % 2]
                    for hf in range(2):
                        op("dve", lambda h: h.tensor_tensor(out=xx[:, hf * 512:(hf + 1) * 512], in0=pO[:, hf * 512:(hf + 1) * 512], in1=gate1[:, hf * 512:(hf + 1) * 512], op=ALU.mult), R=[pO, gate1], W=[xx])
                    op("pool", lambda h: h.tensor_tensor(out=xx[:], in0=xx[:], in1=xt4[:, sub, :], op=ALU.add), R=[xx, xt4], W=[xx])
                    dma(x2_d[r0:r0 + 128, :], xx[:], x2sem[sub % 2], R=[xx])
                    norm_to_hT(xx, junk, ss, xn, pT, h2T, sub * 128, A2, cols[:, 2, :], tmpf)
                    S.group("pe", [lambda h, kc=kc: h.matmul(pR[:, 0:36], lhsT=h2T[:, kc, sub * 128:(sub + 1) * 128], rhs=wr[:, kc, :], start=(kc == 0), stop=(kc == 7)) for kc in range(8)], R=[h2T, wr], W=[pR])
                    op("dve", lambda h: h.tensor_tensor(out=lg[:], in0=pR[:, 0:36], in1=br[:], op=ALU.add), R=[pR, br], W=[lg])
                    def route(sub, lg=lg, gmx=gmx, ngm=ngm, gex=gex, gsum=gsum, oh=oh, elm=elm, m8=m8, dd=dd, w12=w12, mm1=mm1, mm2=mm2):
                        op("dve", lambda h: h.reduce_max(out=gmx[:], in_=lg[:, 0:4], axis=AX.X), R=[lg], W=[gmx])
                        yield
                        op("dve", lambda h: h.tensor_scalar_mul(out=ngm[:], in0=gmx[:], scalar1=-1.0), R=[gmx], W=[ngm])
                        yield
                        op("act", lambda h: h.activation(out=gex[:], in_=lg[:, 0:4], func=AF.Exp, bias=ngm[:], accum_out=gsum[:]), R=[lg, ngm], W=[gex, gsum])
                        yield
                        op("dve", lambda h: h.reciprocal(out=gsum[:], in_=gsum[:]), R=[gsum], W=[gsum])
                        yield
                        op("dve", lambda h: h.tensor_scalar(out=oh[:], in0=lg[:, 0:4], scalar1=gmx[:], scalar2=1.0e9, op0=ALU.is_equal, op1=ALU.mult), R=[lg, gmx], W=[oh])
                        yield
                        op("dve", lambda h: h.tensor_scalar_add(out=oh[:], in0=oh[:], scalar1=-1.0e9), R=[oh], W=[oh])
                        yield
                        op("dve", lambda h: h.tensor_tensor(out=elm[:], in0=lg[:, 4:36].rearrange("p (a b) -> p a b", a=4), in1=oh[:].unsqueeze(2).to_broadcast([128, 4, 8]), op=ALU.add), R=[lg, oh], W=[elm])
                        yield
                        op("dve", lambda h: h.max(out=m8[:], in_=elm[:].rearrange("p a b -> p (a b)")), R=[elm], W=[m8])
                        yield
                        op("dve", lambda h: h.tensor_tensor(out=dd[:], in0=m8[:, 1:2], in1=m8[:, 0:1], op=ALU.subtract), R=[m8], W=[dd])
                        yield
                        op("act", lambda h: h.activation(out=dd[:], in_=dd[:], func=AF.Exp), R=[dd], W=[dd])
                        yield
                        op("dve", lambda h: h.tensor_scalar_add(out=w12[:, 0:1], in0=dd[:], scalar1=1.0), R=[dd], W=[w12])
                        yield
                        op("dve", lambda h: h.reciprocal(out=w12[:, 0:1], in_=w12[:, 0:1]), R=[w12], W=[w12])
                        yield
                        op("dve", lambda h: h.tensor_tensor(out=w12[:, 0:1], in0=w12[:, 0:1], in1=gsum[:], op=ALU.mult), R=[w12, gsum], W=[w12])
                        yield
                        op("dve", lambda h: h.tensor_tensor(out=w12[:, 1:2], in0=w12[:, 0:1], in1=dd[:], op=ALU.mult), R=[w12, dd], W=[w12])
                        yield
                        op("dve", lambda h: h.tensor_scalar(out=mm1[:], in0=elm[:].rearrange("p a b -> p (a b)"), scalar1=m8[:, 0:1], scalar2=w12[:, 0:1], op0=ALU.is_equal, op1=ALU.mult), R=[elm, m8, w12], W=[mm1])
                        yield
                        op("dve", lambda h: h.tensor_scalar(out=mm2[:], in0=elm[:].rearrange("p a b -> p (a b)"), scalar1=m8[:, 1:2], scalar2=w12[:, 1:2], op0=ALU.is_equal, op1=ALU.mult), R=[elm, m8, w12], W=[mm2])
                        yield
                        op("dve", lambda h: h.tensor_tensor(out=wte[:, ti * 4 + sub, :], in0=mm1[:], in1=mm2[:], op=ALU.add), R=[mm1, mm2], W=[wte])
                        yield
                    routes.append(route(sub))
                interleave2(routes)
                dma(h2T_d[:, c0:c0 + T2].rearrange("(c p) n -> p c n", p=128), h2T[:], h2sem, R=[h2T])
            S.barrier()

        if phases >= 3:
          with ExitStack() as s3:
            Q = min(1024, NO); NQS = Q // 128
            h2q = sb(s3, "h2q", [128, 8, Q], BF16); hqsem = S.newsem("hq")
            acc = sb(s3, "acc", [128, NQS, D])
            wgf = sb(s3, "wgf", [128, 8, DE]); wuf = sb(s3, "wuf", [128, 8, DE]); wdf = sb(s3, "wdf", [128, 4, D])
            fsem = [S.newsem("mf%d" % i) for i in range(3)]
            wgb = [sb(s3, "wgb%d" % i, [128, 8, DE], BF16) for i in range(2)]; wub = [sb(s3, "wub%d" % i, [128, 8, DE], BF16) for i in range(2)]
            wdb = [sb(s3, "wdb%d" % i, [128, 4, D], BF16) for i in range(2)]
            sgt = sb(s3, "sgt", [128, 512]); AT = sb(s3, "AT", [128, 4, 512], BF16)
            xf = [sb(s3, "xf%d" % i, [128, D]) for i in range(2)]; xfsem = [S.newsem("xf%d" % i) for i in range(2)]
            ss3 = sb(s3, "ss3", [128, 1]); junk3 = sb(s3, "junk3", [128, D], BF16)
            ob = [sb(s3, "ob%d" % i, [128, D]) for i in range(2)]; osem = [S.newsem("ob%d" % i) for i in range(2)]
            pG = [ps(s3, "pG%d" % i, [128, 512]) for i in range(2)]; pU = [ps(s3, "pU%d" % i, [128, 512]) for i in range(2)]
            pY = [ps(s3, "pY%d" % i, [128, 512]) for i in range(2)]
            gate2 = sb(s3, "gate2", [128, D]); fnw = sb(s3, "fnw", [128, D]); g3sem = S.newsem("g3sem")
            dma(gate2[:], g12_d[1], g3sem, W=[gate2])
            gi_ = [0]; yi_ = [0]
            f3sem = S.newsem("f3sem"); dma(fnw[:], fnw_d, f3sem, W=[fnw])
            for qi in range(NO // Q):
                q0 = qi * Q
                dma(h2q[:], h2T_d[:, q0:q0 + Q].rearrange("(c p) n -> p c n", p=128), hqsem, W=[h2q])
                op("pool", lambda h: h.memset(acc[:], 0.0), W=[acc])
                for e in range(NE):
                    b = e % 2
                    dma(wgf[:], moe_wg[e].rearrange("(kc p) n -> p kc n", p=128), fsem[0], W=[wgf])
                    dma(wuf[:], moe_wu[e].rearrange("(kc p) n -> p kc n", p=128), fsem[1], W=[wuf])
                    dma(wdf[:], moe_wd[e].rearrange("(kc p) n -> p kc n", p=128), fsem[2], W=[wdf])
                    op("act", lambda h: h.activation(out=wgb[b][:], in_=wgf[:], func=AF.Copy), R=[wgf], W=[wgb[b]])
                    op("act", lambda h: h.activation(out=wub[b][:], in_=wuf[:], func=AF.Copy), R=[wuf], W=[wub[b]])
                    op("pool", lambda h: h.tensor_copy(out=wdb[b][:, 0:2, :], in_=wdf[:, 0:2, :]), R=[wdf], W=[wdb[b]])
                    op("dve", lambda h: h.tensor_copy(out=wdb[b][:, 2:4, :], in_=wdf[:, 2:4, :]), R=[wdf], W=[wdb[b]])
                    for hf in range(Q // 512):
                        ts_ = slice(hf * 512, (hf + 1) * 512)
                        for oc in range(4):
                            g_ = pG[gi_[0] % 2]; u_ = pU[gi_[0] % 2]; gi_[0] += 1
                            S.group("pe", [lambda h, kc=kc: h.matmul(g_[:], lhsT=wgb[b][:, kc, oc * 128:(oc + 1) * 128], rhs=h2q[:, kc, ts_], start=(kc == 0), stop=(kc == 7)) for kc in range(8)], R=[wgb[b], h2q], W=[g_])
                            S.group("pe", [lambda h, kc=kc: h.matmul(u_[:], lhsT=wub[b][:, kc, oc * 128:(oc + 1) * 128], rhs=h2q[:, kc, ts_], start=(kc == 0), stop=(kc == 7)) for kc in range(8)], R=[wub[b], h2q], W=[u_])
                            op("act", lambda h: h.activation(out=sgt[:], in_=g_[:], func=AF.Silu), R=[g_], W=[sgt])
                            op("dve", lambda h: h.tensor_tensor(out=AT[:, oc, :], in0=u_[:], in1=sgt[:], op=ALU.mult), R=[u_, sgt], W=[AT])
                        for sub in range(4):
                            si = hf * 4 + sub
                            for ch in range(2):
                                y_ = pY[yi_[0] % 2]; yi_[0] += 1
                                S.group("pe", [lambda h, kc=kc: h.matmul(y_[:], lhsT=AT[:, kc, sub * 128:(sub + 1) * 128], rhs=wdb[b][:, kc, ch * 512:(ch + 1) * 512], start=(kc == 0), stop=(kc == 3)) for kc in range(4)], R=[AT, wdb[b]], W=[y_])
                                gs = qi * NQS + si
                                op("dve", lambda h: h.scalar_tensor_tensor(out=acc[:, si, ch * 512:(ch + 1) * 512], in0=y_[:], scalar=wte[:, gs, e:e + 1], in1=acc[:, si, ch * 512:(ch + 1) * 512], op0=ALU.mult, op1=ALU.add), R=[y_, wte, acc], W=[acc])
                for si in range(NQS):
                    r0 = q0 + si * 128
                    x_ = xf[si % 2]; o_ = ob[si % 2]
                    dma(x_[:], x2_d[r0:r0 + 128, :], xfsem[si % 2], W=[x_])
                    op("pool", lambda h: h.tensor_tensor(out=acc[:, si, :], in0=acc[:, si, :], in1=gate2[:], op=ALU.mult), R=[acc, gate2], W=[acc])
                    op("dve", lambda h: h.tensor_tensor(out=x_[:], in0=x_[:], in1=acc[:, si, :], op=ALU.add), R=[x_, acc], W=[x_])
                    op("act", lambda h: h.activation(out=junk3[:], in_=x_[:], func=AF.Square, scale=1.0 / 32, accum_out=ss3[:]), R=[x_], W=[junk3, ss3])
                    op("dve", lambda h: h.tensor_scalar_add(out=ss3[:], in0=ss3[:], scalar1=EPS), R=[ss3], W=[ss3])
                    op("act", lambda h: h.activation(out=ss3[:], in_=ss3[:], func=AF.Sqrt), R=[ss3], W=[ss3])
                    op("dve", lambda h: h.reciprocal(out=ss3[:], in_=ss3[:]), R=[ss3], W=[ss3])
                    op("dve", lambda h: h.scalar_tensor_tensor(out=o_[:], in0=x_[:], scalar=ss3[:, 0:1], in1=fnw[:], op0=ALU.mult, op1=ALU.mult), R=[x_, ss3, fnw], W=[o_])
                    dma(out_d[r0:r0 + 128, :], o_[:], osem[si % 2], R=[o_])
            S.barrier()

        if dbg and phases == 1:
            with ExitStack() as sd:
                a = sb(sd, "dba", [128, NO], BF16); b = sb(sd, "dbb", [128, D], BF16)
                for ch in range(8):
                    dma(a[:], yrgT_d[ch * 128:(ch + 1) * 128, :], ld, W=[a]); dma(dbg_out["d_yrgT"][ch * 128:(ch + 1) * 128, :], a[:], ld, R=[a])
                for r in range(NO // 128):
                    dma(b[:], ydn_d[r * 128:(r + 1) * 128, :], ld, W=[b]); dma(dbg_out["d_ydn"][r * 128:(r + 1) * 128, :], b[:], ld, R=[b])
                S.barrier()
        if dbg and phases >= 2:
            with ExitStack() as sd:
                b = sb(sd, "dbc", [128, D]); dsa = S.newsem("dsa"); dsb = S.newsem("dsb"); dsc = S.newsem("dsc")
                for r in range(NO // 128):
                    dma(b[:], x2_d[r * 128:(r + 1) * 128, :], dsa, W=[b]); dma(dbg_out["d_x2"][r * 128:(r + 1) * 128, :], b[:], dsb, R=[b])
                    dma(dbg_out["d_wte"][r * 128:(r + 1) * 128, :], wte[:, r, :], dsc, R=[wte])
                S.barrier()
        S.barrier()
    return nc


def _col(v, n=8):
    return np.ascontiguousarray(np.asarray(v, np.float32).reshape(n, 128).T)


def _rep(v, p=128):
    v = np.asarray(v, np.float32).reshape(1, -1)
    return np.ascontiguousarray(np.repeat(v, p, axis=0))


def shared_inputs(I):
    f = lambda a: np.ascontiguousarray(np.asarray(a, np.float32))
    d = {}
    d["w_ada"] = f(I["w_ada"][0]); d["b_ada_rep"] = _rep(I["b_ada"][0])
    d["n1w_col"] = _col(I["norm1_w"][0]); d["n2w_col"] = _col(I["norm2_w"][0]); d["fnw_rep"] = _rep(I["final_norm_w"])
    d["w_in"] = f(I["w_in"][0])
    d["rgcw"] = np.ascontiguousarray(f(I["rg_conv_w"][0]).reshape(4, 8, 128).transpose(2, 1, 0))
    d["rgcb"] = _col(I["rg_conv_b"][0])
    d["dncw"] = np.ascontiguousarray(f(I["dn_conv_w"][0]).reshape(4, 24, 128).transpose(2, 1, 0))
    ga = f(I["rg_gate_a_w"][0]).reshape(4, 2, 128, 256); gx = f(I["rg_gate_x_w"][0]).reshape(4, 2, 128, 256)
    d["rga_w"] = np.ascontiguousarray(ga.transpose(2, 0, 1, 3).reshape(128, 8, 256))
    d["rgx_w"] = np.ascontiguousarray(gx.transpose(2, 0, 1, 3).reshape(128, 8, 256))
    d["rga_b"] = _col(f(I["rg_gate_a_b"][0]).reshape(-1)); d["rgx_b"] = _col(f(I["rg_gate_x_b"][0]).reshape(-1))
    d["lam"] = _col(I["rg_lambda"][0])
    d["alog_rep"] = _rep(I["dn_a_log"][0], 64); d["dtb_rep"] = _rep(I["dn_dt_bias"][0], 64)
    d["dnw_rep"] = _rep(np.tile(f(I["dn_norm_w"][0]), 8), 64)
    d["w_brg"] = f(I["w_branch_rg"][0]); d["w_bdn"] = f(I["w_branch_dn"][0]); d["w_out"] = f(I["w_out"][0])
    d["w_r36"] = np.ascontiguousarray(np.concatenate([f(I["moe_w_group"][0]), f(I["moe_w_router"][0])], axis=1))
    d["b_r36_rep"] = _rep(np.concatenate([f(I["moe_b_group"][0]), f(I["moe_b_router"][0])]))
    d["moe_wg"] = f(I["moe_w_gate"][0]); d["moe_wu"] = f(I["moe_w_up"][0]); d["moe_wd"] = f(I["moe_w_down"][0])
    i = np.arange(64)
    d["c_ident"] = np.eye(128, dtype=np.float32)
    d["c_ut"] = (i[:, None] <= i[None, :]).astype(np.float32)
    d["c_maskgt"] = (i[:, None] > i[None, :]).astype(np.float32)
    d["c_neg"] = np.where(i[None, :] > i[:, None], -30000.0, 0.0).astype(np.float32)
    d["c_strict"] = (i[:, None] > i[None, :]).astype(np.float32)
    return d


def core_inputs(I, shared, b, half, NP, NO):
    x = np.asarray(I["x"], np.float32)
    d = dict(shared)
    own = x[b, half * NO:(half + 1) * NO]
    d["xo"] = np.ascontiguousarray(own)
    d["xp"] = np.ascontiguousarray(x[b, 0:NP]) if half == 1 else np.ascontiguousarray(own[0:NP])
    d["flag"] = np.full((128, 1), float(half), np.float32)
    d["ccol"] = _col(np.asarray(I["c"], np.float32)[b])
    return d


def kernel(**inputs):
    NP = NO = 4096
    nc = build(NP, NO)
    sh = shared_inputs(inputs)
    in_maps = [core_inputs(inputs, sh, b, half, NP, NO) for b in range(4) for half in range(2)]
    res = run_bass_kernel_spmd(nc, in_maps, core_ids=list(range(8)))
    out = np.empty((4, 2 * NO, D), np.float32)
    for i, r in enumerate(res.results):
        b, half = divmod(i, 2)
        out[b, half * NO:(half + 1) * NO] = np.asarray(r["out"], np.float32)
    return out
```

```python
import numpy as np
from contextlib import ExitStack
import concourse.bass as bass
import concourse.mybir as mybir
from concourse.bass_utils import run_bass_kernel_spmd

F32 = mybir.dt.float32
BF16 = mybir.dt.bfloat16
AF = mybir.ActivationFunctionType
ALU = mybir.AluOpType
AX = mybir.AxisListType

D = 1024
D_IN = 8208
OFF_RGX, OFF_RGY, OFF_Q, OFF_K, OFF_V, OFF_Z, OFF_BA, OFF_GRG, OFF_GDN = 0, 1024, 2048, 3072, 4096, 5120, 6144, 6160, 7184
NE = 32
DE = 512
EPS = 1e-6
TT = 256
CH = 64
NCH = TT // CH


class Buf:
    __slots__ = ("w", "r")

    def __init__(self):
        self.w = None
        self.r = {}


class T:
    def __init__(self, t):
        self.t = t
        self.b = Buf()

    def __getitem__(self, k):
        return self.t[k]


class TV:
    def __init__(self, t, lo, hi):
        self.t = t; self.lo = lo; self.hi = hi
        self.b = Buf()

    def __getitem__(self, k):
        assert k == slice(None)
        return self.t[:, self.lo:self.hi]


class TV3:
    def __init__(self, parent, sub):
        self.p = parent; self.sub = sub
        self.b = parent.b

    def __getitem__(self, k):
        assert k == slice(None)
        return self.p.t[:, self.sub, :]


class SemCounter:
    def __init__(self, nc, stack, name):
        self.h = stack.enter_context(nc.semaphore(name))
        self.n = 0


class Sync:
    def __init__(self, nc, stack):
        self.nc = nc
        self.eng = {"pe": nc.tensor, "act": nc.scalar, "dve": nc.vector, "pool": nc.gpsimd, "sp": nc.sync}
        self.sem = {k: stack.enter_context(nc.semaphore("s_" + k)) for k in ("pe", "act", "dve", "pool")}
        self.cnt = {k: 0 for k in self.sem}
        self.seen = {k: {} for k in self.eng}
        self.dsems = []
        self.stack = stack

    def newsem(self, name):
        s = SemCounter(self.nc, self.stack, name)
        self.dsems.append(s)
        return s

    def _need(self, e, tok, waits):
        if tok is None:
            return
        k, v = tok
        if k == e and e == "pe":
            return
        if self.seen[e].get(k, 0) < v:
            waits[k] = max(waits.get(k, 0), v)

    def _waits(self, e, reads, writes):
        waits = {}
        for b in reads:
            self._need(e, b.b.w, waits)
        for b in writes:
            self._need(e, b.b.w, waits)
            for k, v in b.b.r.items():
                self._need(e, (k, v), waits)
        h = self.eng[e]
        for k, v in waits.items():
            h.wait_ge(self.sem[k] if isinstance(k, str) else k, v)
            self.seen[e][k] = v
        return h

    def op(self, e, fn, R=(), W=()):
        h = self._waits(e, R, W)
        ins = fn(h)
        self.cnt[e] += 1
        ins.then_inc(self.sem[e], 1)
        tok = (e, self.cnt[e])
        for b in R:
            b.b.r[e] = self.cnt[e]
        for b in W:
            b.b.w = tok
            b.b.r = {}
        return tok

    def group(self, e, fns, R=(), W=()):
        h = self._waits(e, R, W)
        ins = None
        for fn in fns:
            ins = fn(h)
        self.cnt[e] += 1
        ins.then_inc(self.sem[e], 1)
        tok = (e, self.cnt[e])
        for b in R:
            b.b.r[e] = self.cnt[e]
        for b in W:
            b.b.w = tok
            b.b.r = {}
        return tok

    def dma(self, out, in_, sem, R=(), W=(), q="sp", **kw):
        h = self._waits(q, R, W)
        sem.n += 16
        h.dma_start(out=out, in_=in_, **kw).then_inc(sem.h, 16)
        tok = (sem.h, sem.n)
        for b in R:
            b.b.r[sem.h] = sem.n
        for b in W:
            b.b.w = tok
            b.b.r = {}
        return tok

    def barrier(self):
        for e, h in self.eng.items():
            for k in self.sem:
                if not (k == e and e == "pe") and self.seen[e].get(k, 0) < self.cnt[k]:
                    h.wait_ge(self.sem[k], self.cnt[k])
                    self.seen[e][k] = self.cnt[k]
            for s in self.dsems:
                if s.n and self.seen[e].get(s.h, 0) < s.n:
                    h.wait_ge(s.h, s.n)
                    self.seen[e][s.h] = s.n


def build(NP, NO, phases=3, dbg=False, cut=99):
    assert NP % TT == 0 and NO % TT == 0
    nc = bass.Bass("TRN2", target_bir_lowering=False)

    def din(name, shape, dt=F32):
        return nc.dram_tensor(name, list(shape), dt, kind="ExternalInput").ap()

    def dscr(name, shape, dt):
        return nc.dram_tensor(name, list(shape), dt, kind="Internal").ap()

    xp = din("xp", [NP, D]); xo = din("xo", [NO, D]); flag_d = din("flag", [128, 1]); ccol_d = din("ccol", [128, 8])
    w_ada = din("w_ada", [D, 6 * D]); b_ada_rep = din("b_ada_rep", [128, 6 * D])
    n1w_d = din("n1w_col", [128, 8]); n2w_d = din("n2w_col", [128, 8]); fnw_d = din("fnw_rep", [128, D])
    w_in = din("w_in", [D, D_IN])
    rgcw_d = din("rgcw", [128, 8, 4]); rgcb_d = din("rgcb", [128, 8]); dncw_d = din("dncw", [128, 24, 4])
    rgaw_d = din("rga_w", [128, 8, 256]); rgxw_d = din("rgx_w", [128, 8, 256])
    rgab_d = din("rga_b", [128, 8]); rgxb_d = din("rgx_b", [128, 8]); lam_d = din("lam", [128, 8])
    alog_d = din("alog_rep", [64, 8]); dtb_d = din("dtb_rep", [64, 8]); dnw_d = din("dnw_rep", [64, D])
    w_brg = din("w_brg", [D, D]); w_bdn = din("w_bdn", [D, D]); w_out = din("w_out", [D, D])
    w_r36 = din("w_r36", [D, 36]); b_r36_d = din("b_r36_rep", [128, 36])
    moe_wg = din("moe_wg", [NE, D, DE]); moe_wu = din("moe_wu", [NE, D, DE]); moe_wd = din("moe_wd", [NE, DE, D])
    c_ident = din("c_ident", [128, 128]); c_ut = din("c_ut", [64, 64]); c_maskgt = din("c_maskgt", [64, 64])
    c_neg = din("c_neg", [64, 64]); c_strict = din("c_strict", [64, 64])
    out_d = nc.dram_tensor("out", [NO, D], F32, kind="ExternalOutput").ap()
    w_in_bf = dscr("w_in_bf", [D, D_IN], BF16)
    yrgT_d = dscr("yrgT", [D, NO], BF16)
    ydn_d = dscr("ydn", [NO, D], BF16)
    x2_d = dscr("x2", [NO, D], F32)
    h2T_d = dscr("h2T", [D, NO], BF16)
    wte_d = dscr("wte", [NO, NE], F32)
    g12_d = dscr("g12", [2, 128, D], F32)
    dbg_out = {}
    if dbg:
        dbg_out["d_yrgT"] = nc.dram_tensor("d_yrgT", [D, NO], BF16, kind="ExternalOutput").ap()
        dbg_out["d_ydn"] = nc.dram_tensor("d_ydn", [NO, D], BF16, kind="ExternalOutput").ap()
        dbg_out["d_x2"] = nc.dram_tensor("d_x2", [NO, D], F32, kind="ExternalOutput").ap()
        dbg_out["d_wte"] = nc.dram_tensor("d_wte", [NO, NE], F32, kind="ExternalOutput").ap()

    with ExitStack() as st:
        S = Sync(nc, st)
        op, dma = S.op, S.dma

        def act_sigmoid(out_ap, outT, in_ap, inR, scale=1.0, nbias=None, nbR=()):
            if nbias is not None:
                op("act", lambda h: h.activation(out=out_ap, in_=in_ap, func=AF.Exp, scale=-scale, bias=nbias), R=list(inR) + list(nbR), W=[outT])
            else:
                op("act", lambda h: h.activation(out=out_ap, in_=in_ap, func=AF.Exp, scale=-scale), R=list(inR), W=[outT])
            op("act", lambda h: h.activation(out=out_ap, in_=out_ap, func=AF.Ln, bias=1.0), R=[outT], W=[outT])
            op("act", lambda h: h.activation(out=out_ap, in_=out_ap, func=AF.Exp, scale=-1.0), R=[outT], W=[outT])

        def act_rsqrt(out_ap, outT):
            op("act", lambda h: h.activation(out=out_ap, in_=out_ap, func=AF.Ln), R=[outT], W=[outT])
            op("act", lambda h: h.activation(out=out_ap, in_=out_ap, func=AF.Exp, scale=-0.5), R=[outT], W=[outT])

        def sb(stk, name, shape, dt=F32):
            return T(stk.enter_context(nc.sbuf_tensor("s_" + name, list(shape), dt)))

        def ps(stk, name, shape, dt=F32):
            return T(stk.enter_context(nc.psum_tensor("p_" + name, list(shape), dt)))

        st.enter_context(nc.Block())
        ld = S.newsem("ld")
        so = [S.newsem("so%d" % i) for i in range(4)]

        ident_f = sb(st, "ident_f", [128, 128]); ident_b = sb(st, "ident_b", [128, 128], BF16)
        ones_b = sb(st, "ones_b", [128, 128], BF16); ones_f = sb(st, "ones_f", [64, 128])
        flag = sb(st, "flag", [128, 1])
        cols = sb(st, "cols", [128, 4, 8])
        A1 = sb(st, "A1", [128, 8]); A2 = sb(st, "A2", [128, 8]); n1w = sb(st, "n1w", [128, 8]); n2w = sb(st, "n2w", [128, 8])
        gsem = S.newsem("gsem")
        for (t_, d_) in ((ident_f, c_ident), (flag, flag_d), (n1w, n1w_d), (n2w, n2w_d)):
            dma(t_[:], d_, ld, W=[t_])
        S.barrier()
        op("dve", lambda h: h.tensor_copy(out=ident_b[:], in_=ident_f[:]), R=[ident_f], W=[ident_b])
        op("pool", lambda h: h.memset(ones_b[:], 1.0), W=[ones_b])
        op("pool", lambda h: h.memset(ones_f[:], 1.0), W=[ones_f])

        with ExitStack() as s0:
            ccol = sb(s0, "ccol", [128, 8]); crep = sb(s0, "crep", [128, 8, 128])
            wsl = [sb(s0, "wsl%d" % i, [128, 8, 512]) for i in range(2)]
            wsem = [S.newsem("wsl%d" % i) for i in range(2)]
            bsl = sb(s0, "bsl", [128, 512]); mtmp = sb(s0, "mtmp", [128, 4, 128]); mt2 = sb(s0, "mt2", [128, 4, 128])
            pm = ps(s0, "pm", [128, 512])
            bsem = S.newsem("bsl")
            dma(ccol[:], ccol_d, ld, W=[ccol])
            S.barrier()
            op("act", lambda h: h.activation(out=ccol[:], in_=ccol[:], func=AF.Silu), R=[ccol], W=[ccol])
            op("dve", lambda h: h.tensor_copy(out=crep[:], in_=ccol[:].unsqueeze(2).to_broadcast([128, 8, 128])), R=[ccol], W=[crep])
            for s in range(12):
                w_ = wsl[s % 2]
                dma(w_[:], w_ada[:, s * 512:(s + 1) * 512].rearrange("(kc p) n -> p kc n", p=128), wsem[s % 2], W=[w_])
                dma(bsl[:], b_ada_rep[:, s * 512:(s + 1) * 512], bsem, W=[bsl])
                S.group("pe", [lambda h, kc=kc: h.matmul(pm[:], lhsT=crep[:, kc, :], rhs=w_[:, kc, :], start=(kc == 0), stop=(kc == 7)) for kc in range(8)], R=[crep, w_], W=[pm])
                v, half = s // 2, s % 2
                if v in (2, 5):
                    op("dve", lambda h: h.tensor_tensor(out=mtmp[:].rearrange("p a b -> p (a b)"), in0=pm[:], in1=bsl[:], op=ALU.add), R=[pm, bsl], W=[mtmp])
                    dma(g12_d[0 if v == 2 else 1][:, half * 512:(half + 1) * 512], mtmp[:].rearrange("p a b -> p (a b)"), gsem, R=[mtmp])
                else:
                    ci = {0: 0, 1: 1, 3: 2, 4: 3}[v]
                    op("dve", lambda h: h.tensor_tensor(out=mtmp[:].rearrange("p a b -> p (a b)"), in0=pm[:], in1=bsl[:], op=ALU.add), R=[pm, bsl], W=[mtmp])
                    op("pool", lambda h: h.tensor_tensor(out=mt2[:], in0=mtmp[:], in1=ident_f[:].unsqueeze(1).to_broadcast([128, 4, 128]), op=ALU.mult), R=[mtmp, ident_f], W=[mt2])
                    op("dve", lambda h: h.reduce_sum(out=cols[:, ci, half * 4:(half + 1) * 4], in_=mt2[:], axis=AX.X), R=[mt2], W=[cols])
            op("dve", lambda h: h.scalar_tensor_tensor(out=A1[:], in0=cols[:, 1, :], scalar=1.0, in1=n1w[:], op0=ALU.add, op1=ALU.mult), R=[cols, n1w], W=[A1])
            op("dve", lambda h: h.scalar_tensor_tensor(out=A2[:], in0=cols[:, 3, :], scalar=1.0, in1=n2w[:], op0=ALU.add, op1=ALU.mult), R=[cols, n2w], W=[A2])
            S.barrier()

        with ExitStack() as s0:
          if cut >= 2:
            wf = [sb(s0, "wf%d" % i, [128, 8, 256]) for i in range(4)]
            wb = [sb(s0, "wb%d" % i, [128, 8, 256], BF16) for i in range(4)]
            lsem = [S.newsem("wfl%d" % i) for i in range(4)]
            nsl = (D_IN + 255) // 256
            for s in range(nsl):
                c0 = s * 256; cw = min(256, D_IN - c0)
                f_, b_ = wf[s % 4], wb[s % 4]
                dma(f_[:, :, 0:cw], w_in[:, c0:c0 + cw].rearrange("(kc p) n -> p kc n", p=128), lsem[s % 4], W=[f_])
                op(("dve", "pool", "dve", "act")[s % 4], (lambda h: h.activation(out=b_[:, :, 0:cw], in_=f_[:, :, 0:cw], func=AF.Copy)) if s % 4 == 3 else (lambda h: h.tensor_copy(out=b_[:, :, 0:cw], in_=f_[:, :, 0:cw])), R=[f_], W=[b_])
                dma(w_in_bf[:, c0:c0 + cw].rearrange("(kc p) n -> p kc n", p=128), b_[:, :, 0:cw], so[s % 4], R=[b_], q=("sp" if s % 4 == 3 else "act"))
            S.barrier()

        def norm_to_hT(xt, junk, ss, xn, pT, hT, col0, Acol, Bcol_ap, tmpf):
            op("act", lambda h: h.activation(out=junk[:], in_=xt[:], func=AF.Square, scale=1.0 / 32, accum_out=ss[:]), R=[xt], W=[junk, ss])
            op("dve", lambda h: h.tensor_scalar_add(out=ss[:], in0=ss[:], scalar1=EPS), R=[ss], W=[ss])
            act_rsqrt(ss[:], ss)
            op("dve", lambda h: h.tensor_scalar_mul(out=xn[:], in0=xt[:], scalar1=ss[:]), R=[xt, ss], W=[xn])
            S.group("pe", [lambda h, kc=kc: h.transpose(out=pT[:, kc * 128:(kc + 1) * 128], in_=xn[:, kc * 128:(kc + 1) * 128], identity=ident_b[:]) for kc in range(8)], R=[xn, ident_b], W=[pT])
            op("dve", lambda h: h.tensor_tensor(out=tmpf[:], in0=pT[:].rearrange("p (a b) -> p a b", a=8), in1=Acol[:].unsqueeze(2).to_broadcast([128, 8, 128]), op=ALU.mult), R=[pT, Acol], W=[tmpf])
            op("pool", lambda h: h.tensor_tensor(out=hT[:, :, col0:col0 + 128], in0=tmpf[:], in1=Bcol_ap.unsqueeze(2).to_broadcast([128, 8, 128]), op=ALU.add), R=[tmpf, cols], W=[hT])

        with ExitStack() as s1:
            rgcw = sb(s1, "rgcw", [128, 8, 4]); rgcb = sb(s1, "rgcb", [128, 8]); dncw = sb(s1, "dncw", [128, 24, 4])
            rgab = sb(s1, "rgab", [128, 8]); rgxb = sb(s1, "rgxb", [128, 8]); lamc = sb(s1, "lamc", [128, 8])
            rgaw = sb(s1, "rgaw", [128, 8, 256], BF16); rgxw = sb(s1, "rgxw", [128, 8, 256], BF16)
            alog = sb(s1, "alog", [64, 8]); dtb = sb(s1, "dtb", [64, 8]); dnw = sb(s1, "dnw", [64, D])
            ut = sb(s1, "ut", [64, 64]); maskgt = sb(s1, "maskgt", [64, 64]); negm = sb(s1, "negm", [64, 64]); strict = sb(s1, "strict", [64, 64])
            for (t_, d_) in ((rgcw, rgcw_d), (rgcb, rgcb_d), (dncw, dncw_d), (rgab, rgab_d), (rgxb, rgxb_d), (lamc, lam_d),
                             (alog, alog_d), (dtb, dtb_d), (dnw, dnw_d), (ut, c_ut), (maskgt, c_maskgt), (negm, c_neg), (strict, c_strict)):
                dma(t_[:], d_, ld, W=[t_])
            with ExitStack() as s1a:
                rgw_f = sb(s1a, "rgw_f", [128, 8, 256]); rgw_f2 = sb(s1a, "rgw_f2", [128, 8, 256])
                dma(rgw_f[:], rgaw_d, ld, W=[rgw_f])
                dma(rgw_f2[:], rgxw_d, ld, W=[rgw_f2])
                S.barrier()
                op("dve", lambda h: h.tensor_copy(out=rgaw[:], in_=rgw_f[:]), R=[rgw_f], W=[rgaw])
                op("dve", lambda h: h.tensor_copy(out=rgxw[:], in_=rgw_f2[:]), R=[rgw_f2], W=[rgxw])
                S.barrier()
            nrgab = sb(s1, "nrgab", [128, 8]); nrgxb = sb(s1, "nrgxb", [128, 8])
            op("dve", lambda h: h.tensor_scalar_mul(out=nrgab[:], in0=rgab[:], scalar1=-1.0), R=[rgab], W=[nrgab])
            op("dve", lambda h: h.tensor_scalar_mul(out=nrgxb[:], in0=rgxb[:], scalar1=-1.0), R=[rgxb], W=[nrgxb])
            op("act", lambda h: h.activation(out=lamc[:], in_=lamc[:], func=AF.Exp, scale=-1.0), R=[lamc], W=[lamc])
            op("act", lambda h: h.activation(out=lamc[:], in_=lamc[:], func=AF.Ln, bias=1.0), R=[lamc], W=[lamc])
            op("dve", lambda h: h.tensor_scalar_mul(out=lamc[:], in0=lamc[:], scalar1=-8.0), R=[lamc], W=[lamc])
            op("act", lambda h: h.activation(out=alog[:], in_=alog[:], func=AF.Exp), R=[alog], W=[alog])
            op("dve", lambda h: h.tensor_scalar_mul(out=alog[:], in0=alog[:], scalar1=-1.0), R=[alog], W=[alog])
            i8f = sb(s1, "i8f", [64, 8, 64])
            strict8 = sb(s1, "strict8", [64, 8, 64]); maskgt8 = strict8; neg8 = sb(s1, "neg8", [64, 8, 64])
            op("dve", lambda h: h.tensor_copy(out=i8f[:], in_=ident_f[0:64, 0:64].unsqueeze(1).to_broadcast([64, 8, 64])), R=[ident_f], W=[i8f])
            op("dve", lambda h: h.tensor_copy(out=strict8[:], in_=strict[:].unsqueeze(1).to_broadcast([64, 8, 64])), R=[strict], W=[strict8])
            op("dve", lambda h: h.tensor_copy(out=neg8[:], in_=negm[:].unsqueeze(1).to_broadcast([64, 8, 64])), R=[negm], W=[neg8])

            carry = sb(s1, "carry", [128, 32, 3]); hst = sb(s1, "hst", [128, 8])
            Sf = sb(s1, "Sf", [128, 8, 128]); Sb = sb(s1, "Sb", [128, 8, 128], BF16)
            for t_ in (carry, hst, Sf, Sb):
                op("pool", lambda h: h.memset(t_[:], 0.0), W=[t_])

            xt = [sb(s1, "xt%d" % i, [128, D]) for i in range(2)]; xsem = [S.newsem("xs%d" % i) for i in range(2)]
            junk = sb(s1, "junk", [128, D], BF16); ss = sb(s1, "ss", [128, 1]); xn = sb(s1, "xn", [128, D], BF16)
            tmpf = sb(s1, "tmpf", [128, 8, 128])
            hT = sb(s1, "hT", [128, 8, TT], BF16); hT_b = sb(s1, "hT_b", [128, 8, TT], BF16)
            NSLAB = 4
            slabs = [sb(s1, "slab%d" % i, [128, 8, 128], BF16) for i in range(NSLAB)]; slsem = [S.newsem("sl%d" % i) for i in range(NSLAB)]
            pre_r = sb(s1, "pre", [128, TT + 3]); preD = [sb(s1, "preD%d" % i, [128, TT + 3]) for i in range(2)]; d1 = [sb(s1, "d1_%d" % i, [128, TT]) for i in range(2)]; d2 = [sb(s1, "d2_%d" % i, [128, TT]) for i in range(2)]; d3 = [sb(s1, "d3_%d" % i, [128, TT]) for i in range(2)]; sqbD = [sb(s1, "sqbD%d" % i, [128, TT], BF16) for i in range(2)]; vTD = [sb(s1, "vTD%d" % i, [128, TT], BF16) for i in range(2)]; xc = sb(s1, "xc", [128, 2, TT]); xcb = sb(s1, "xcb", [128, 2, TT], BF16)
            t1 = sb(s1, "t1", [128, TT]); t2 = sb(s1, "t2", [128, TT]); t3 = sb(s1, "t3", [128, TT]); t4 = sb(s1, "t4", [128, TT])
            sqb = sb(s1, "sqb", [128, TT], BF16)
            yst = [sb(s1, "yst%d" % i, [128, TT], BF16) for i in range(2)]
            qTs = [sb(s1, "qT%d" % i, [128, 8, TT], BF16) for i in range(2)]; kTs = [sb(s1, "kT%d" % i, [128, 8, TT], BF16) for i in range(2)]
            vtoks = [sb(s1, "vtok%d" % i, [64, NCH, 8, 128], BF16) for i in range(2)]; ktoks = [sb(s1, "ktok%d" % i, [64, NCH, 8, 128], BF16) for i in range(2)]
            zws = [sb(s1, "zw%d" % i, [64, NCH, D], BF16) for i in range(2)]; ztmp = sb(s1, "ztmp", [64, NCH, 128])
            bas = [sb(s1, "ba%d" % i, [64, NCH, 16]) for i in range(2)]
            beta = sb(s1, "beta", [64, 8]); gstep = sb(s1, "gstep", [64, 8]); gsb = sb(s1, "gsb", [64, 16]); Gam = sb(s1, "Gam", [64, 8])
            dec = sb(s1, "dec", [64, 8]); bG = sb(s1, "bG", [64, 8]); geT = sb(s1, "geT", [128, 8])
            Rm = sb(s1, "Rm", [64, 8, 64]); E = sb(s1, "E", [64, 8, 64]); Es = sb(s1, "Es", [64, 8, 64]); tA = sb(s1, "tA", [64, 8, 64])
            Ak = [sb(s1, "Ak%d" % i, [64, 8, 64]) for i in range(2)]; Bk = [sb(s1, "Bk%d" % i, [64, 8, 64]) for i in range(2)]
            Mk = [sb(s1, "Mk%d" % i, [64, 8, 64]) for i in range(2)]; Mb = sb(s1, "Mb", [64, 8, 64], BF16)
            qk = sb(s1, "qk", [64, 8, 64], BF16); qkT_ = sb(s1, "qkT", [64, 8, 64], BF16)
            bv = sb(s1, "bv", [64, 8, 128], BF16); bgk = sb(s1, "bgk", [64, 8, 128], BF16); kdec = sb(s1, "kdec", [64, 8, 128], BF16)
            wTn = sb(s1, "wTn", [128, 8, 64], BF16); dG = sb(s1, "dG", [64, 8, 64]); qg = sb(s1, "qg", [128, 8, 64], BF16)
            vnew = sb(s1, "vnew", [64, 8, 128], BF16); osb = sb(s1, "osb", [64, 8, 128]); osq = sb(s1, "osq", [64, 8, 128])
            oss = sb(s1, "oss", [64, 8]); ydst = [sb(s1, "ydst%d" % i, [64, D], BF16) for i in range(2)]
            pA = [ps(s1, "pA%d" % i, [128, TT]) for i in range(2)]
            pT = ps(s1, "pT", [128, 1024], BF16)
            pX = ps(s1, "pX", [128, 512]); pK = ps(s1, "pK", [128, 512]); pCh = ps(s1, "pCh", [128, 512]); pV = ps(s1, "pV", [128, 1024])
            S.barrier()

            ntile = (NP + NO) // TT
            npre = NP // TT

            def rg_slabs(own):
                l = []
                for g in range(4):
                    l += [OFF_RGX + (2 * g) * 128, OFF_RGX + (2 * g + 1) * 128]
                    if own:
                        l += [OFF_RGY + (2 * g) * 128, OFF_RGY + (2 * g + 1) * 128]
                return l

            def dnt_slabs(own):
                l = []
                for pp in range(4):
                    for o_ in (OFF_Q, OFF_K, OFF_V):
                        l += [o_ + (2 * pp) * 128, o_ + (2 * pp + 1) * 128]
                l += [OFF_BA]
                if own:
                    l += [OFF_Z + j * 128 for j in range(8)]
                return l

            sched = []
            for ti in range(ntile):
                sched += rg_slabs(ti >= npre) + dnt_slabs(ti >= npre)
            state = {"issued": 0, "used": 0}

            def issue_slab():
                i = state["issued"]
                if i >= len(sched):
                    return
                c0 = sched[i]; cw = min(128, D_IN - c0)
                sl = slabs[i % NSLAB]
                dma(sl[:, :, 0:cw], w_in_bf[:, c0:c0 + cw].rearrange("(kc p) n -> p kc n", p=128), slsem[i % NSLAB], W=[sl])
                state["issued"] += 1

            def next_slab(expect=None):
                assert expect is None or sched[state["used"]] == expect, (expect, sched[state["used"]], state["used"])
                while state["issued"] < min(len(sched), state["used"] + NSLAB - 1) or state["issued"] <= state["used"]:
                    issue_slab()
                sl = slabs[state["used"] % NSLAB]
                state["used"] += 1
                return sl

            pa_i = [0]

            def proj_fm(sl, hT, ptile=None):
                if ptile is not None:
                    p = ptile
                else:
                    p = pA[pa_i[0] % 2]; pa_i[0] += 1
                S.group("pe", [lambda h, kc=kc: h.matmul(p[:], lhsT=sl[:, kc, :], rhs=hT[:, kc, :], start=(kc == 0), stop=(kc == 7)) for kc in range(8)], R=[sl, hT], W=[p])
                return p

            def conv(p, cidx, wcol, bias_ap, dst_ap, dstT, pre=None):
                pre = pre if pre is not None else pre_r
                op("pool", lambda h: h.tensor_copy(out=pre[:, 0:3], in_=carry[:, cidx, :]), R=[carry], W=[pre])
                op("act", lambda h: h.activation(out=pre[:, 3:3 + TT], in_=p[:], func=AF.Copy), R=[p], W=[pre])
                if bias_ap is not None:
                    op("act", lambda h: h.activation(out=dst_ap, in_=p[:], func=AF.Identity, scale=wcol[:, 3:4], bias=bias_ap), R=[p], W=[dstT])
                else:
                    op("act", lambda h: h.activation(out=dst_ap, in_=p[:], func=AF.Identity, scale=wcol[:, 3:4]), R=[p], W=[dstT])
                op("pool", lambda h: h.tensor_copy(out=carry[:, cidx, :], in_=pre[:, TT:TT + 3]), R=[pre], W=[carry])
                for j in range(0, 3):
                    op("dve", lambda h: h.scalar_tensor_tensor(out=dst_ap, in0=pre[:, j:j + TT], scalar=wcol[:, j:j + 1], in1=dst_ap, op0=ALU.mult, op1=ALU.add), R=[pre, dstT], W=[dstT])

            st_i = [0]; st_j = [0]
            pend_rg = []; pend_ch = []

            def defer_store(pend, fn):
                while pend:
                    pend.pop(0)()
                pend.append(fn)

            hTs = [hT, hT_b]

            def emit_prep(ti):
                hTt = hTs[ti % 2]
                own = ti >= npre
                tok0 = ti * TT
                for sub in range(TT // 128):
                    r0 = tok0 + sub * 128
                    src = xp[r0:r0 + 128, :] if r0 < NP else xo[r0 - NP:r0 - NP + 128, :]
                    xi = (ti * (TT // 128) + sub) % 2
                    dma(xt[xi][:], src, xsem[xi], W=[xt[xi]])
                    norm_to_hT(xt[xi], junk, ss, xn, pT, hTt, sub * 128, A1, cols[:, 0, :], tmpf)

            def gen_rg(ti):
                own = ti >= npre; tok0 = ti * TT; hTt = hTs[ti % 2]
                if ti == npre:
                    op("dve", lambda h: h.tensor_scalar_mul(out=carry[:, 0:8, :], in0=carry[:, 0:8, :], scalar1=flag[:, 0:1]), R=[carry, flag], W=[carry])
                    op("dve", lambda h: h.tensor_scalar_mul(out=hst[:], in0=hst[:], scalar1=flag[:, 0:1]), R=[hst, flag], W=[hst])
                for g in range(4):
                    for jc in range(2):
                        ch = 2 * g + jc
                        p = proj_fm(next_slab(), hTt)
                        yield
                        conv(p, ch, rgcw[:, ch, :], rgcb[:, ch:ch + 1], xc[:, jc, :], xc)
                        yield
                    ypp = []
                    if own:
                        for jc in range(2):
                            ypp.append(next_slab())
                    op("pool", lambda h: h.tensor_copy(out=xcb[:], in_=xc[:]), R=[xc], W=[xcb])
                    yield
                    for oc in range(2):
                        ch = 2 * g + oc
                        p = pA[pa_i[0] % 2]; pa_i[0] += 1
                        S.group("pe", [lambda h, ic=ic: h.matmul(p[:], lhsT=rgaw[:, 2 * g + ic, oc * 128:(oc + 1) * 128], rhs=xcb[:, ic, :], start=(ic == 0), stop=(ic == 1)) for ic in range(2)], R=[rgaw, xcb], W=[p])
                        yield
                        act_sigmoid(t1[:], t1, p[:], [p], nbias=nrgab[:, ch:ch + 1], nbR=[nrgab])
                        yield
                        p = pA[pa_i[0] % 2]; pa_i[0] += 1
                        S.group("pe", [lambda h, ic=ic: h.matmul(p[:], lhsT=rgxw[:, 2 * g + ic, oc * 128:(oc + 1) * 128], rhs=xcb[:, ic, :], start=(ic == 0), stop=(ic == 1)) for ic in range(2)], R=[rgxw, xcb], W=[p])
                        yield
                        act_sigmoid(t2[:], t2, p[:], [p], nbias=nrgxb[:, ch:ch + 1], nbR=[nrgxb])
                        yield
                        op("act", lambda h: h.activation(out=t1[:], in_=t1[:], func=AF.Exp, scale=lamc[:, ch:ch + 1]), R=[t1, lamc], W=[t1])
                        yield
                        op("pool", lambda h: h.tensor_tensor(out=t3[:], in0=t1[:], in1=t1[:], op=ALU.mult), R=[t1], W=[t3])
                        yield
                        op("act", lambda h: h.activation(out=t3[:], in_=t3[:], func=AF.Ln, scale=-1.0, bias=1.0), R=[t3], W=[t3])
                        yield
                        op("act", lambda h: h.activation(out=t3[:], in_=t3[:], func=AF.Exp, scale=0.5), R=[t3], W=[t3])
                        yield
                        op("dve", lambda h: h.tensor_tensor(out=t2[:], in0=t2[:], in1=xc[:, oc, :], op=ALU.mult), R=[t2, xc], W=[t2])
                        yield
                        op("dve", lambda h: h.tensor_tensor(out=t2[:], in0=t2[:], in1=t3[:], op=ALU.mult), R=[t2, t3], W=[t2])
                        yield
                        op("dve", lambda h: h.tensor_tensor_scan(out=t4[:], data0=t1[:], data1=t2[:], initial=hst[:, ch:ch + 1], op0=ALU.mult, op1=ALU.add), R=[t1, t2, hst], W=[t4])
                        yield
                        op("pool", lambda h: h.tensor_copy(out=hst[:, ch:ch + 1], in_=t4[:, TT - 1:TT]), R=[t4], W=[hst])
                        yield
                        if own:
                            p = proj_fm(ypp[oc], hTt)
                            yield
                            op("act", lambda h: h.activation(out=t1[:], in_=p[:], func=AF.Square), R=[p], W=[t1])
                            yield
                            op("dve", lambda h: h.tensor_scalar(out=t1[:], in0=t1[:], scalar1=0.044715, scalar2=1.0, op0=ALU.mult, op1=ALU.add), R=[t1], W=[t1])
                            yield
                            op("dve", lambda h: h.tensor_tensor(out=t1[:], in0=t1[:], in1=p[:], op=ALU.mult), R=[t1, p], W=[t1])
                            yield
                            act_sigmoid(t1[:], t1, t1[:], [t1], scale=1.5957691216057308)
                            yield
                            op("dve", lambda h: h.tensor_tensor(out=t1[:], in0=t1[:], in1=p[:], op=ALU.mult), R=[t1, p], W=[t1])
                            yield
                            ys = yst[st_i[0] % 2]; ssm = so[st_i[0] % 2]; st_i[0] += 1
                            op("dve", lambda h: h.tensor_tensor(out=ys[:], in0=t1[:], in1=t4[:], op=ALU.mult), R=[t1, t4], W=[ys])
                            yield
                            c0 = tok0 - NP
                            defer_store(pend_rg, lambda ys=ys, ssm=ssm, ch=ch, c0=c0: dma(yrgT_d[ch * 128:(ch + 1) * 128, c0:c0 + TT], ys[:], ssm, R=[ys]))
                            yield

            def gen_dnt(ti):
                own = ti >= npre; tok0 = ti * TT; hTt = hTs[ti % 2]
                qT = qTs[ti % 2]; kT = kTs[ti % 2]; vtok = vtoks[ti % 2]; ktok = ktoks[ti % 2]; zw = zws[ti % 2]; ba = bas[ti % 2]
                if ti == npre:
                    op("dve", lambda h: h.tensor_scalar_mul(out=carry[:, 8:32, :], in0=carry[:, 8:32, :], scalar1=flag[:, 0:1]), R=[carry, flag], W=[carry])
                def gen_head(hh, k):
                    t1 = d1[k]; t2 = d2[k]; t3 = d3[k]; sqb = sqbD[k]; vT = vTD[k]
                    for which, dstT in ((0, qT), (1, kT), (2, vT)):
                        cid = which * 8 + hh
                        p = proj_fm(next_slab((OFF_Q, OFF_K, OFF_V)[which] + hh * 128), hTt, ptile=pA[k])
                        yield
                        conv(p, 8 + cid, dncw[:, cid, :], None, t1[:], t1, pre=preD[k])
                        yield
                        if which == 2:
                            act_sigmoid(t2[:], t2, t1[:], [t1])
                            yield
                            op("dve", lambda h: h.tensor_tensor(out=vT[:], in0=t1[:], in1=t2[:], op=ALU.mult), R=[t1, t2], W=[vT])
                            yield
                            S.group("pe", [lambda h, c=c: h.transpose(out=pT[0:64, c * 128:(c + 1) * 128], in_=vT[:, c * 64:(c + 1) * 64], identity=ident_b[:]) for c in range(NCH)], R=[vT, ident_b], W=[pT])
                            op("dve", lambda h: h.tensor_copy(out=vtok[:, :, hh, :], in_=pT[0:64, 0:NCH * 128].rearrange("p (c d) -> p c d", c=NCH)), R=[pT], W=[vtok])
                            yield
                        else:
                            act_sigmoid(t2[:], t2, t1[:], [t1])
                            yield
                            op("dve", lambda h: h.tensor_tensor(out=t2[:], in0=t1[:], in1=t2[:], op=ALU.mult), R=[t1, t2], W=[t2])
                            yield
                            op("pool", lambda h: h.tensor_tensor(out=sqb[:], in0=t2[:], in1=t2[:], op=ALU.mult), R=[t2], W=[sqb])
                            yield
                            pn = pA[k]
                            op("pe", lambda h: h.matmul(pn[:], lhsT=ones_b[:], rhs=sqb[:], start=True, stop=True), R=[ones_b, sqb], W=[pn])
                            yield
                            op("dve", lambda h: h.tensor_scalar_add(out=t3[:], in0=pn[:], scalar1=EPS), R=[pn], W=[t3])
                            yield
                            act_rsqrt(t3[:], t3)
                            yield
                            sc = (128.0 ** -0.5) if which == 0 else 1.0
                            op("dve", lambda h: h.scalar_tensor_tensor(out=dstT[:, hh, :], in0=t2[:], scalar=sc, in1=t3[:], op0=ALU.mult, op1=ALU.mult), R=[t2, t3], W=[dstT])
                            yield
                            if which == 1:
                                S.group("pe", [lambda h, c=c: h.transpose(out=pT[0:64, c * 128:(c + 1) * 128], in_=kT[:, hh, c * 64:(c + 1) * 64], identity=ident_b[:]) for c in range(NCH)], R=[kT, ident_b], W=[pT])
                                op("dve", lambda h: h.tensor_copy(out=ktok[:, :, hh, :], in_=pT[0:64, 0:NCH * 128].rearrange("p (c d) -> p c d", c=NCH)), R=[pT], W=[ktok])
                                yield
                yield from interleave_gen([chain(*[gen_head(hh, 0) for hh in (0, 2, 4, 6)]), chain(*[gen_head(hh, 1) for hh in (1, 3, 5, 7)])])
                sl = next_slab()
                for c in range(NCH):
                    S.group("pe", [lambda h, kc=kc: h.matmul(pA[0][:][0:64, 0:16], lhsT=hTt[:, kc, c * 64:(c + 1) * 64], rhs=sl[:, kc, 0:16], start=(kc == 0), stop=(kc == 7)) for kc in range(8)], R=[hTt, sl], W=[pA[0]])
                    op("act", lambda h: h.activation(out=ba[:, c, :], in_=pA[0][:][0:64, 0:16], func=AF.Copy), R=[pA[0]], W=[ba])
                    yield
                if own:
                    for j in range(8):
                        sl = next_slab()
                        for c in range(NCH):
                            S.group("pe", [lambda h, kc=kc: h.matmul(pT[0:64, :].bitcast(F32)[:, c * 128:(c + 1) * 128], lhsT=hTt[:, kc, c * 64:(c + 1) * 64], rhs=sl[:, kc, :], start=(kc == 0), stop=(kc == 7)) for kc in range(8)], R=[hTt, sl], W=[pT])
                            yield
                        act_sigmoid(ztmp[:], ztmp, pT[0:64, :].bitcast(F32)[:, 0:NCH * 128].rearrange("p (c d) -> p c d", c=NCH), [pT])
                        yield
                        op("dve", lambda h: h.tensor_tensor(out=ztmp[:], in0=ztmp[:], in1=pT[0:64, :].bitcast(F32)[:, 0:NCH * 128].rearrange("p (c d) -> p c d", c=NCH), op=ALU.mult), R=[ztmp, pT], W=[ztmp])
                        yield
                        op("pool", lambda h: h.tensor_tensor(out=zw[:, :, j * 128:(j + 1) * 128], in0=ztmp[:], in1=dnw[:, j * 128:(j + 1) * 128].unsqueeze(1).to_broadcast([64, NCH, 128]), op=ALU.mult), R=[ztmp, dnw], W=[zw])

            def gen_ch(ti):
                own = ti >= npre; tok0 = ti * TT
                qT = qTs[ti % 2]; kT = kTs[ti % 2]; vtok = vtoks[ti % 2]; ktok = ktoks[ti % 2]; zw = zws[ti % 2]; ba = bas[ti % 2]
                if ti == npre:
                    op("dve", lambda h: h.tensor_scalar_mul(out=Sf[:], in0=Sf[:], scalar1=flag[:, 0:1]), R=[Sf, flag], W=[Sf])
                    op("act", lambda h: h.activation(out=Sb[:], in_=Sf[:], func=AF.Copy), R=[Sf], W=[Sb])
                for c in range(NCH):
                    cs = slice(c * 64, (c + 1) * 64)
                    act_sigmoid(beta[:], beta, ba[:, c, 0:8], [ba])
                    yield
                    op("dve", lambda h: h.tensor_tensor(out=gstep[:], in0=ba[:, c, 8:16], in1=dtb[:], op=ALU.add), R=[ba, dtb], W=[gstep])
                    yield
                    op("act", lambda h: h.activation(out=gstep[:], in_=gstep[:], func=AF.Exp), R=[gstep], W=[gstep])
                    yield
                    op("act", lambda h: h.activation(out=gstep[:], in_=gstep[:], func=AF.Ln, bias=1.0), R=[gstep], W=[gstep])
                    yield
                    op("dve", lambda h: h.tensor_tensor(out=gstep[:], in0=gstep[:], in1=alog[:], op=ALU.mult), R=[gstep, alog], W=[gstep])
                    yield
                    op("dve", lambda h: h.tensor_tensor(out=Rm[:], in0=maskgt8[:], in1=gstep[:].unsqueeze(2).to_broadcast([64, 8, 64]), op=ALU.mult), R=[maskgt8, gstep], W=[Rm])
                    yield
                    S.group("pe", [lambda h: h.matmul(pX[0:64, :], lhsT=ut[:], rhs=Rm[:].rearrange("p a b -> p (a b)"), start=True, stop=False),
                                   lambda h: h.matmul(pX[0:64, :], lhsT=ident_f[0:64, 0:64], rhs=neg8[:].rearrange("p a b -> p (a b)"), start=False, stop=True),
                                   lambda h: h.matmul(pV[0:64, 0:8], lhsT=ut[:], rhs=gstep[:], start=True, stop=True),
                                   lambda h: h.matmul(pV[:, 8:16], lhsT=ones_f[:], rhs=gstep[:], start=True, stop=True)],
                            R=[ut, Rm, ident_f, neg8, gstep, ones_f], W=[pX, pV])
                    yield
                    op("act", lambda h: h.activation(out=E[:].rearrange("p a b -> p (a b)"), in_=pX[0:64, :], func=AF.Exp), R=[pX], W=[E])
                    yield
                    op("act", lambda h: h.activation(out=Gam[:], in_=pV[0:64, 0:8], func=AF.Exp), R=[pV], W=[Gam])
                    yield
                    op("act", lambda h: h.activation(out=geT[:], in_=pV[:, 8:16], func=AF.Exp), R=[pV], W=[geT])
                    yield
                    op("dve", lambda h: h.tensor_copy(out=gsb[:], in_=pV[0:64, 0:16]), R=[pV], W=[gsb])
                    yield
                    op("dve", lambda h: h.tensor_tensor(out=dec[:], in0=gsb[:, 8:16], in1=gsb[:, 0:8], op=ALU.subtract), R=[gsb], W=[dec])
                    yield
                    op("act", lambda h: h.activation(out=dec[:], in_=dec[:], func=AF.Exp), R=[dec], W=[dec])
                    yield
                    op("dve", lambda h: h.tensor_tensor(out=bG[:], in0=beta[:], in1=Gam[:], op=ALU.mult), R=[beta, Gam], W=[bG])
                    yield
                    op("dve", lambda h: h.tensor_tensor(out=Es[:], in0=E[:], in1=strict8[:], op=ALU.mult), R=[E, strict8], W=[Es])
                    yield
                    op("pool", lambda h: h.tensor_tensor(out=bv[:], in0=vtok[:, c, :, :], in1=beta[:].unsqueeze(2).to_broadcast([64, 8, 128]), op=ALU.mult), R=[vtok, beta], W=[bv])
                    yield
                    op("pool", lambda h: h.tensor_tensor(out=bgk[:], in0=ktok[:, c, :, :], in1=bG[:].unsqueeze(2).to_broadcast([64, 8, 128]), op=ALU.mult), R=[ktok, bG], W=[bgk])
                    yield
                    op("pool", lambda h: h.tensor_tensor(out=kdec[:], in0=ktok[:, c, :, :], in1=dec[:].unsqueeze(2).to_broadcast([64, 8, 128]), op=ALU.mult), R=[ktok, dec], W=[kdec])
                    yield
                    op("pool", lambda h: h.tensor_tensor(out=Sf[:], in0=Sf[:], in1=geT[:].unsqueeze(2).to_broadcast([128, 8, 128]), op=ALU.mult), R=[Sf, geT], W=[Sf])
                    yield
                    S.group("pe", [lambda h, hh=hh: h.matmul(pK[0:64, hh * 64:(hh + 1) * 64], lhsT=kT[:, hh, cs], rhs=kT[:, hh, cs], start=True, stop=True) for hh in range(8)], R=[kT], W=[pK])
                    yield
                    op("dve", lambda h: h.tensor_tensor(out=tA[:].rearrange("p a b -> p (a b)"), in0=pK[0:64, :], in1=Es[:].rearrange("p a b -> p (a b)"), op=ALU.mult), R=[pK, Es], W=[tA])
                    yield
                    op("dve", lambda h: h.tensor_tensor(out=Ak[0][:], in0=tA[:], in1=beta[:].unsqueeze(2).to_broadcast([64, 8, 64]), op=ALU.mult), R=[tA, beta], W=[Ak[0]])
                    yield
                    if own:
                        S.group("pe", [lambda h, hh=hh: h.matmul(pK[0:64, hh * 64:(hh + 1) * 64], lhsT=qT[:, hh, cs], rhs=kT[:, hh, cs], start=True, stop=True) for hh in range(8)], R=[qT, kT], W=[pK])
                        yield
                        op("dve", lambda h: h.tensor_tensor(out=qk[:].rearrange("p a b -> p (a b)"), in0=pK[0:64, :], in1=E[:].rearrange("p a b -> p (a b)"), op=ALU.mult), R=[pK, E], W=[qk])
                        yield
                    S.group("pe", [lambda h, hh=hh: h.transpose(out=pCh[0:64, hh * 64:(hh + 1) * 64], in_=Ak[0][:, hh, :], identity=ident_f[0:64, 0:64]) for hh in range(8)], R=[Ak[0], ident_f], W=[pCh])
                    yield
                    op("act", lambda h: h.activation(out=Bk[0][:].rearrange("p a b -> p (a b)"), in_=pCh[0:64, :], func=AF.Copy), R=[pCh], W=[Bk[0]])
                    yield
                    if own:
                        S.group("pe", [lambda h, hh=hh: h.transpose(out=pX[0:64, 0:256].bitcast(BF16)[:, hh * 64:(hh + 1) * 64], in_=qk[:, hh, :], identity=ident_b[0:64, 0:64]) for hh in range(8)], R=[qk, ident_b], W=[pX])
                        yield
                        op("act", lambda h: h.activation(out=qkT_[:].rearrange("p a b -> p (a b)"), in_=pX[0:64, 0:256].bitcast(BF16), func=AF.Copy), R=[pX], W=[qkT_])
                        yield
                    op("dve", lambda h: h.tensor_tensor(out=Mk[0][:], in0=i8f[:], in1=Bk[0][:], op=ALU.subtract), R=[i8f, Bk[0]], W=[Mk[0]])
                    yield
                    cur = 0
                    for lvl in range(1, 6):
                        a0, b0, m0 = Ak[cur], Bk[cur], Mk[cur]
                        a1, b1, m1 = Ak[1 - cur], Bk[1 - cur], Mk[1 - cur]
                        S.group("pe", [lambda h, hh=hh: h.matmul(pCh[0:64, hh * 64:(hh + 1) * 64], lhsT=b0[:, hh, :], rhs=a0[:, hh, :], start=True, stop=True) for hh in range(8)], R=[a0, b0], W=[pCh])
                        yield
                        op("act", lambda h: h.activation(out=a1[:].rearrange("p a b -> p (a b)"), in_=pCh[0:64, :], func=AF.Copy), R=[pCh], W=[a1])
                        yield
                        if lvl < 5:
                            S.group("pe", [lambda h, hh=hh: h.matmul(pK[0:64, hh * 64:(hh + 1) * 64], lhsT=a0[:, hh, :], rhs=b0[:, hh, :], start=True, stop=True) for hh in range(8)], R=[a0, b0], W=[pK])
                            yield
                            op("dve", lambda h: h.tensor_copy(out=b1[:].rearrange("p a b -> p (a b)"), in_=pK[0:64, :]), R=[pK], W=[b1])
                            yield
                        S.group("pe", [lambda h, hh=hh: h.matmul(pX[0:64, hh * 64:(hh + 1) * 64], lhsT=a1[:, hh, :], rhs=m0[:, hh, :], start=True, stop=True) for hh in range(8)], R=[a1, m0], W=[pX])
                        yield
                        mo = Mb if lvl == 5 else m1
                        op("dve", lambda h: h.tensor_tensor(out=mo[:].rearrange("p a b -> p (a b)"), in0=pX[0:64, :], in1=m0[:].rearrange("p a b -> p (a b)"), op=ALU.add), R=[pX, m0], W=[mo])
                        yield
                        cur = 1 - cur
                    M = Mb
                    S.group("pe", [lambda h, hh=hh: h.matmul(pX[:, hh * 64:(hh + 1) * 64], lhsT=bgk[:, hh, :], rhs=M[:, hh, :], start=True, stop=True) for hh in range(8)], R=[bgk, M], W=[pX])
                    yield
                    op("act", lambda h: h.activation(out=wTn[:].rearrange("p a b -> p (a b)"), in_=pX[:], func=AF.Copy, scale=-1.0), R=[pX], W=[wTn])
                    yield
                    if own:
                        op("dve", lambda h: h.tensor_tensor(out=dG[:], in0=i8f[:], in1=Gam[:].unsqueeze(2).to_broadcast([64, 8, 64]), op=ALU.mult), R=[i8f, Gam], W=[dG])
                        yield
                        op("pe", lambda h: h.matmul(pK[:], lhsT=ones_f[:], rhs=dG[:].rearrange("p a b -> p (a b)"), start=True, stop=True), R=[ones_f, dG], W=[pK])
                        yield
                        op("dve", lambda h: h.tensor_tensor(out=qg[:], in0=qT[:, :, cs], in1=pK[:].rearrange("p (a b) -> p a b", a=8), op=ALU.mult), R=[qT, pK], W=[qg])
                        yield
                    fns = []
                    for hh in range(8):
                        fns.append(lambda h, hh=hh: h.matmul(pV[0:64, hh * 128:(hh + 1) * 128], lhsT=M[:, hh, :], rhs=bv[:, hh, :], start=True, stop=False))
                        fns.append(lambda h, hh=hh: h.matmul(pV[0:64, hh * 128:(hh + 1) * 128], lhsT=wTn[:, hh, :], rhs=Sb[:, hh, :], start=False, stop=True))
                    S.group("pe", fns, R=[M, bv, wTn, Sb], W=[pV])
                    yield
                    op("act", lambda h: h.activation(out=vnew[:, 0:4, :].rearrange("p a b -> p (a b)"), in_=pV[0:64, 0:512], func=AF.Copy), R=[pV], W=[vnew])
                    yield
                    op("dve", lambda h: h.tensor_copy(out=vnew[:, 4:8, :].rearrange("p a b -> p (a b)"), in_=pV[0:64, 512:1024]), R=[pV], W=[vnew])
                    yield
                    if own:
                        fns = []
                        for hh in range(8):
                            fns.append(lambda h, hh=hh: h.matmul(pV[0:64, hh * 128:(hh + 1) * 128], lhsT=qg[:, hh, :], rhs=Sb[:, hh, :], start=True, stop=False))
                            fns.append(lambda h, hh=hh: h.matmul(pV[0:64, hh * 128:(hh + 1) * 128], lhsT=qkT_[:, hh, :], rhs=vnew[:, hh, :], start=False, stop=True))
                        S.group("pe", fns, R=[qg, Sb, qkT_, vnew], W=[pV])
                        yield
                        op("act", lambda h: h.activation(out=osb[:, 0:4, :].rearrange("p a b -> p (a b)"), in_=pV[0:64, 0:512], func=AF.Copy), R=[pV], W=[osb])
                        yield
                        op("dve", lambda h: h.tensor_copy(out=osb[:, 4:8, :].rearrange("p a b -> p (a b)"), in_=pV[0:64, 512:1024]), R=[pV], W=[osb])
                        yield
                        op("pool", lambda h: h.tensor_tensor(out=osq[:], in0=osb[:], in1=osb[:], op=ALU.mult), R=[osb], W=[osq])
                        yield
                        op("dve", lambda h: h.reduce_sum(out=oss[:], in_=osq[:], axis=AX.X), R=[osq], W=[oss])
                        yield
                        op("dve", lambda h: h.tensor_scalar(out=oss[:], in0=oss[:], scalar1=1.0 / 128, scalar2=EPS, op0=ALU.mult, op1=ALU.add), R=[oss], W=[oss])
                        yield
                        act_rsqrt(oss[:], oss)
                        yield
                        op("dve", lambda h: h.tensor_tensor(out=osb[:], in0=osb[:], in1=oss[:].unsqueeze(2).to_broadcast([64, 8, 128]), op=ALU.mult), R=[osb, oss], W=[osb])
                        yield
                        yd = ydst[st_j[0] % 2]; ssm = so[2 + st_j[0] % 2]; st_j[0] += 1
                        op("dve", lambda h: h.tensor_tensor(out=yd[:], in0=osb[:].rearrange("p a b -> p (a b)"), in1=zw[:, c, :], op=ALU.mult), R=[osb, zw], W=[yd])
                        yield
                        r0 = tok0 - NP + c * 64
                        defer_store(pend_ch, lambda yd=yd, ssm=ssm, r0=r0: dma(ydn_d[r0:r0 + 64, :], yd[:], ssm, R=[yd]))
                        yield
                    S.group("pe", [lambda h, hh=hh: h.matmul(pV[:, hh * 128:(hh + 1) * 128], lhsT=kdec[:, hh, :], rhs=vnew[:, hh, :], start=True, stop=True) for hh in range(8)], R=[kdec, vnew], W=[pV])
                    yield
                    op("dve", lambda h: h.tensor_tensor(out=Sf[:, 0:4, :].rearrange("p a b -> p (a b)"), in0=Sf[:, 0:4, :].rearrange("p a b -> p (a b)"), in1=pV[:, 0:512], op=ALU.add), R=[Sf, pV], W=[Sf])
                    yield
                    op("dve", lambda h: h.tensor_tensor(out=Sf[:, 4:8, :].rearrange("p a b -> p (a b)"), in0=Sf[:, 4:8, :].rearrange("p a b -> p (a b)"), in1=pV[:, 512:1024], op=ALU.add), R=[Sf, pV], W=[Sf])
                    yield
                    op("act", lambda h: h.activation(out=Sb[:], in_=Sf[:], func=AF.Copy), R=[Sf], W=[Sb])
                    yield

            def interleave(gens):
                gens = list(gens)
                while gens:
                    for g_ in list(gens):
                        try:
                            next(g_)
                        except StopIteration:
                            gens.remove(g_)

            def chain(*gs):
                for g_ in gs:
                    yield from g_

            def gen_prep(ti):
                emit_prep(ti)
                yield

            def interleave_gen(gens):
                gens = list(gens)
                while gens:
                    for g_ in list(gens):
                        try:
                            next(g_)
                            yield
                        except StopIteration:
                            gens.remove(g_)

            emit_prep(0)
            interleave([chain(gen_rg(0), gen_dnt(0))])
            for ti in range(ntile):
                gs = [gen_ch(ti)]
                if ti + 1 < ntile:
                    gs.append(chain(gen_prep(ti + 1), gen_rg(ti + 1), gen_dnt(ti + 1)))
                interleave(gs)
            for pend in (pend_rg, pend_ch):
                while pend:
                    pend.pop(0)()
            S.barrier()

        NSUB = NO // 128
        wte = sb(st, "wte", [128, NSUB, NE])
        if phases >= 2:
          with ExitStack() as s2:
            wbrg = sb(s2, "wbrg", [128, 8, D], BF16); wbdn = sb(s2, "wbdn", [128, 8, D], BF16); wout = sb(s2, "wout", [128, 8, D], BF16)
            wgt = sb(s2, "wgt", [128, 8, 2048], BF16); wr = sb(s2, "wr", [128, 8, 36], BF16); br = sb(s2, "br", [128, 36])
            stgsem = S.newsem("stg")
            with ExitStack() as s2a:
                stg = sb(s2a, "stg", [128, 8, D]); wrf = sb(s2a, "wrf", [128, 8, 36])
                dma(wrf[:], w_r36.rearrange("(kc p) n -> p kc n", p=128), ld, W=[wrf])
                dma(br[:], b_r36_d, ld, W=[br])
                dma(wgt[:], w_in_bf[:, OFF_GRG:OFF_GRG + 2048].rearrange("(kc p) n -> p kc n", p=128), ld, W=[wgt])
                S.barrier()
                op("dve", lambda h: h.tensor_copy(out=wr[:], in_=wrf[:]), R=[wrf], W=[wr])
                for i_, (dst, src) in enumerate(((wbrg, w_brg), (wbdn, w_bdn), (wout, w_out))):
                    dma(stg[:], src.rearrange("(kc p) n -> p kc n", p=128), stgsem, W=[stg])
                    op(("dve", "pool", "dve")[i_], lambda h: h.tensor_copy(out=dst[:], in_=stg[:]), R=[stg], W=[dst])
                S.barrier()
            T2 = 512
            gate1 = sb(s2, "gate1", [128, D]); dma(gate1[:], g12_d[0], gsem, W=[gate1])
            xt4 = sb(s2, "xt4", [128, 4, D]); x4sem = S.newsem("x4")
            junk = sb(s2, "junk2", [128, D], BF16); ss = sb(s2, "ss2", [128, 1]); xn = sb(s2, "xn2", [128, D], BF16); tmpf = sb(s2, "tmpf2", [128, 8, 128])
            hT = sb(s2, "hT2", [128, 8, T2], BF16); sgr = sb(s2, "sgr", [128, 8, T2], BF16); sgd = sb(s2, "sgd", [128, 8, T2], BF16)
            yrgT = sb(s2, "yrgT2", [128, 8, T2], BF16); ysem = S.newsem("yr2")
            ydn = sb(s2, "ydn2", [128, 4, D], BF16); ydsem = S.newsem("yd2"); ydnT = sb(s2, "ydnT", [128, 8, T2], BF16)
            mg = sb(s2, "mg", [128, 8, T2], BF16); m1 = sb(s2, "m1", [128, T2]); m2 = sb(s2, "m2", [128, T2])
            x2 = [sb(s2, "x2_%d" % i, [128, D]) for i in range(2)]; x2sem = [S.newsem("x2s%d" % i) for i in range(2)]
            h2T = sb(s2, "h2T", [128, 8, T2], BF16); h2sem = S.newsem("h2s")
            rt = []
            for i_ in range(4):
                rt.append((sb(s2, "lg%d" % i_, [128, 36]), sb(s2, "gmx%d" % i_, [128, 1]), sb(s2, "ngm%d" % i_, [128, 1]), sb(s2, "gex%d" % i_, [128, 4]), sb(s2, "gsum%d" % i_, [128, 1]),
                           sb(s2, "oh%d" % i_, [128, 4]), sb(s2, "elm%d" % i_, [128, 4, 8]), sb(s2, "m8%d" % i_, [128, 8]), sb(s2, "dd%d" % i_, [128, 1]), sb(s2, "w12%d" % i_, [128, 2]),
                           sb(s2, "mm1%d" % i_, [128, 32]), sb(s2, "mm2%d" % i_, [128, 32])))

            def interleave2(gens):
                gens = list(gens)
                while gens:
                    for g_ in list(gens):
                        try:
                            next(g_)
                        except StopIteration:
                            gens.remove(g_)
            pT = ps(s2, "pT2", [128, 1024], BF16); pP = [ps(s2, "pP%d" % i, [128, T2]) for i in range(2)]
            pO = ps(s2, "pO", [128, D]); pR = ps(s2, "pR", [128, 64])
            pp_i = [0]
            for ti in range(NO // T2):
                c0 = ti * T2
                dma(xt4[:], xo[c0:c0 + T2, :].rearrange("(s p) d -> p s d", p=128), x4sem, W=[xt4])
                dma(yrgT[:], yrgT_d[:, c0:c0 + T2].rearrange("(c p) n -> p c n", p=128), ysem, W=[yrgT])
                dma(ydn[:], ydn_d[c0:c0 + T2, :].rearrange("(s p) d -> p s d", p=128), ydsem, W=[ydn])
                for sub in range(4):
                    xs = TV3(xt4, sub)
                    norm_to_hT(xs, junk, ss, xn, pT, hT, sub * 128, A1, cols[:, 0, :], tmpf)
                for sub in range(4):
                    S.group("pe", [lambda h, kc=kc: h.transpose(out=pT[:, kc * 128:(kc + 1) * 128], in_=ydn[:, sub, kc * 128:(kc + 1) * 128], identity=ident_b[:]) for kc in range(8)], R=[ydn, ident_b], W=[pT])
                    op("act", lambda h: h.activation(out=ydnT[:, :, sub * 128:(sub + 1) * 128], in_=pT[:].rearrange("p (a b) -> p a b", a=8), func=AF.Copy), R=[pT], W=[ydnT])
                for gi, sg in ((0, sgr), (1, sgd)):
                    for oc in range(8):
                        p = pP[pp_i[0] % 2]; pp_i[0] += 1
                        S.group("pe", [lambda h, kc=kc: h.matmul(p[:], lhsT=wgt[:, kc, gi * 1024 + oc * 128:gi * 1024 + (oc + 1) * 128], rhs=hT[:, kc, :], start=(kc == 0), stop=(kc == 7)) for kc in range(8)], R=[wgt, hT], W=[p])
                        op("act", lambda h: h.activation(out=sg[:, oc, :], in_=p[:], func=AF.Sigmoid), R=[p], W=[sg])
                for oc in range(8):
                    p = pP[pp_i[0] % 2]; pp_i[0] += 1
                    S.group("pe", [lambda h, kc=kc: h.matmul(p[:], lhsT=wbrg[:, kc, oc * 128:(oc + 1) * 128], rhs=yrgT[:, kc, :], start=(kc == 0), stop=(kc == 7)) for kc in range(8)], R=[wbrg, yrgT], W=[p])
                    op("dve", lambda h: h.tensor_tensor(out=m1[:], in0=p[:], in1=sgr[:, oc, :], op=ALU.mult), R=[p, sgr], W=[m1])
                    p = pP[pp_i[0] % 2]; pp_i[0] += 1
                    S.group("pe", [lambda h, kc=kc: h.matmul(p[:], lhsT=wbdn[:, kc, oc * 128:(oc + 1) * 128], rhs=ydnT[:, kc, :], start=(kc == 0), stop=(kc == 7)) for kc in range(8)], R=[wbdn, ydnT], W=[p])
                    op("dve", lambda h: h.tensor_tensor(out=m2[:], in0=p[:], in1=sgd[:, oc, :], op=ALU.mult), R=[p, sgd], W=[m2])
                    op("pool", lambda h: h.tensor_tensor(out=mg[:, oc, :], in0=m1[:], in1=m2[:], op=ALU.add), R=[m1, m2], W=[mg])
                routes = []
                for sub in range(4):
                    r0 = c0 + sub * 128
                    lg, gmx, ngm, gex, gsum, oh, elm, m8, dd, w12, mm1, mm2 = rt[sub]
                    fns = []
                    for hf in range(2):
                        fns += [lambda h, kc=kc, hf=hf: h.matmul(pO[:, hf * 512:(hf + 1) * 512], lhsT=mg[:, kc, sub * 128:(sub + 1) * 128], rhs=wout[:, kc, hf * 512:(hf + 1) * 512], start=(kc == 0), stop=(kc == 7)) for kc in range(8)]
                    S.group("pe", fns, R=[mg, wout], W=[pO])
                    xx = x2[sub % 2]
                    for hf in range(2):
                        op("dve", lambda h: h.tensor_tensor(out=xx[:, hf * 512:(hf + 1) * 512], in0=pO[:, hf * 512:(hf + 1) * 512], in1=gate1[:, hf * 512:(hf + 1) * 512], op=ALU.mult), R=[pO, gate1], W=[xx])
                    op("pool", lambda h: h.tensor_tensor(out=xx[:], in0=xx[:], in1=xt4[:, sub, :], op=ALU.add), R=[xx, xt4], W=[xx])
                    dma(x2_d[r0:r0 + 128, :], xx[:], x2sem[sub % 2], R=[xx])
                    norm_to_hT(xx, junk, ss, xn, pT, h2T, sub * 128, A2, cols[:, 2, :], tmpf)
                    S.group("pe", [lambda h, kc=kc: h.matmul(pR[:, 0:36], lhsT=h2T[:, kc, sub * 128:(sub + 1) * 128], rhs=wr[:, kc, :], start=(kc == 0), stop=(kc == 7)) for kc in range(8)], R=[h2T, wr], W=[pR])
                    op("dve", lambda h: h.tensor_tensor(out=lg[:], in0=pR[:, 0:36], in1=br[:], op=ALU.add), R=[pR, br], W=[lg])
                    def route(sub, lg=lg, gmx=gmx, ngm=ngm, gex=gex, gsum=gsum, oh=oh, elm=elm, m8=m8, dd=dd, w12=w12, mm1=mm1, mm2=mm2):
                        op("dve", lambda h: h.reduce_max(out=gmx[:], in_=lg[:, 0:4], axis=AX.X), R=[lg], W=[gmx])
                        yield
                        op("dve", lambda h: h.tensor_scalar_mul(out=ngm[:], in0=gmx[:], scalar1=-1.0), R=[gmx], W=[ngm])
                        yield
                        op("act", lambda h: h.activation(out=gex[:], in_=lg[:, 0:4], func=AF.Exp, bias=ngm[:], accum_out=gsum[:]), R=[lg, ngm], W=[gex, gsum])
                        yield
                        op("dve", lambda h: h.reciprocal(out=gsum[:], in_=gsum[:]), R=[gsum], W=[gsum])
                        yield
                        op("dve", lambda h: h.tensor_scalar(out=oh[:], in0=lg[:, 0:4], scalar1=gmx[:], scalar2=1.0e9, op0=ALU.is_equal, op1=ALU.mult), R=[lg, gmx], W=[oh])
                        yield
                        op("dve", lambda h: h.tensor_scalar_add(out=oh[:], in0=oh[:], scalar1=-1.0e9), R=[oh], W=[oh])
                        yield
                        op("dve", lambda h: h.tensor_tensor(out=elm[:], in0=lg[:, 4:36].rearrange("p (a b) -> p a b", a=4), in1=oh[:].unsqueeze(2).to_broadcast([128, 4, 8]), op=ALU.add), R=[lg, oh], W=[elm])
                        yield
                        op("dve", lambda h: h.max(out=m8[:], in_=elm[:].rearrange("p a b -> p (a b)")), R=[elm], W=[m8])
                        yield
                        op("dve", lambda h: h.tensor_tensor(out=dd[:], in0=m8[:, 1:2], in1=m8[:, 0:1], op=ALU.subtract), R=[m8], W=[dd])
                        yield
                        op("act", lambda h: h.activation(out=dd[:], in_=dd[:], func=AF.Exp), R=[dd], W=[dd])
                        yield
                        op("dve", lambda h: h.tensor_scalar_add(out=w12[:, 0:1], in0=dd[:], scalar1=1.0), R=[dd], W=[w12])
                        yield
                        op("dve", lambda h: h.reciprocal(out=w12[:, 0:1], in_=w12[:, 0:1]), R=[w12], W=[w12])
                        yield
                        op("dve", lambda h: h.tensor_tensor(out=w12[:, 0:1], in0=w12[:, 0:1], in1=gsum[:], op=ALU.mult), R=[w12, gsum], W=[w12])
                        yield
                        op("dve", lambda h: h.tensor_tensor(out=w12[:, 1:2], in0=w12[:, 0:1], in1=dd[:], op=ALU.mult), R=[w12, dd], W=[w12])
                        yield
                        op("dve", lambda h: h.tensor_scalar(out=mm1[:], in0=elm[:].rearrange("p a b -> p (a b)"), scalar1=m8[:, 0:1], scalar2=w12[:, 0:1], op0=ALU.is_equal, op1=ALU.mult), R=[elm, m8, w12], W=[mm1])
                        yield
                        op("dve", lambda h: h.tensor_scalar(out=mm2[:], in0=elm[:].rearrange("p a b -> p (a b)"), scalar1=m8[:, 1:2], scalar2=w12[:, 1:2], op0=ALU.is_equal, op1=ALU.mult), R=[elm, m8, w12], W=[mm2])
                        yield
                        op("dve", lambda h: h.tensor_tensor(out=wte[:, ti * 4 + sub, :], in0=mm1[:], in1=mm2[:], op=ALU.add), R=[mm1, mm2], W=[wte])
                        yield
                    routes.append(route(sub))
                interleave2(routes)
                dma(h2T_d[:, c0:c0 + T2].rearrange("(c p) n -> p c n", p=128), h2T[:], h2sem, R=[h2T])
            S.barrier()

        if phases >= 3:
          with ExitStack() as s3:
            Q = min(1024, NO); NQS = Q // 128
            h2q = sb(s3, "h2q", [128, 8, Q], BF16); hqsem = S.newsem("hq")
            acc = sb(s3, "acc", [128, NQS, D])
            wgf = sb(s3, "wgf", [128, 8, DE]); wuf = sb(s3, "wuf", [128, 8, DE]); wdf = sb(s3, "wdf", [128, 4, D])
            fsem = [S.newsem("mf%d" % i) for i in range(3)]
            wgb = [sb(s3, "wgb%d" % i, [128, 8, DE], BF16) for i in range(2)]; wub = [sb(s3, "wub%d" % i, [128, 8, DE], BF16) for i in range(2)]
            wdb = [sb(s3, "wdb%d" % i, [128, 4, D], BF16) for i in range(2)]
            sgt = sb(s3, "sgt", [128, 512]); AT = sb(s3, "AT", [128, 4, 512], BF16)
            xf = [sb(s3, "xf%d" % i, [128, D]) for i in range(2)]; xfsem = [S.newsem("xf%d" % i) for i in range(2)]
            ss3 = sb(s3, "ss3", [128, 1]); junk3 = sb(s3, "junk3", [128, D], BF16)
            ob = [sb(s3, "ob%d" % i, [128, D]) for i in range(2)]; osem = [S.newsem("ob%d" % i) for i in range(2)]
            pG = [ps(s3, "pG%d" % i, [128, 512]) for i in range(2)]; pU = [ps(s3, "pU%d" % i, [128, 512]) for i in range(2)]
            pY = [ps(s3, "pY%d" % i, [128, 512]) for i in range(2)]
            gate2 = sb(s3, "gate2", [128, D]); fnw = sb(s3, "fnw", [128, D]); g3sem = S.newsem("g3sem")
            dma(gate2[:], g12_d[1], g3sem, W=[gate2])
            gi_ = [0]; yi_ = [0]
            f3sem = S.newsem("f3sem"); dma(fnw[:], fnw_d, f3sem, W=[fnw])
            for qi in range(NO // Q):
                q0 = qi * Q
                dma(h2q[:], h2T_d[:, q0:q0 + Q].rearrange("(c p) n -> p c n", p=128), hqsem, W=[h2q])
                op("pool", lambda h: h.memset(acc[:], 0.0), W=[acc])
                for e in range(NE):
                    b = e % 2
                    dma(wgf[:], moe_wg[e].rearrange("(kc p) n -> p kc n", p=128), fsem[0], W=[wgf])
                    dma(wuf[:], moe_wu[e].rearrange("(kc p) n -> p kc n", p=128), fsem[1], W=[wuf])
                    dma(wdf[:], moe_wd[e].rearrange("(kc p) n -> p kc n", p=128), fsem[2], W=[wdf])
                    op("act", lambda h: h.activation(out=wgb[b][:], in_=wgf[:], func=AF.Copy), R=[wgf], W=[wgb[b]])
                    op("act", lambda h: h.activation(out=wub[b][:], in_=wuf[:], func=AF.Copy), R=[wuf], W=[wub[b]])
                    op("pool", lambda h: h.tensor_copy(out=wdb[b][:, 0:2, :], in_=wdf[:, 0:2, :]), R=[wdf], W=[wdb[b]])
                    op("dve", lambda h: h.tensor_copy(out=wdb[b][:, 2:4, :], in_=wdf[:, 2:4, :]), R=[wdf], W=[wdb[b]])
                    for hf in range(Q // 512):
                        ts_ = slice(hf * 512, (hf + 1) * 512)
                        for oc in range(4):
                            g_ = pG[gi_[0] % 2]; u_ = pU[gi_[0] % 2]; gi_[0] += 1
                            S.group("pe", [lambda h, kc=kc: h.matmul(g_[:], lhsT=wgb[b][:, kc, oc * 128:(oc + 1) * 128], rhs=h2q[:, kc, ts_], start=(kc == 0), stop=(kc == 7)) for kc in range(8)], R=[wgb[b], h2q], W=[g_])
                            S.group("pe", [lambda h, kc=kc: h.matmul(u_[:], lhsT=wub[b][:, kc, oc * 128:(oc + 1) * 128], rhs=h2q[:, kc, ts_], start=(kc == 0), stop=(kc == 7)) for kc in range(8)], R=[wub[b], h2q], W=[u_])
                            op("act", lambda h: h.activation(out=sgt[:], in_=g_[:], func=AF.Silu), R=[g_], W=[sgt])
                            op("dve", lambda h: h.tensor_tensor(out=AT[:, oc, :], in0=u_[:], in1=sgt[:], op=ALU.mult), R=[u_, sgt], W=[AT])
                        for sub in range(4):
                            si = hf * 4 + sub
                            for ch in range(2):
                                y_ = pY[yi_[0] % 2]; yi_[0] += 1
                                S.group("pe", [lambda h, kc=kc: h.matmul(y_[:], lhsT=AT[:, kc, sub * 128:(sub + 1) * 128], rhs=wdb[b][:, kc, ch * 512:(ch + 1) * 512], start=(kc == 0), stop=(kc == 3)) for kc in range(4)], R=[AT, wdb[b]], W=[y_])
                                gs = qi * NQS + si
                                op("dve", lambda h: h.scalar_tensor_tensor(out=acc[:, si, ch * 512:(ch + 1) * 512], in0=y_[:], scalar=wte[:, gs, e:e + 1], in1=acc[:, si, ch * 512:(ch + 1) * 512], op0=ALU.mult, op1=ALU.add), R=[y_, wte, acc], W=[acc])
                for si in range(NQS):
                    r0 = q0 + si * 128
                    x_ = xf[si % 2]; o_ = ob[si % 2]
                    dma(x_[:], x2_d[r0:r0 + 128, :], xfsem[si % 2], W=[x_])
                    op("pool", lambda h: h.tensor_tensor(out=acc[:, si, :], in0=acc[:, si, :], in1=gate2[:], op=ALU.mult), R=[acc, gate2], W=[acc])
                    op("dve", lambda h: h.tensor_tensor(out=x_[:], in0=x_[:], in1=acc[:, si, :], op=ALU.add), R=[x_, acc], W=[x_])
                    op("act", lambda h: h.activation(out=junk3[:], in_=x_[:], func=AF.Square, scale=1.0 / 32, accum_out=ss3[:]), R=[x_], W=[junk3, ss3])
                    op("dve", lambda h: h.tensor_scalar_add(out=ss3[:], in0=ss3[:], scalar1=EPS), R=[ss3], W=[ss3])
                    op("act", lambda h: h.activation(out=ss3[:], in_=ss3[:], func=AF.Sqrt), R=[ss3], W=[ss3])
                    op("dve", lambda h: h.reciprocal(out=ss3[:], in_=ss3[:]), R=[ss3], W=[ss3])
                    op("dve", lambda h: h.scalar_tensor_tensor(out=o_[:], in0=x_[:], scalar=ss3[:, 0:1], in1=fnw[:], op0=ALU.mult, op1=ALU.mult), R=[x_, ss3, fnw], W=[o_])
                    dma(out_d[r0:r0 + 128, :], o_[:], osem[si % 2], R=[o_])
            S.barrier()

        if dbg and phases == 1:
            with ExitStack() as sd:
                a = sb(sd, "dba", [128, NO], BF16); b = sb(sd, "dbb", [128, D], BF16)
                for ch in range(8):
                    dma(a[:], yrgT_d[ch * 128:(ch + 1) * 128, :], ld, W=[a]); dma(dbg_out["d_yrgT"][ch * 128:(ch + 1) * 128, :], a[:], ld, R=[a])
                for r in range(NO // 128):
                    dma(b[:], ydn_d[r * 128:(r + 1) * 128, :], ld, W=[b]); dma(dbg_out["d_ydn"][r * 128:(r + 1) * 128, :], b[:], ld, R=[b])
                S.barrier()
        if dbg and phases >= 2:
            with ExitStack() as sd:
                b = sb(sd, "dbc", [128, D]); dsa = S.newsem("dsa"); dsb = S.newsem("dsb"); dsc = S.newsem("dsc")
                for r in range(NO // 128):
                    dma(b[:], x2_d[r * 128:(r + 1) * 128, :], dsa, W=[b]); dma(dbg_out["d_x2"][r * 128:(r + 1) * 128, :], b[:], dsb, R=[b])
                    dma(dbg_out["d_wte"][r * 128:(r + 1) * 128, :], wte[:, r, :], dsc, R=[wte])
                S.barrier()
        S.barrier()
    return nc


def _col(v, n=8):
    return np.ascontiguousarray(np.asarray(v, np.float32).reshape(n, 128).T)


def _rep(v, p=128):
    v = np.asarray(v, np.float32).reshape(1, -1)
    return np.ascontiguousarray(np.repeat(v, p, axis=0))


def shared_inputs(I):
    f = lambda a: np.ascontiguousarray(np.asarray(a, np.float32))
    d = {}
    d["w_ada"] = f(I["w_ada"][0]); d["b_ada_rep"] = _rep(I["b_ada"][0])
    d["n1w_col"] = _col(I["norm1_w"][0]); d["n2w_col"] = _col(I["norm2_w"][0]); d["fnw_rep"] = _rep(I["final_norm_w"])
    d["w_in"] = f(I["w_in"][0])
    d["rgcw"] = np.ascontiguousarray(f(I["rg_conv_w"][0]).reshape(4, 8, 128).transpose(2, 1, 0))
    d["rgcb"] = _col(I["rg_conv_b"][0])
    d["dncw"] = np.ascontiguousarray(f(I["dn_conv_w"][0]).reshape(4, 24, 128).transpose(2, 1, 0))
    ga = f(I["rg_gate_a_w"][0]).reshape(4, 2, 128, 256); gx = f(I["rg_gate_x_w"][0]).reshape(4, 2, 128, 256)
    d["rga_w"] = np.ascontiguousarray(ga.transpose(2, 0, 1, 3).reshape(128, 8, 256))
    d["rgx_w"] = np.ascontiguousarray(gx.transpose(2, 0, 1, 3).reshape(128, 8, 256))
    d["rga_b"] = _col(f(I["rg_gate_a_b"][0]).reshape(-1)); d["rgx_b"] = _col(f(I["rg_gate_x_b"][0]).reshape(-1))
    d["lam"] = _col(I["rg_lambda"][0])
    d["alog_rep"] = _rep(I["dn_a_log"][0], 64); d["dtb_rep"] = _rep(I["dn_dt_bias"][0], 64)
    d["dnw_rep"] = _rep(np.tile(f(I["dn_norm_w"][0]), 8), 64)
    d["w_brg"] = f(I["w_branch_rg"][0]); d["w_bdn"] = f(I["w_branch_dn"][0]); d["w_out"] = f(I["w_out"][0])
    d["w_r36"] = np.ascontiguousarray(np.concatenate([f(I["moe_w_group"][0]), f(I["moe_w_router"][0])], axis=1))
    d["b_r36_rep"] = _rep(np.concatenate([f(I["moe_b_group"][0]), f(I["moe_b_router"][0])]))
    d["moe_wg"] = f(I["moe_w_gate"][0]); d["moe_wu"] = f(I["moe_w_up"][0]); d["moe_wd"] = f(I["moe_w_down"][0])
    i = np.arange(64)
    d["c_ident"] = np.eye(128, dtype=np.float32)
    d["c_ut"] = (i[:, None] <= i[None, :]).astype(np.float32)
    d["c_maskgt"] = (i[:, None] > i[None, :]).astype(np.float32)
    d["c_neg"] = np.where(i[None, :] > i[:, None], -30000.0, 0.0).astype(np.float32)
    d["c_strict"] = (i[:, None] > i[None, :]).astype(np.float32)
    return d


def core_inputs(I, shared, b, half, NP, NO):
    x = np.asarray(I["x"], np.float32)
    d = dict(shared)
    own = x[b, half * NO:(half + 1) * NO]
    d["xo"] = np.ascontiguousarray(own)
    d["xp"] = np.ascontiguousarray(x[b, 0:NP]) if half == 1 else np.ascontiguousarray(own[0:NP])
    d["flag"] = np.full((128, 1), float(half), np.float32)
    d["ccol"] = _col(np.asarray(I["c"], np.float32)[b])
    return d


def kernel(**inputs):
    NP = NO = 4096
    nc = build(NP, NO)
    sh = shared_inputs(inputs)
    in_maps = [core_inputs(inputs, sh, b, half, NP, NO) for b in range(4) for half in range(2)]
    res = run_bass_kernel_spmd(nc, in_maps, core_ids=list(range(8)))
    out = np.empty((4, 2 * NO, D), np.float32)
    for i, r in enumerate(res.results):
        b, half = divmod(i, 2)
        out[b, half * NO:(half + 1) * NO] = np.asarray(r["out"], np.float32)
    return out
```
